# Optimizing a Trainium2 kernel written in Bass

```python
import jax, jax.numpy as jnp
from jax import lax
import numpy as np

D_MODEL = 1024
BATCH = 4
SEQ = 8192
DEPTH = 2

CHUNK = 64
PLE_DIM = 256
EPS = 1e-6
POOL_WINDOWS = (2, 4, 8, 16)
POOL_GROUPS = len(POOL_WINDOWS)
POOL_WIDTH = D_MODEL // 4
POOL_GROUP_DIM = POOL_WIDTH // POOL_GROUPS
SGU_BLOCK = 128
SGU_WIDTH = D_MODEL // 4
SGU_GROUPS = 4
SGU_GROUP_DIM = SGU_WIDTH // SGU_GROUPS
HEAD_DIM = 64
ATT_WIDTH = D_MODEL // 2
ATT_HEADS = ATT_WIDTH // HEAD_DIM
LEFT_CHUNKS = 8
BAND = (LEFT_CHUNKS + 1) * CHUNK
REL_CLIP = 128
N_BRANCH = 3
SPLITS = (POOL_WIDTH, SGU_WIDTH, SGU_WIDTH, ATT_WIDTH, ATT_WIDTH, ATT_WIDTH)
IN_WIDTH = sum(SPLITS) + N_BRANCH * D_MODEL
N_GROUPS = 4
EXPERTS_PER_GROUP = 8
N_EXPERTS = N_GROUPS * EXPERTS_PER_GROUP
D_EXPERT = 128
TOP_K = 2

kernel_name = "hybrid_pool_sgu_chunkattn_hmoe_ple"


def rms_norm(x, g):
    xf = x.astype(jnp.float32)
    y = xf * lax.rsqrt(jnp.mean(xf * xf, axis=-1, keepdims=True) + EPS)
    return (y * g.astype(jnp.float32)).astype(x.dtype)


def pool_mixer(a, w_pool, scale):
    B, S, _ = a.shape
    a4 = a.reshape(B, S, POOL_GROUPS, POOL_GROUP_DIM)
    af = a4.astype(jnp.float32)
    cs = jnp.cumsum(af, axis=1)
    pos = jnp.arange(1, S + 1, dtype=jnp.float32)
    means = []
    for gi, w in enumerate(POOL_WINDOWS):
        c = cs[:, :, gi]
        lag = jnp.pad(c, ((0, 0), (w, 0), (0, 0)))[:, :S]
        cnt = jnp.minimum(pos, float(w))[None, :, None]
        means.append((c - lag) / cnt)
    pooled = (jnp.stack(means, axis=2) - af).astype(a.dtype)
    y = jnp.einsum('bsgc,gcd->bsgd', pooled, w_pool)
    return y.reshape(B, S, POOL_WIDTH) * scale


def sgu_mixer(u, v, g_norm, w_s, b_s):
    B, S, _ = u.shape
    nb = S // SGU_BLOCK
    v = rms_norm(v, g_norm)
    vb = v.reshape(B, nb, SGU_BLOCK, SGU_GROUPS, SGU_GROUP_DIM)
    i = jnp.arange(SGU_BLOCK)
    mask = (i[None, :] // CHUNK) <= (i[:, None] // CHUNK)
    w = jnp.where(mask[None], w_s, 0)
    mixed = jnp.einsum('gij,bnjgc->bnigc', w, vb) + b_s.T[None, None, :, :, None]
    return u * mixed.reshape(B, S, SGU_WIDTH)


def chunk_attention(q, k, v, gq, gk, rel_bias):
    B, S, H, Dh = q.shape
    nc = S // CHUNK
    q = rms_norm(q, gq)
    k = rms_norm(k, gk)
    pad = LEFT_CHUNKS * CHUNK
    kp = jnp.pad(k, ((0, 0), (pad, 0), (0, 0), (0, 0))).reshape(B, nc + LEFT_CHUNKS, CHUNK, H, Dh)
    vp = jnp.pad(v, ((0, 0), (pad, 0), (0, 0), (0, 0))).reshape(B, nc + LEFT_CHUNKS, CHUNK, H, Dh)
    qc = q.reshape(B, nc, CHUNK, H, Dh)
    band_idx = jnp.arange(nc)[:, None] + jnp.arange(LEFT_CHUNKS + 1)[None, :]
    valid = jnp.repeat(band_idx >= LEFT_CHUNKS, CHUNK, axis=1)
    rel = jnp.clip(jnp.arange(CHUNK)[:, None] + pad - jnp.arange(BAND)[None, :],
                   -REL_CLIP, REL_CLIP) + REL_CLIP
    bias = rel_bias[:, rel].astype(jnp.float32)
    scale = HEAD_DIM ** -0.5

    def one_sequence(args):
        qb, kb, vb = args
        kband = kb[band_idx].reshape(nc, BAND, H, Dh)
        vband = vb[band_idx].reshape(nc, BAND, H, Dh)
        s = jnp.einsum('nqhd,nkhd->nhqk', qb, kband).astype(jnp.float32) * scale + bias[None]
        s = jnp.where(valid[:, None, None, :], s, -1e30)
        pr = jax.nn.softmax(s, axis=-1).astype(vb.dtype)
        return jnp.einsum('nhqk,nkhd->nqhd', pr, vband)

    o = lax.map(one_sequence, (qc, kp, vp))
    return o.reshape(B, S, H * Dh)


def hier_moe(h, w_gr, b_gr, w_er, b_er, w_e_in, w_e_out):
    def per_sequence(hs):
        S = hs.shape[0]
        g_prob = jax.nn.softmax((hs @ w_gr).astype(jnp.float32) + b_gr, axis=-1)
        g_p, g_idx = lax.top_k(g_prob, 1)
        e_logits = ((hs @ w_er).astype(jnp.float32) + b_er).reshape(S, N_GROUPS, EXPERTS_PER_GROUP)
        e_logits = jnp.einsum('sge,sg->se', e_logits,
                              jax.nn.one_hot(g_idx[:, 0], N_GROUPS, dtype=jnp.float32))
        e_prob = jax.nn.softmax(e_logits, axis=-1)
        e_p, e_idx = lax.top_k(e_prob, TOP_K)
        w = g_p * e_p / jnp.sum(e_p, axis=-1, keepdims=True)
        gid = g_idx * EXPERTS_PER_GROUP + e_idx
        gates = jnp.einsum('sk,ske->se', w, jax.nn.one_hot(gid, N_EXPERTS, dtype=jnp.float32))
        hu = jnp.einsum('sd,edf->sef', hs, w_e_in)
        gt, up = jnp.split(hu, 2, axis=-1)
        act = jax.nn.silu(gt) * up * gates[:, :, None].astype(hs.dtype)
        return jnp.einsum('sef,efd->sd', act, w_e_out)
    return lax.map(per_sequence, h)


def setup_inputs(seed: int = 0) -> dict:
    key = jax.random.key(seed)
    ks = iter(jax.random.split(key, 32))
    f32 = jnp.float32

    def nrm(shape, scale):
        return jax.random.normal(next(ks), shape, f32) * scale

    L = DEPTH
    return {
        "x": nrm((BATCH, SEQ, D_MODEL), 1.0),
        "p": nrm((DEPTH, BATCH, SEQ, PLE_DIM), 1.0),
        "mix_norm_g": 1.0 + nrm((L, D_MODEL), 0.05),
        "w_in": nrm((L, D_MODEL, IN_WIDTH), D_MODEL ** -0.5),
        "pool_w": nrm((L, POOL_GROUPS, POOL_GROUP_DIM, POOL_GROUP_DIM), POOL_GROUP_DIM ** -0.5),
        "pool_scale": 1.0 + nrm((L, POOL_WIDTH), 0.1),
        "sgu_norm_g": 1.0 + nrm((L, SGU_WIDTH), 0.05),
        "sgu_w": nrm((L, SGU_GROUPS, SGU_BLOCK, SGU_BLOCK), SGU_BLOCK ** -0.5),
        "sgu_b": 1.0 + nrm((L, SGU_GROUPS, SGU_BLOCK), 0.1),
        "q_norm_g": 1.0 + nrm((L, HEAD_DIM), 0.05),
        "k_norm_g": 1.0 + nrm((L, HEAD_DIM), 0.05),
        "rel_bias": nrm((L, ATT_HEADS, 2 * REL_CLIP + 1), 0.5),
        "gate_b": nrm((L, N_BRANCH, D_MODEL), 0.1),
        "w_branch_a": nrm((L, POOL_WIDTH, D_MODEL), POOL_WIDTH ** -0.5),
        "w_branch_b": nrm((L, SGU_WIDTH, D_MODEL), SGU_WIDTH ** -0.5),
        "w_branch_c": nrm((L, ATT_WIDTH, D_MODEL), ATT_WIDTH ** -0.5),
        "w_out": nrm((L, D_MODEL, D_MODEL), D_MODEL ** -0.5),
        "ffn_norm_g": 1.0 + nrm((L, D_MODEL), 0.05),
        "w_group_router": nrm((L, D_MODEL, N_GROUPS), D_MODEL ** -0.5),
        "b_group_router": nrm((L, N_GROUPS), 0.01),
        "w_expert_router": nrm((L, D_MODEL, N_EXPERTS), D_MODEL ** -0.5),
        "b_expert_router": nrm((L, N_EXPERTS), 0.01),
        "w_expert_in": nrm((L, N_EXPERTS, D_MODEL, 2 * D_EXPERT), D_MODEL ** -0.5),
        "w_expert_out": nrm((L, N_EXPERTS, D_EXPERT, D_MODEL), D_EXPERT ** -0.5),
        "ple_norm_g": 1.0 + nrm((L, D_MODEL), 0.05),
        "w_ple_in": nrm((L, PLE_DIM, D_MODEL), PLE_DIM ** -0.5),
        "w_ple_gate": nrm((L, D_MODEL, D_MODEL), D_MODEL ** -0.5),
    }


def reference(x, p, mix_norm_g, w_in, pool_w, pool_scale, sgu_norm_g, sgu_w, sgu_b,
              q_norm_g, k_norm_g, rel_bias, gate_b, w_branch_a, w_branch_b, w_branch_c,
              w_out, ffn_norm_g, w_group_router, b_group_router, w_expert_router,
              b_expert_router, w_expert_in, w_expert_out, ple_norm_g, w_ple_in, w_ple_gate):
    B, S, D = x.shape
    cuts = list(np.cumsum(SPLITS))
    for l in range(DEPTH):
        h = rms_norm(x, mix_norm_g[l])
        z = h @ w_in[l]
        a, u, v, q, k, vv, g = jnp.split(z, cuts, axis=-1)
        y_a = pool_mixer(a, pool_w[l], pool_scale[l])
        y_b = sgu_mixer(jax.nn.gelu(u), jax.nn.gelu(v), sgu_norm_g[l], sgu_w[l], sgu_b[l])
        y_c = chunk_attention(q.reshape(B, S, ATT_HEADS, HEAD_DIM),
                              k.reshape(B, S, ATT_HEADS, HEAD_DIM),
                              vv.reshape(B, S, ATT_HEADS, HEAD_DIM),
                              q_norm_g[l], k_norm_g[l], rel_bias[l])
        gates = jax.nn.sigmoid(g.reshape(B, S, N_BRANCH, D) + gate_b[l])
        merged = (gates[:, :, 0] * (y_a @ w_branch_a[l])
                  + gates[:, :, 1] * (y_b @ w_branch_b[l])
                  + gates[:, :, 2] * (y_c @ w_branch_c[l]))
        x = x + merged @ w_out[l]
        x = x + hier_moe(rms_norm(x, ffn_norm_g[l]), w_group_router[l], b_group_router[l],
                         w_expert_router[l], b_expert_router[l], w_expert_in[l], w_expert_out[l])
        x = x + (p[l] @ w_ple_in[l]) * jax.nn.sigmoid(rms_norm(x, ple_norm_g[l]) @ w_ple_gate[l])
    return x
```

```python
import contextlib
import numpy as np
import concourse.bass as bass
import concourse.mybir as mybir
from concourse.bass_utils import run_bass_kernel_spmd

F32 = mybir.dt.float32
BF16 = mybir.dt.bfloat16
AF = mybir.ActivationFunctionType
ALU = mybir.AluOpType
AX = mybir.AxisListType

D = 1024
T = 512
KC = 8
NOWN = 8
EPS = 1e-6
NSLOT = 4
SLOT = 4096
NE = 32
EPP = 8
NPASS = NE // EPP

C_MIXG, C_FFNG, C_PLEG = 0, 8, 16
C_PSCALE, C_INVW = 24, 26
C_GQ, C_GK = 28, 29
C_GATEB = 30
NCOL = 54


class Buf:
    __slots__ = ("name", "w", "r")

    def __init__(self, name):
        self.name = name
        self.w = None
        self.r = {}


class Eng:
    def __init__(self, name, handle, sem, in_order=False):
        self.name = name
        self.h = handle
        self.sem = sem
        self.cnt = 0
        self.waited = {}
        self.in_order = in_order


class Stream:
    def __init__(self, sem):
        self.sem = sem
        self.cnt = 0


class Sched:
    def __init__(self, nc):
        self.nc = nc
        self.n_wait = 0

    def _waits(self, eng, reads, writes):
        need = {}

        def add(ev):
            if ev is None:
                return
            sem, val = ev
            k = id(sem)
            if k not in need or need[k][1] < val:
                need[k] = (sem, val)

        for b in reads:
            add(b.w)
        for b in writes:
            add(b.w)
            for ev in b.r.values():
                add(ev)
        for k, (sem, val) in need.items():
            if sem is eng.sem and eng.in_order:
                continue
            if eng.waited.get(k, 0) >= val:
                continue
            eng.h.wait_ge(sem, val)
            eng.waited[k] = val
            self.n_wait += 1

    def op(self, eng, fn, reads=(), writes=(), last=True):
        self._waits(eng, reads, writes)
        ins = fn()
        ev = (eng.sem, eng.cnt + 1)
        if last:
            eng.cnt += 1
            ins.then_inc(eng.sem, 1)
        for b in reads:
            b.r[id(eng.sem)] = ev
        for b in writes:
            b.w = ev
            b.r = {}
        return ins

    def dma(self, qeng, stream, fn, reads=(), writes=()):
        self._waits(qeng, reads, writes)
        ins = fn()
        stream.cnt += 16
        ins.then_inc(stream.sem, 16)
        ev = (stream.sem, stream.cnt)
        for b in reads:
            b.r[id(stream.sem)] = ev
        for b in writes:
            b.w = ev
            b.r = {}
        return ins


class TB:
    def __init__(self, t, name):
        self.t = t
        self.b = Buf(name)

    def __getitem__(self, idx):
        return self.t[idx]


class Rot:
    def __init__(self, items):
        self.items = items
        self.i = 0

    def next(self):
        it = self.items[self.i % len(self.items)]
        self.i += 1
        return it


class PsumPool:
    def __init__(self, banks):
        self.free = list(banks)

    def alloc(self):
        assert self.free, "psum pool exhausted"
        return self.free.pop(0)

    def release(self, b):
        self.free.append(b)


def unit_sizes():
    u = [4096, 2048, 4096, 4096, 4096]
    u += [4096] * 8
    u += [4096, 4096]
    for _ in range(NPASS):
        u += [4096] * (EPP // 2)
        u += [4096, 4096]
    u += [2048, 4096, 4096]
    return u


def pack_layer_weights(w_in, wa, wb, wc, w_out, we_in, we_out, wple_in, wple_gate):
    def kp(m):
        K = m.shape[0] // 128
        return m.reshape(K, 128, m.shape[1]).transpose(1, 0, 2)

    units = []
    win = kp(w_in)
    units.append(win[:, :, 0:512])
    units.append(win[:, :, 512:768])
    units.append(win[:, :, 768:1280])
    units.append(win[:, :, 1280:1792])
    units.append(win[:, :, 1792:2304])
    wak, wbk, wck = kp(wa), kp(wb), kp(wc)
    for dc in range(8):
        cs = slice(dc * 128, (dc + 1) * 128)
        parts = []
        for br in range(3):
            c0 = 2304 + br * 1024 + dc * 128
            parts.append(win[:, :, c0:c0 + 128].reshape(128, -1))
        parts.append(wak[:, :, cs].reshape(128, -1))
        parts.append(wbk[:, :, cs].reshape(128, -1))
        parts.append(wck[:, :, cs].reshape(128, -1))
        units.append(np.concatenate(parts, axis=1))
    wo = kp(w_out)
    units.append(wo[:, :, 0:512])
    units.append(wo[:, :, 512:1024])
    for pi in range(NPASS):
        for j in range(0, EPP, 2):
            e = pi * EPP + j
            units.append(np.concatenate([kp(we_in[e]).reshape(128, -1),
                                         kp(we_in[e + 1]).reshape(128, -1)], axis=1))
        eo = we_out[pi * EPP:(pi + 1) * EPP].transpose(1, 0, 2)
        units.append(eo[:, :, 0:512])
        units.append(eo[:, :, 512:1024])
    units.append(kp(wple_in))
    wg = kp(wple_gate)
    units.append(wg[:, :, 0:512])
    units.append(wg[:, :, 512:1024])
    flat = [np.ascontiguousarray(u).reshape(128, -1) for u in units]
    sizes = unit_sizes()
    assert [f.shape[1] for f in flat] == sizes, ([f.shape[1] for f in flat], sizes)
    return np.ascontiguousarray(np.concatenate(flat, axis=1), dtype=np.float32)


INTERLEAVE_QK = False


def tile_units(mode):
    if mode == "full":
        n = len(unit_sizes())
        return ([0, 1, 4, 2, 3] if INTERLEAVE_QK else [0, 1, 2, 3, 4]) + list(range(5, n))
    if mode == "kva":
        return [0, 3, 4]
    if mode == "kv":
        return [3, 4]
    return []


def tile_modes(NL, NH, NT):
    modes = {}
    for t in range(NT):
        for l in range(NL):
            if t >= NH:
                m = "full"
            elif NL == 1:
                m = "kva"
            else:
                d = NH - t
                if d == 1:
                    m = "full" if l < NL - 1 else "kva"
                else:
                    m = "kva" if l == 0 else "skip"
            modes[(t, l)] = m
    return modes


def build_program(NL, NH, NOWN=NOWN):
    NT = NH + NOWN
    NTOK = NT * T
    sizes = unit_sizes()
    TOT = sum(sizes)
    offs = np.concatenate([[0], np.cumsum(sizes)]).astype(int)
    modes = tile_modes(NL, NH, NT)

    nc = bass.Bass("TRN2", target_bir_lowering=False)
    xTd = nc.dram_tensor("xT", [D, NTOK], F32, kind="ExternalInput").ap()
    pTd = nc.dram_tensor("pT", [NL, 256, NTOK], F32, kind="ExternalInput").ap()
    wsd = nc.dram_tensor("wstream", [NL, 128, TOT], F32, kind="ExternalInput").ap()
    gcold = nc.dram_tensor("gcols", [NL, 128, NCOL], F32, kind="ExternalInput").ap()
    gnbd = nc.dram_tensor("gnb", [NL, 128, 256], F32, kind="ExternalInput").ap()
    bTd = nc.dram_tensor("sgubT", [NL, 128, 256], F32, kind="ExternalInput").ap()
    wmTd = nc.dram_tensor("sguwT", [NL, 128, 512], F32, kind="ExternalInput").ap()
    wbdd = nc.dram_tensor("poolbd", [NL, 128, 256], F32, kind="ExternalInput").ap()
    wrd = nc.dram_tensor("wrouter", [NL, 128, 8 * 36], F32, kind="ExternalInput").ap()
    brd = nc.dram_tensor("brouter", [NL, 128, 36], F32, kind="ExternalInput").ap()
    toepd = nc.dram_tensor("toep", [NL, 128, 8 * 640], F32, kind="ExternalInput").ap()
    flagd = nc.dram_tensor("flags", [128, 56], F32, kind="ExternalInput").ap()
    constd = nc.dram_tensor("consts", [128, 128 + 32 * 128], F32, kind="ExternalInput").ap()
    yTd = nc.dram_tensor("yT", [D, NOWN * T], F32, kind="ExternalOutput").ap()
    escr = nc.dram_tensor("escratch", [NL, 128, 8 * 640], BF16).ap()

    es = contextlib.ExitStack()
    with es:
        def sb(name, shape, dt):
            return TB(es.enter_context(nc.sbuf_tensor(name, shape, dt)), name)

        def views(tb, n):
            return [TB(tb.t, f"{tb.b.name}_{i}") for i in range(n)]

        xT = sb("xT_sb", [128, KC * T], F32)
        xTk = views(xT, KC)
        hT = sb("hT", [128, KC * T], BF16)
        xside = sb("xside", [128, 2 * T], F32)
        xsidek = views(xside, 2)
        hTk = views(hT, KC)
        sqr = Rot([sb(f"sq{i}", [128, T], BF16) for i in range(3)])
        rstd = sb("rstd", [128, T], F32)
        rstdtm = sb("rstdtm", [128, 4], F32)
        aext = sb("aext", [128, 2 * 528], F32)
        sA = sb("sA", [128, 2 * 528], F32)
        sB = sb("sB", [128, 2 * 528], F32)
        pooled = sb("pooled", [128, 2 * T], BF16)
        yaT = sb("yaT", [128, 2 * T], BF16)
        guT = sb("guT", [128, 2 * T], BF16)
        vtr = Rot([sb(f"vt{i}", [128, 256], BF16) for i in range(4)])
        vss = sb("vss", [128, 4], F32)
        vn = sb("vn", [128, 4 * 256], BF16)
        ybT = sb("ybT", [128, 2 * T], BF16)
        qn = sb("qn", [128, 4 * T], BF16)
        qnk = views(qn, 4)
        rsr = Rot([sb(f"rs{i}", [128, T], F32) for i in range(3)])
        kTc = [sb(f"kTc{l}", [128, 4 * 2 * T], BF16) for l in range(NL)]
        kTck = [views(kTc[l], 4) for l in range(NL)]
        Vaug = [sb(f"Vaug{l}", [128, 8 * 768], BF16) for l in range(NL)]
        atail = [sb(f"atail{l}", [128, 32], F32) for l in range(NL)]
        Etab = sb("Etab", [128, 8 * 640], BF16)
        NROT = 4
        esr = Rot([sb(f"es{i}", [128, T], BF16) for i in range(NROT)])
        ptr = Rot([sb(f"pt{i}", [128, T], BF16) for i in range(NROT)])
        rden = rsr.items[0]
        ycT = sb("ycT", [128, 4 * T], BF16)
        ycTk = views(ycT, 4)
        sigr = Rot([sb(f"sig{i}", [128, T], F32) for i in range(3)])
        mtr = Rot([sb(f"mt{i}", [128, T], F32) for i in range(3)])
        merged = sb("merged", [128, KC * T], BF16)
        mergedk = views(merged, KC)
        lg = sb("lg", [128, 36], F32)
        rsm = Rot([sb(f"rsm{i}", [128, 16], F32) for i in range(2)])
        ohg = sb("ohg", [128, 4], F32)
        gexp = sb("gexp", [128, 4], F32)
        le = sb("le", [128, 8], F32)
        m8 = sb("m8", [128, 8], F32)
        ee = sb("ee", [128, 8], F32)
        wsel = sb("wsel", [128, 8], F32)
        gates = Rot([sb(f"gates{i}", [128, 32], F32) for i in range(4)])
        gatesT = sb("gatesT", [32, T], BF16)
        pTs = sb("pTs", [128, 2 * T], BF16)
        ring = [sb(f"wslot{i}", [128, SLOT], BF16) for i in range(NSLOT)]
        ones_bf = sb("ones_bf", [128, 128], BF16)
        bd_ones = sb("bd_ones", [128, 128], BF16)
        ones_f = sb("ones_f", [128, 256], F32)
        ident = sb("ident", [128, 128], F32)
        sel = sb("sel", [32, 32 * 128], BF16)
        flags = sb("flags_sb", [128, 56], F32)
        gcols = [sb(f"gcols{l}", [128, NCOL], F32) for l in range(NL)]
        gnb = [sb(f"gnb{l}", [128, 256], F32) for l in range(NL)]
        bT = [sb(f"bT{l}", [128, 256], F32) for l in range(NL)]
        wmT = [sb(f"wmT{l}", [128, 512], BF16) for l in range(NL)]
        wbd = [sb(f"wbd{l}", [128, 256], BF16) for l in range(NL)]
        wr = [sb(f"wr{l}", [128, 8 * 36], F32) for l in range(NL)]
        brt = [sb(f"br{l}", [128, 36], F32) for l in range(NL)]

        banks = [TB(es.enter_context(nc.psum_tensor(f"ps{i}", [128, T], F32)), f"ps{i}") for i in range(8)]
        psum = PsumPool(banks)

        def sem(name):
            return es.enter_context(nc.semaphore(name))

        PE = Eng("pe", nc.tensor, sem("s_pe"), in_order=True)
        ACT = Eng("act", nc.scalar, sem("s_act"))
        DVE = Eng("dve", nc.vector, sem("s_dve"))
        POOL = Eng("pool", nc.gpsimd, sem("s_pool"))
        SP = Eng("sp", nc.sync, sem("s_sp"))
        slot_streams = [Stream(sem(f"s_slot{i}")) for i in range(NSLOT)]
        st_x = [Stream(sem(f"s_x{k}")) for k in range(KC)]
        st_p = Stream(sem("s_p"))
        st_xs = [Stream(sem(f"s_xs{k}")) for k in range(2)]
        st_y = [Stream(sem(f"s_y{k}")) for k in range(KC)]
        st_e = [Stream(sem(f"s_e{i}")) for i in range(2)]
        st_el = Stream(sem("s_el"))
        st_c = Stream(sem("s_c"))
        st_c2 = Stream(sem("s_c2"))

        S = Sched(nc)
        block = es.enter_context(nc.Block())

        def mm(out, lhsT, rhs, start, stop, reads, writes):
            return S.op(PE, lambda: nc.tensor.matmul(out, lhsT=lhsT, rhs=rhs, start=start, stop=stop),
                        reads=[r.b for r in reads], writes=[w.b for w in writes], last=True)

        def act(fn, reads, writes):
            return S.op(ACT, fn, reads=[r.b for r in reads], writes=[w.b for w in writes])

        def dve(fn, reads, writes):
            return S.op(DVE, fn, reads=[r.b for r in reads], writes=[w.b for w in writes])

        def pool(fn, reads, writes):
            return S.op(POOL, fn, reads=[r.b for r in reads], writes=[w.b for w in writes])

        def A_(out, in_, func, **kw):
            return lambda: nc.scalar.activation(out=out, in_=in_, func=func, **kw)

        unit_list = []
        for t in range(NT):
            for li in range(NL):
                for u in tile_units(modes[(t, li)]):
                    unit_list.append((li, u))
        wstate = {"issued": 0, "consumed": 0, "hold": None}

        def issue_loads(upto):
            if wstate["hold"] is not None:
                upto = min(upto, wstate["hold"] + NSLOT)
            while wstate["issued"] < min(upto, len(unit_list)):
                i = wstate["issued"]
                li, u = unit_list[i]
                n = sizes[u]
                slot = ring[i % NSLOT]
                src = wsd[li, :, int(offs[u]):int(offs[u]) + n]
                S.dma(POOL, slot_streams[i % NSLOT],
                      lambda slot=slot, src=src, n=n: nc.gpsimd.dma_start(out=slot[:, 0:n], in_=src,
                                                                            max_dma_last_dim=8192),
                      writes=[slot.b])
                wstate["issued"] += 1

        def next_unit(expect_u):
            i = wstate["consumed"]
            li, u = unit_list[i]
            assert u == expect_u, (u, expect_u)
            issue_loads(i + NSLOT)
            wstate["consumed"] += 1
            return ring[i % NSLOT]

        def body():
            def cload(dst, src):
                S.dma(SP, st_c, lambda: nc.sync.dma_start(out=dst, in_=src), writes=[])
            cload(flags[:, :], flagd[:, :])
            cload(ident[:, :], constd[:, 0:128])
            S.dma(POOL, st_c2, lambda: nc.gpsimd.dma_start(out=sel[:, :], in_=constd[0:32, 128:128 + 32 * 128],
                                                            max_dma_last_dim=8192), writes=[])
            for l in range(NL):
                cload(gcols[l][:, :], gcold[l])
                cload(gnb[l][:, :], gnbd[l])
                cload(bT[l][:, :], bTd[l])
                cload(aext[:, l * 288:(l + 1) * 288], wrd[l])
                cload(brt[l][:, :], brd[l])
            for l in range(NL):
                S.dma(POOL, st_c2, lambda l=l: nc.gpsimd.dma_start(out=wmT[l][:, :], in_=wmTd[l]), writes=[])
                S.dma(POOL, st_c2, lambda l=l: nc.gpsimd.dma_start(out=wbd[l][:, :], in_=wbdd[l]), writes=[])
            issue_loads(NSLOT)
            for e in (PE, ACT, DVE):
                e.h.wait_ge(st_c.sem, st_c.cnt)
                e.h.wait_ge(st_c2.sem, st_c2.cnt)
            dve(lambda: nc.vector.memset(ones_bf[:, :], 1.0), [], [ones_bf])
            dve(lambda: nc.vector.memset(ones_f[:, :], 1.0), [], [ones_f])
            dve(lambda: nc.vector.memset(bd_ones[:, :], 0.0), [], [bd_ones])
            dve(lambda: nc.vector.memset(bd_ones[0:64, 0:64], 1.0), [], [bd_ones])
            dve(lambda: nc.vector.memset(bd_ones[64:128, 64:128], 1.0), [], [bd_ones])
            for l in range(NL):
                dve(lambda l=l: nc.vector.memset(kTc[l][:, :], 0.0), [], kTck[l])
                dve(lambda l=l: nc.vector.memset(Vaug[l][:, :], 0.0), [], [Vaug[l]])
                dve(lambda l=l: nc.vector.memset(atail[l][:, :], 0.0), [], [atail[l]])
                for g in range(4):
                    dve(lambda l=l, g=g: nc.vector.memset(wmT[l][64:128, g * 128:g * 128 + 64], 0.0), [], [wmT[l]])
                for k in range(8):
                    dve(lambda l=l, k=k: nc.vector.tensor_scalar(
                        out=wr[l][:, k * 36:(k + 1) * 36], in0=aext[:, l * 288 + k * 36:l * 288 + (k + 1) * 36],
                        scalar1=gcols[l][:, C_FFNG + k:C_FFNG + k + 1], scalar2=None, op0=ALU.mult),
                        [aext, gcols[l]], [wr[l]])
            Ev = Etab[:, :].rearrange("p (h q) -> p h q", h=8)
            for l in range(NL):
                for h in range(8):
                    stg = sA if h % 2 == 0 else sB
                    S.dma(SP, st_e[h % 2], lambda stg=stg, h=h, l=l: nc.sync.dma_start(
                        out=stg[:, 0:640], in_=toepd[l, :, h * 640:(h + 1) * 640]), writes=[stg.b])
                    act(A_(Etab[:, h * 640:(h + 1) * 640], stg[:, 0:640], AF.Exp), [stg], [Etab])
                dve(lambda: nc.vector.memset(Ev[0:64, :, 576:640], 0.0), [], [Etab])
                dve(lambda: nc.vector.memset(Ev[64:128, :, 0:64], 0.0), [], [Etab])
                S.dma(SP, st_el, lambda l=l: nc.sync.dma_start(out=escr[l], in_=Etab[:, :]), reads=[Etab.b])
            etab_state = {"cur": NL - 1}
            nc.sync.wait_ge(st_el.sem, st_el.cnt)

            hv_col = flags[:, 0:1]
            one_col = flags[:, 1:2]

            def fm_norm(l, cbase, want_tm=False, src=None):
                if src is None:
                    src = lambda k: (xT[:, k * T:(k + 1) * T], xTk[k])
                ps = psum.alloc()
                for k in range(KC):
                    sq = sqr.next()
                    xa, xb = src(k)
                    act(A_(sq[:, :], xa, AF.Square), [xb], [sq])
                    mm(ps[:, :], ones_bf[:, :], sq[:, :], k == 0, k == KC - 1, [sq, ones_bf], [ps])
                act(A_(rstd[:, :], ps[:, :], AF.Ln, scale=1.0 / D, bias=EPS), [ps], [rstd])
                psum.release(ps)
                act(A_(rstd[:, :], rstd[:, :], AF.Exp, scale=-0.5), [rstd], [rstd])
                for k in range(KC):
                    xa, xb = src(k)
                    dve(lambda k=k, xa=xa: nc.vector.scalar_tensor_tensor(
                        out=hT[:, k * T:(k + 1) * T], in0=xa,
                        scalar=gcols[l][:, cbase + k:cbase + k + 1], in1=rstd[:, :],
                        op0=ALU.mult, op1=ALU.mult), [xb, rstd, gcols[l]], [hTk[k]])
                if want_tm:
                    ps2 = psum.alloc()
                    for tb in range(4):
                        mm(ps2[:, tb:tb + 1], rstd[0:1, tb * 128:(tb + 1) * 128], ones_f[0:1, 0:1],
                           True, True, [rstd, ones_f], [ps2])
                    dve(lambda: nc.vector.tensor_copy(out=rstdtm[:, :], in_=ps2[:, 0:4]), [ps2], [rstdtm])
                    psum.release(ps2)

            def proj_fm(w, wbase, wstride, ncols_off, rhs_tb, rhs_bufs, nk, rhs_stride):
                ps = psum.alloc()
                for kk in range(nk):
                    o = wbase + kk * wstride + ncols_off
                    mm(ps[:, :], w[:, o:o + 128], rhs_tb[:, kk * rhs_stride:kk * rhs_stride + T],
                       kk == 0, kk == nk - 1, [w, rhs_bufs[kk]], [ps])
                return ps

            def proj_h(w, wbase, wstride, ncols_off):
                return proj_fm(w, wbase, wstride, ncols_off, hT, hTk, KC, T)

            prefetched = set()
            side_tile = {"t": None}

            def xsrc(t, l, k):
                if l == 0 and side_tile["t"] == t and k >= KC - 2:
                    j = k - (KC - 2)
                    return xside[:, j * T:(j + 1) * T], xsidek[j]
                return xT[:, k * T:(k + 1) * T], xTk[k]

            def load_side(t):
                for j in range(2):
                    k = KC - 2 + j
                    S.dma(SP, st_xs[j], lambda j=j, k=k: nc.sync.dma_start(
                        out=xside[:, j * T:(j + 1) * T], in_=xTd[k * 128:(k + 1) * 128, t * T:(t + 1) * T]),
                        writes=[xsidek[j].b])
                side_tile["t"] = t

            def load_x_chunk(t, k):
                S.dma(SP, st_x[k], lambda: nc.sync.dma_start(
                    out=xT[:, k * T:(k + 1) * T], in_=xTd[k * 128:(k + 1) * 128, t * T:(t + 1) * T]),
                    writes=[xTk[k].b])

            def load_x(t):
                if t in prefetched:
                    return
                for k in range(KC):
                    if side_tile["t"] == t and k >= KC - 2:
                        continue
                    load_x_chunk(t, k)

            def load_p(t, l):
                S.dma(POOL, st_p, lambda: nc.gpsimd.dma_start(
                    out=pTs[:, :].rearrange("p (c t) -> p c t", c=2),
                    in_=pTd[l].rearrange("(c p) t -> p c t", p=128)[:, :, t * T:(t + 1) * T]),
                    writes=[pTs.b])

            def load_E(l):
                if etab_state["cur"] == l:
                    return
                S.dma(SP, st_el, lambda: nc.sync.dma_start(out=Etab[:, :], in_=escr[l]), writes=[Etab.b])
                etab_state["cur"] = l

            def qk_chunks(l, jobs, interleave=False):
                stA = {}
                DEPTH = 2

                def stageA(j):
                    w, c, which, slot_cur = jobs[j]
                    ps = proj_h(w, 0, 512, c * 128)
                    sq = sqr.next()
                    act(A_(sq[:, :], ps[:, :], AF.Square), [ps], [sq])
                    stA[j] = (ps, sq)

                def stageB(j):
                    w, c, which, slot_cur = jobs[j]
                    ps, sq = stA.pop(j)
                    ps2 = psum.alloc()
                    mm(ps2[:, :], bd_ones[:, :], sq[:, :], True, True, [bd_ones, sq], [ps2])
                    rs = rsr.next()
                    act(A_(rs[:, :], ps2[:, :], AF.Ln, scale=1.0 / 64, bias=EPS), [ps2], [rs])
                    psum.release(ps2)
                    act(A_(rs[:, :], rs[:, :], AF.Exp, scale=-0.5), [rs], [rs])
                    if which == 0:
                        dst_tb, dst = qnk[c], qn[:, c * T:(c + 1) * T]
                        gc = gcols[l][:, C_GQ:C_GQ + 1]
                    else:
                        o = (c * 2 + slot_cur) * T
                        dst_tb, dst = kTck[l][c], kTc[l][:, o:o + T]
                        gc = gcols[l][:, C_GK:C_GK + 1]
                    dve(lambda: nc.vector.scalar_tensor_tensor(
                        out=dst, in0=ps[:, :], scalar=gc, in1=rs[:, :], op0=ALU.mult, op1=ALU.mult),
                        [ps, rs, gcols[l]], [dst_tb])
                    psum.release(ps)

                if not interleave:
                    for j in range(len(jobs) + DEPTH):
                        if j < len(jobs):
                            stageA(j)
                        if j >= DEPTH:
                            stageB(j - DEPTH)
                return stageA, stageB

            def v_proj(l, w, slot_cur, flagcol):
                Vv = Vaug[l][:, :].rearrange("p (b c x) -> p b c x", b=8, c=4)
                for tb in range(4):
                    ps = psum.alloc()
                    for kk in range(KC):
                        mm(ps[:, :], hT[:, kk * T + tb * 128:kk * T + (tb + 1) * 128],
                           w[:, kk * 512:(kk + 1) * 512], kk == 0, kk == KC - 1, [hTk[kk], w], [ps])
                    bi = slot_cur * 4 + tb
                    psv = ps[:, :].rearrange("p (c two d) -> p c two d", c=4, two=2)
                    dve(lambda bi=bi, psv=psv: nc.vector.tensor_scalar(
                        out=Vv[:, bi, :, 0:64], in0=psv[:, :, 0, :], scalar1=flagcol, scalar2=None,
                        op0=ALU.mult), [ps, flags], [Vaug[l]])
                    dve(lambda bi=bi, psv=psv: nc.vector.tensor_scalar(
                        out=Vv[:, bi, :, 128:192], in0=psv[:, :, 1, :], scalar1=flagcol, scalar2=None,
                        op0=ALU.mult), [ps, flags], [Vaug[l]])
                    psum.release(ps)
                    act(A_(Vv[:, bi, :, 64:128], ones_f[:, :].rearrange("p (c d) -> p c d", c=4), AF.Copy,
                           scale=flagcol, bias=1e-10), [ones_f, flags], [Vaug[l]])

            def a_proj(w, cc):
                ps = proj_h(w, 0, 512, cc * 128)
                act(A_(aext[:, cc * 528 + 16:cc * 528 + 528], ps[:, :], AF.Copy), [ps], [aext])
                psum.release(ps)

            def save_atail(l):
                for c in range(2):
                    dve(lambda c=c: nc.vector.tensor_copy(out=atail[l][:, c * 16:(c + 1) * 16],
                                                          in_=aext[:, c * 528 + 512:c * 528 + 528]),
                        [aext], [atail[l]])

            def layer_tile_partial(t, l, mode):
                slot_cur = t % 2
                fm_norm(l, C_MIXG)
                if mode == "kva":
                    w = next_unit(0)
                    for cc in range(2):
                        a_proj(w, cc)
                    save_atail(l)
                w = next_unit(3)
                qk_chunks(l, [(w, c, 1, slot_cur) for c in range(4)])
                w = next_unit(4)
                v_proj(l, w, slot_cur, hv_col)

            def layer_tile(t, l, is_halo):
                flagcol = hv_col if is_halo else one_col
                slot_cur = t % 2
                load_E(l)

                fm_norm(l, C_MIXG, src=lambda k: xsrc(t, l, k))

                w = next_unit(0)
                for cc in range(2):
                    a_proj(w, cc)
                for c in range(2):
                    if t == NH:
                        pool(lambda c=c: nc.gpsimd.tensor_tensor(
                            out=aext[:, c * 528:c * 528 + 16], in0=atail[l][:, c * 16:(c + 1) * 16],
                            in1=flags[:, 40:56], op=ALU.mult), [atail[l], flags], [aext])
                    else:
                        pool(lambda c=c: nc.gpsimd.tensor_copy(
                            out=aext[:, c * 528:c * 528 + 16], in_=atail[l][:, c * 16:(c + 1) * 16]),
                            [atail[l]], [aext])

                def shadd(dst, src, c, sh, p0, p1):
                    lo = 2 * sh - 1
                    pool(lambda: nc.gpsimd.tensor_tensor(
                        out=dst[p0:p1, c * 528 + lo:c * 528 + 528],
                        in0=src[p0:p1, c * 528 + lo:c * 528 + 528],
                        in1=src[p0:p1, c * 528 + lo - sh:c * 528 + 528 - sh], op=ALU.add), [src], [dst])

                shadd(sA, aext, 0, 1, 0, 128)
                shadd(sB, sA, 0, 2, 64, 128)
                shadd(sA, aext, 1, 1, 0, 128)
                shadd(sB, sA, 1, 2, 0, 128)
                shadd(sA, sB, 1, 4, 0, 128)
                shadd(sB, sA, 1, 8, 64, 128)
                if t == NH:
                    for c in range(2):
                        for (src, p0, p1) in ((sA, 0, 64), (sB, 64, 128)):
                            pool(lambda c=c, src=src, p0=p0, p1=p1: nc.gpsimd.tensor_tensor(
                                out=src[p0:p1, c * 528 + 16:c * 528 + 32], in0=src[p0:p1, c * 528 + 16:c * 528 + 32],
                                in1=flags[p0:p1, 2 + c * 16:2 + (c + 1) * 16], op=ALU.mult), [src, flags], [src])
                for c in range(2):
                    pool(lambda c=c: nc.gpsimd.tensor_copy(out=atail[l][:, c * 16:(c + 1) * 16],
                                                           in_=aext[:, c * 528 + 512:c * 528 + 528]),
                         [aext], [atail[l]])
                for cc in range(2, 4):
                    ps = proj_h(w, 0, 512, cc * 128)
                    act(A_(guT[:, (cc - 2) * T:(cc - 1) * T], ps[:, :], AF.Gelu_apprx_tanh), [ps], [guT])
                    psum.release(ps)
                w = next_unit(1)
                vts = []
                for tb in range(4):
                    ps = psum.alloc()
                    for kk in range(KC):
                        mm(ps[:, 0:256], hT[:, kk * T + tb * 128:kk * T + (tb + 1) * 128],
                           w[:, kk * 256:(kk + 1) * 256], kk == 0, kk == KC - 1, [hTk[kk], w], [ps])
                    vt = vtr.next()
                    act(A_(vt[:, :], ps[:, 0:256], AF.Gelu_apprx_tanh), [ps], [vt])
                    psum.release(ps)
                    act(A_(rstd[:, 0:256], vt[:, :], AF.Square, accum_out=vss[:, tb:tb + 1]), [vt], [rstd, vss])
                    vts.append(vt)
                act(A_(vss[:, :], vss[:, :], AF.Ln, scale=1.0 / 256, bias=EPS), [vss], [vss])
                act(A_(vss[:, :], vss[:, :], AF.Exp, scale=-0.5), [vss], [vss])
                for tb in range(4):
                    vt = vts[tb]
                    dve(lambda tb=tb, vt=vt: nc.vector.scalar_tensor_tensor(
                        out=vn[:, tb * 256:(tb + 1) * 256], in0=vt[:, :], scalar=vss[:, tb:tb + 1],
                        in1=gnb[l][:, :], op0=ALU.mult, op1=ALU.mult), [vt, vss, gnb[l]], [vn])
                if INTERLEAVE_QK:
                    w = next_unit(4)
                    v_proj(l, w, slot_cur, flagcol)
                    wstate["hold"] = wstate["consumed"]
                    wq = next_unit(2)
                    wk = next_unit(3)
                    qk_jobs = []
                    for c in range(4):
                        qk_jobs += [(wq, c, 0, slot_cur), (wk, c, 1, slot_cur)]
                    qkA, qkB = qk_chunks(l, qk_jobs, interleave=True)
                    qkA(0)
                    qkA(1)
                    qkB(0)
                    qkB(1)
                else:
                    wstate["hold"] = wstate["consumed"]
                    wq = next_unit(2)
                    wk = next_unit(3)
                    qk_chunks(l, [(wq, c, 0, slot_cur) for c in range(4)] + [(wk, c, 1, slot_cur) for c in range(4)])
                    wstate["hold"] = None
                    w = next_unit(4)
                    v_proj(l, w, slot_cur, flagcol)

                steps = []
                for h in range(8):
                    order = [3, 0, 1, 2, 5, 6, 7, 4]
                    for i, wi in enumerate(order):
                        steps.append((h, wi, i == 0, i == len(order) - 1))
                LAG = NROT - 1
                pv_of = {}
                pt_of = {}

                def geom(wi):
                    ca, cb = 2 * wi - 8, 2 * wi - 7
                    qlo = max(ca, 0) * 64
                    qhi = (min(cb + 8, 7) + 1) * 64
                    K0 = (wi - 4) * 128
                    return qlo, qhi, K0

                def emit_score(i):
                    h, wi, first, lastk = steps[i]
                    c, hh = h // 2, h % 2
                    qlo, qhi, K0 = geom(wi)
                    N = qhi - qlo
                    kslot = (1 - slot_cur) if wi < 4 else slot_cur
                    kb = wi % 4
                    ko = (c * 2 + kslot) * T + kb * 128
                    sc = psum.alloc()
                    mm(sc[:, 0:N], kTc[l][hh * 64:(hh + 1) * 64, ko:ko + 128],
                       qn[hh * 64:(hh + 1) * 64, c * T + qlo:c * T + qhi], True, True, [kTck[l][c], qnk[c]], [sc])
                    es_ = esr.next()
                    act(A_(es_[:, 0:N], sc[:, 0:N], AF.Exp, scale=0.125), [sc], [es_])
                    psum.release(sc)
                    pt = ptr.next()
                    eo = h * 640 + qlo - K0
                    if i % 2 == 1:
                        pool(lambda: nc.gpsimd.tensor_tensor(out=pt[:, 0:N], in0=es_[:, 0:N], in1=Etab[:, eo:eo + N],
                                                             op=ALU.mult), [es_, Etab], [pt])
                    else:
                        dve(lambda: nc.vector.tensor_tensor(out=pt[:, 0:N], in0=es_[:, 0:N], in1=Etab[:, eo:eo + N],
                                                            op=ALU.mult), [es_, Etab], [pt])
                    pt_of[i] = pt

                def emit_pv(i):
                    h, wi, first, lastk = steps[i]
                    c, hh = h // 2, h % 2
                    qlo, qhi, K0 = geom(wi)
                    N = qhi - qlo
                    if first:
                        pv_of[h] = psum.alloc()
                    pv = pv_of[h]
                    kslot = (1 - slot_cur) if wi < 4 else slot_cur
                    bi = kslot * 4 + (wi % 4)
                    vo = bi * 768 + c * 192 + (0 if hh == 0 else 64)
                    pt = pt_of.pop(i)
                    mm(pv[:, qlo:qhi], Vaug[l][:, vo:vo + 128], pt[:, 0:N], first, lastk, [Vaug[l], pt], [pv])
                    if lastk:
                        n0, n1 = (0, 64) if hh == 0 else (64, 128)
                        d0, d1 = (64, 128) if hh == 0 else (0, 64)
                        act(A_(rden[n0:n1, :], pv[d0:d1, :], AF.Ln), [pv], [rden])
                        act(A_(rden[n0:n1, :], rden[n0:n1, :], AF.Exp, scale=-1.0), [rden], [rden])
                        dve(lambda: nc.vector.tensor_tensor(out=ycT[n0:n1, c * T:(c + 1) * T], in0=pv[n0:n1, :],
                                                            in1=rden[n0:n1, :], op=ALU.mult), [pv, rden], [ycTk[c]])
                        psum.release(pv)

                def emit_pooled():
                    for c in range(2):
                        for (src, p0, p1) in ((sA, 0, 64), (sB, 64, 128)):
                            dve(lambda c=c, src=src, p0=p0, p1=p1: nc.vector.scalar_tensor_tensor(
                                out=pooled[p0:p1, c * T:(c + 1) * T], in0=src[p0:p1, c * 528 + 16:c * 528 + 528],
                                scalar=gcols[l][p0:p1, C_INVW + c:C_INVW + c + 1],
                                in1=aext[p0:p1, c * 528 + 16:c * 528 + 528], op0=ALU.mult, op1=ALU.subtract),
                                [src, aext, gcols[l]], [pooled])

                inject = {}
                for c in (range(1, 4) if INTERLEAVE_QK else ()):
                    b0 = 16 * (c - 1)
                    inject[b0 + 1] = (qkA, 2 * c)
                    inject[b0 + 5] = (qkA, 2 * c + 1)
                    inject[b0 + 9] = (qkB, 2 * c)
                    inject[b0 + 13] = (qkB, 2 * c + 1)
                for i in range(len(steps) + LAG):
                    if i in inject:
                        inject[i][0](inject[i][1])
                    if i == 24:
                        emit_pooled()
                    if i < len(steps):
                        emit_score(i)
                    if i >= LAG:
                        emit_pv(i - LAG)
                wstate["hold"] = None

                for c in range(2):
                    ps = psum.alloc()
                    mm(ps[:, :], wbd[l][:, c * 128:(c + 1) * 128], pooled[:, c * T:(c + 1) * T], True, True,
                       [wbd[l], pooled], [ps])
                    act(A_(yaT[:, c * T:(c + 1) * T], ps[:, :], AF.Copy,
                           scale=gcols[l][:, C_PSCALE + c:C_PSCALE + c + 1]), [ps, gcols[l]], [yaT])
                    psum.release(ps)

                for c in range(2):
                    ps = psum.alloc()
                    for blk in range(4):
                        for gg in range(2):
                            g = 2 * c + gg
                            mm(ps[gg * 64:(gg + 1) * 64, blk * 128:(blk + 1) * 128],
                               vn[:, blk * 256 + g * 64:blk * 256 + (g + 1) * 64],
                               wmT[l][:, g * 128:(g + 1) * 128], True, True, [vn, wmT[l]], [ps])
                    mt = mtr.next()
                    for blk in range(4):
                        dve(lambda blk=blk, mt=mt, ps=ps, c=c: nc.vector.tensor_tensor(
                            out=mt[:, blk * 128:(blk + 1) * 128], in0=ps[:, blk * 128:(blk + 1) * 128],
                            in1=bT[l][:, c * 128:(c + 1) * 128], op=ALU.add), [ps, bT[l]], [mt])
                    psum.release(ps)
                    dve(lambda mt=mt, c=c: nc.vector.tensor_tensor(
                        out=ybT[:, c * T:(c + 1) * T], in0=mt[:, :], in1=guT[:, c * T:(c + 1) * T], op=ALU.mult),
                        [mt, guT], [ybT])

                ybr = ((yaT, [yaT] * 2), (ybT, [ybT] * 2), (ycT, ycTk))
                nkb = (2, 2, 4)
                boff = (0, 256, 512)
                for dc in range(KC):
                    w = next_unit(5 + dc)
                    macc = None
                    for br in range(3):
                        psg = proj_h(w, br * 1024, 128, 0)
                        sg = sigr.next()
                        act(A_(sg[:, :], psg[:, :], AF.Sigmoid,
                               bias=gcols[l][:, C_GATEB + br * 8 + dc:C_GATEB + br * 8 + dc + 1]),
                            [psg, gcols[l]], [sg])
                        psum.release(psg)
                        psb = proj_fm(w, 3072 + boff[br], 128, 0, ybr[br][0], ybr[br][1], nkb[br], T)
                        if br == 0:
                            macc = mtr.next()
                            dve(lambda psb=psb, sg=sg, macc=macc: nc.vector.tensor_tensor(
                                out=macc[:, :], in0=psb[:, :], in1=sg[:, :], op=ALU.mult), [psb, sg], [macc])
                        else:
                            tmp = mtr.next()
                            dve(lambda psb=psb, sg=sg, tmp=tmp: nc.vector.tensor_tensor(
                                out=tmp[:, :], in0=psb[:, :], in1=sg[:, :], op=ALU.mult), [psb, sg], [tmp])
                            if br == 1:
                                dve(lambda tmp=tmp, macc=macc: nc.vector.tensor_tensor(
                                    out=macc[:, :], in0=macc[:, :], in1=tmp[:, :], op=ALU.add), [macc, tmp], [macc])
                            else:
                                dve(lambda tmp=tmp, macc=macc, dc=dc: nc.vector.tensor_tensor(
                                    out=merged[:, dc * T:(dc + 1) * T], in0=macc[:, :], in1=tmp[:, :], op=ALU.add),
                                    [macc, tmp], [mergedk[dc]])
                        psum.release(psb)
                for half in range(2):
                    w = next_unit(13 + half)
                    for o4 in range(4):
                        oc = half * 4 + o4
                        ps = proj_fm(w, 0, 512, o4 * 128, merged, mergedk, KC, T)
                        xa, xb = xsrc(t, l, oc)
                        dve(lambda ps=ps, oc=oc, xa=xa: nc.vector.tensor_tensor(
                            out=xT[:, oc * T:(oc + 1) * T], in0=xa, in1=ps[:, :], op=ALU.add),
                            [ps, xb, xTk[oc]], [xTk[oc]])
                        psum.release(ps)
                if l == NL - 1 and t >= NH and t + 1 < NT:
                    load_side(t + 1)

                load_p(t, l)

                fm_norm(l, C_FFNG, want_tm=True)
                lps = []
                for tb in range(4):
                    ps = psum.alloc()
                    for kk in range(KC):
                        mm(ps[:, 0:36], xT[:, kk * T + tb * 128:kk * T + (tb + 1) * 128],
                           wr[l][:, kk * 36:(kk + 1) * 36], kk == 0, kk == KC - 1, [xTk[kk], wr[l]], [ps])
                    lps.append(ps)
                gts = []
                for tb in range(4):
                    ps = lps[tb]
                    dve(lambda ps=ps, tb=tb: nc.vector.scalar_tensor_tensor(
                        out=lg[:, :], in0=ps[:, 0:36], scalar=rstdtm[:, tb:tb + 1], in1=brt[l][:, :],
                        op0=ALU.mult, op1=ALU.add), [ps, rstdtm, brt[l]], [lg])
                    psum.release(ps)
                    sm = rsm.next()
                    dve(lambda sm=sm: nc.vector.reduce_max(out=sm[:, 0:1], in_=lg[:, 0:4], axis=AX.X), [lg], [sm])
                    dve(lambda sm=sm: nc.vector.tensor_scalar(out=ohg[:, :], in0=lg[:, 0:4], scalar1=sm[:, 0:1],
                                                               scalar2=None, op0=ALU.is_ge), [lg, sm], [ohg])
                    dve(lambda sm=sm: nc.vector.tensor_scalar(out=sm[:, 1:2], in0=sm[:, 0:1], scalar1=-1.0,
                                                               scalar2=None, op0=ALU.mult), [sm], [sm])
                    act(A_(gexp[:, :], lg[:, 0:4], AF.Exp, bias=sm[:, 1:2], accum_out=sm[:, 2:3]), [lg, sm], [gexp, sm])
                    dve(lambda: nc.vector.tensor_scalar(out=le[:, :], in0=lg[:, 4:12], scalar1=ohg[:, 0:1],
                                                        scalar2=None, op0=ALU.mult), [lg, ohg], [le])
                    for g in range(1, 4):
                        dve(lambda g=g: nc.vector.scalar_tensor_tensor(
                            out=le[:, :], in0=lg[:, 4 + g * 8:12 + g * 8], scalar=ohg[:, g:g + 1], in1=le[:, :],
                            op0=ALU.mult, op1=ALU.add), [lg, ohg, le], [le])
                    dve(lambda: nc.vector.max(out=m8[:, :], in_=le[:, :]), [le], [m8])
                    dve(lambda sm=sm: nc.vector.tensor_scalar(out=sm[:, 3:4], in0=m8[:, 0:1], scalar1=-1.0,
                                                               scalar2=None, op0=ALU.mult), [m8], [sm])
                    act(A_(ee[:, :], le[:, :], AF.Exp, bias=sm[:, 3:4]), [le, sm], [ee])
                    dve(lambda sm=sm: nc.vector.scalar_tensor_tensor(
                        out=wsel[:, :], in0=le[:, :], scalar=m8[:, 1:2], in1=ee[:, :], op0=ALU.is_ge, op1=ALU.mult,
                        accum_out=sm[:, 4:5]), [le, m8, ee], [wsel, sm])
                    dve(lambda sm=sm: nc.vector.tensor_tensor(out=sm[:, 5:6], in0=sm[:, 4:5], in1=sm[:, 2:3],
                                                               op=ALU.mult), [sm], [sm])
                    dve(lambda sm=sm: nc.vector.reciprocal(out=sm[:, 5:6], in_=sm[:, 5:6]), [sm], [sm])
                    gt_ = gates.next()
                    for g in range(4):
                        dve(lambda g=g, gt_=gt_, sm=sm: nc.vector.tensor_scalar(
                            out=gt_[:, g * 8:(g + 1) * 8], in0=wsel[:, :], scalar1=ohg[:, g:g + 1],
                            scalar2=sm[:, 5:6], op0=ALU.mult, op1=ALU.mult), [wsel, ohg, sm], [gt_])
                    gts.append(gt_)

                def emit_gatesT():
                    gps = psum.alloc()
                    for tb in range(4):
                        gt_ = gts[tb]
                        S.op(PE, lambda gt_=gt_, tb=tb: nc.tensor.transpose(
                            out=gps[0:32, tb * 128:(tb + 1) * 128], in_=gt_[:, :], identity=ident[:, :]),
                            reads=[gt_.b, ident.b], writes=[gps.b])
                    act(A_(gatesT[:, :], gps[0:32, :], AF.Copy), [gps], [gatesT])
                    psum.release(gps)

                ub = 15
                for pi in range(NPASS):
                    for j in range(EPP):
                        if j % 2 == 0:
                            w = next_unit(ub + pi * (EPP // 2 + 2) + j // 2)
                        base = (j % 2) * 2048
                        psg = proj_h(w, base, 256, 0)
                        psu = proj_h(w, base, 256, 128)
                        sl = sigr.next()
                        act(A_(sl[:, :], psg[:, :], AF.Silu), [psg], [sl])
                        psum.release(psg)
                        dve(lambda psu=psu, sl=sl, j=j: nc.vector.tensor_tensor(
                            out=merged[:, j * T:(j + 1) * T], in0=psu[:, :], in1=sl[:, :], op=ALU.mult),
                            [psu, sl], [mergedk[j]])
                        psum.release(psu)
                    if pi == 0:
                        emit_gatesT()
                    for j in range(EPP):
                        e = pi * EPP + j
                        psb = psum.alloc()
                        mm(psb[:, :], sel[0:32, e * 128:(e + 1) * 128], gatesT[0:32, :], True, True,
                           [sel, gatesT], [psb])
                        dve(lambda psb=psb, j=j: nc.vector.tensor_tensor(
                            out=merged[:, j * T:(j + 1) * T], in0=psb[:, :], in1=merged[:, j * T:(j + 1) * T],
                            op=ALU.mult), [psb, mergedk[j]], [mergedk[j]])
                        psum.release(psb)
                    for half in range(2):
                        w = next_unit(ub + pi * (EPP // 2 + 2) + EPP // 2 + half)
                        for o4 in range(4):
                            oc = half * 4 + o4
                            ps = proj_fm(w, 0, 512, o4 * 128, merged, mergedk, EPP, T)
                            dve(lambda ps=ps, oc=oc: nc.vector.tensor_tensor(
                                out=xT[:, oc * T:(oc + 1) * T], in0=xT[:, oc * T:(oc + 1) * T], in1=ps[:, :],
                                op=ALU.add), [ps, xTk[oc]], [xTk[oc]])
                            psum.release(ps)

                fm_norm(l, C_PLEG)
                ub2 = 15 + NPASS * (EPP // 2 + 2)
                wstate["hold"] = wstate["consumed"]
                wpi = next_unit(ub2)
                last_layer = (l == NL - 1)
                for half in range(2):
                    w = next_unit(ub2 + 1 + half)
                    for o4 in range(4):
                        oc = half * 4 + o4
                        psg = proj_h(w, 0, 512, o4 * 128)
                        sg = sigr.next()
                        act(A_(sg[:, :], psg[:, :], AF.Sigmoid), [psg], [sg])
                        psum.release(psg)
                        pse = proj_fm(wpi, 0, 1024, oc * 128, pTs, [pTs] * 2, 2, T)
                        tmp = mtr.next()
                        dve(lambda pse=pse, sg=sg, tmp=tmp: nc.vector.tensor_tensor(
                            out=tmp[:, :], in0=pse[:, :], in1=sg[:, :], op=ALU.mult), [pse, sg], [tmp])
                        psum.release(pse)
                        dve(lambda tmp=tmp, oc=oc: nc.vector.tensor_tensor(
                            out=xT[:, oc * T:(oc + 1) * T], in0=xT[:, oc * T:(oc + 1) * T], in1=tmp[:, :],
                            op=ALU.add), [tmp, xTk[oc]], [xTk[oc]])
                        if last_layer and t >= NH:
                            to = t - NH
                            S.dma(SP, st_y[oc], lambda oc=oc, to=to: nc.sync.dma_start(
                                out=yTd[oc * 128:(oc + 1) * 128, to * T:(to + 1) * T],
                                in_=xT[:, oc * T:(oc + 1) * T]), reads=[xTk[oc].b])
                            if t + 1 < NT and 1 <= oc <= KC - 2:
                                load_x_chunk(t + 1, oc - 1)
                if last_layer and t >= NH and t + 1 < NT:
                    prefetched.add(t + 1)
                wstate["hold"] = None

            for t in range(NT):
                if any(modes[(t, l)] != "skip" for l in range(NL)):
                    load_x(t)
                for l in range(NL):
                    m = modes[(t, l)]
                    if m == "full":
                        layer_tile(t, l, t < NH)
                    elif m in ("kva", "kv"):
                        layer_tile_partial(t, l, m)
            for k in range(KC):
                nc.sync.wait_ge(st_y[k].sem, st_y[k].cnt)

        @block.sync
        def _(sync):
            body()
        assert wstate["consumed"] == len(unit_list) and wstate["issued"] == len(unit_list)
    return nc


_PROG_CACHE = {}


def _get_prog(NL, NH, NOWN):
    key = (NL, NH, NOWN)
    if key not in _PROG_CACHE:
        _PROG_CACHE[key] = build_program(NL, NH, NOWN)
    return _PROG_CACHE[key]


def _consts():
    c = np.zeros((128, 128 + 32 * 128), np.float32)
    c[:, 0:128] = np.eye(128, dtype=np.float32)
    for e in range(32):
        c[e, 128 + e * 128:128 + (e + 1) * 128] = 1.0
    return c


def _flags(first_half):
    f = np.zeros((128, 56), np.float32)
    f[:, 0] = 0.0 if first_half else 1.0
    f[:, 40:56] = 0.0 if first_half else 1.0
    f[:, 1] = 1.0
    wins = (2, 4, 8, 16)
    for c in range(2):
        for p in range(128):
            w = wins[2 * c + p // 64]
            for i in range(16):
                f[p, 2 + c * 16 + i] = (w / min(i + 1, w)) if first_half else 1.0
    return f


def _layer_tables(inp, l):
    f32 = np.float32
    g = np.zeros((128, NCOL), f32)
    g[:, C_MIXG:C_MIXG + 8] = inp["mix_norm_g"][l].reshape(8, 128).T
    g[:, C_FFNG:C_FFNG + 8] = inp["ffn_norm_g"][l].reshape(8, 128).T
    g[:, C_PLEG:C_PLEG + 8] = inp["ple_norm_g"][l].reshape(8, 128).T
    g[:, C_PSCALE:C_PSCALE + 2] = inp["pool_scale"][l].reshape(2, 128).T
    wins = np.array([2, 4, 8, 16], f32)
    for c in range(2):
        g[0:64, C_INVW + c] = 1.0 / wins[2 * c]
        g[64:128, C_INVW + c] = 1.0 / wins[2 * c + 1]
    g[:, C_GQ] = np.tile(inp["q_norm_g"][l], 2)
    g[:, C_GK] = np.tile(inp["k_norm_g"][l], 2)
    g[:, C_GATEB:C_GATEB + 24] = inp["gate_b"][l].reshape(3, 8, 128).transpose(2, 0, 1).reshape(128, 24)
    gnb = np.broadcast_to(inp["sgu_norm_g"][l][None, :], (128, 256)).astype(f32)
    sb_ = inp["sgu_b"][l]
    bT = np.zeros((128, 256), f32)
    for c in range(2):
        bT[0:64, c * 128:(c + 1) * 128] = sb_[2 * c][None, :]
        bT[64:128, c * 128:(c + 1) * 128] = sb_[2 * c + 1][None, :]
    wmT = inp["sgu_w"][l].transpose(2, 0, 1).reshape(128, 512).astype(f32)
    wbd = np.zeros((128, 256), f32)
    for c in range(2):
        for gp in range(2):
            wbd[gp * 64:(gp + 1) * 64, c * 128 + gp * 64:c * 128 + (gp + 1) * 64] = inp["pool_w"][l][2 * c + gp]
    wrt = np.concatenate([inp["w_group_router"][l], inp["w_expert_router"][l]], axis=1)
    wr = wrt.reshape(8, 128, 36).transpose(1, 0, 2).reshape(128, 288).astype(f32)
    br = np.broadcast_to(np.concatenate([inp["b_group_router"][l], inp["b_expert_router"][l]])[None, :],
                         (128, 36)).astype(f32)
    kk = np.arange(128)[:, None]
    qq = np.arange(640)[None, :]
    idx = np.clip(qq - kk, -128, 128) + 128
    toep = inp["rel_bias"][l][:, idx].transpose(1, 0, 2).reshape(128, 8 * 640).astype(f32)
    return dict(gcols=g, gnb=gnb, sgubT=bT, sguwT=wmT, poolbd=wbd, wrouter=wr, brouter=br, toep=toep)


def _run(inp, x, layers, NH):
    NL = len(layers)
    B, S_, _ = x.shape
    NOWN = S_ // 2 // T
    NT = NH + NOWN
    ncores = 2 * B
    nc = _get_prog(NL, NH, NOWN)
    tabs = [_layer_tables(inp, l) for l in layers]
    shared = {k: np.ascontiguousarray(np.stack([tb[k] for tb in tabs])) for k in tabs[0]}
    shared["wstream"] = np.ascontiguousarray(np.stack([
        pack_layer_weights(inp["w_in"][l], inp["w_branch_a"][l], inp["w_branch_b"][l], inp["w_branch_c"][l],
                           inp["w_out"][l], inp["w_expert_in"][l], inp["w_expert_out"][l],
                           inp["w_ple_in"][l], inp["w_ple_gate"][l]) for l in layers]))
    shared["consts"] = _consts()
    in_maps = []
    for c in range(ncores):
        b, half = c // 2, c % 2
        lo = half * (S_ // 2) - NH * T
        xs = np.zeros((NT * T, D), np.float32)
        ps_ = np.zeros((NL, NT * T, 256), np.float32)
        src_lo = max(lo, 0)
        xs[src_lo - lo:] = x[b, src_lo:lo + NT * T]
        for i, l in enumerate(layers):
            ps_[i, src_lo - lo:] = inp["p"][l, b, src_lo:lo + NT * T]
        m = dict(shared)
        m["xT"] = np.ascontiguousarray(xs.T)
        m["pT"] = np.ascontiguousarray(ps_.transpose(0, 2, 1))
        m["flags"] = _flags(half == 0)
        in_maps.append(m)
    res = RUNNER(nc, in_maps, core_ids=list(range(ncores)))
    out = np.zeros((B, S_, D), np.float32)
    for c in range(ncores):
        b, half = c // 2, c % 2
        out[b, half * (S_ // 2):(half + 1) * (S_ // 2)] = res.results[c]["yT"].T
    return out


FUSED = True


def RUNNER(nc, in_maps, core_ids):
    return run_bass_kernel_spmd(nc, in_maps, core_ids=core_ids)


def kernel(**inputs):
    inp = {k: np.asarray(v) for k, v in inputs.items()}
    x = inp["x"].astype(np.float32, copy=False)
    if FUSED:
        return _run(inp, x, [0, 1], 2)
    for l in range(2):
        x = _run(inp, x, [l], 1)
    return x
```

```python
import contextlib
import numpy as np
import concourse.bass as bass
import concourse.mybir as mybir
from concourse.bass_utils import run_bass_kernel_spmd

F32 = mybir.dt.float32
BF16 = mybir.dt.bfloat16
AF = mybir.ActivationFunctionType
ALU = mybir.AluOpType
AX = mybir.AxisListType

D = 1024
T = 512
KC = 8
NOWN = 8
EPS = 1e-6
NSLOT = 4
SLOT = 4096
NE = 32
EPP = 8
NPASS = NE // EPP

C_MIXG, C_FFNG, C_PLEG = 0, 8, 16
C_PSCALE, C_INVW = 24, 26
C_GQ, C_GK = 28, 29
C_GATEB = 30
NCOL = 54


class Buf:
    __slots__ = ("name", "w", "r")

    def __init__(self, name):
        self.name = name
        self.w = None
        self.r = {}


class Eng:
    def __init__(self, name, handle, sem, in_order=False):
        self.name = name
        self.h = handle
        self.sem = sem
        self.cnt = 0
        self.waited = {}
        self.in_order = in_order


class Stream:
    def __init__(self, sem):
        self.sem = sem
        self.cnt = 0


class Sched:
    def __init__(self, nc):
        self.nc = nc
        self.n_wait = 0

    def _waits(self, eng, reads, writes):
        need = {}

        def add(ev):
            if ev is None:
                return
            sem, val = ev
            k = id(sem)
            if k not in need or need[k][1] < val:
                need[k] = (sem, val)

        for b in reads:
            add(b.w)
        for b in writes:
            add(b.w)
            for ev in b.r.values():
                add(ev)
        for k, (sem, val) in need.items():
            if sem is eng.sem and eng.in_order:
                continue
            if eng.waited.get(k, 0) >= val:
                continue
            eng.h.wait_ge(sem, val)
            eng.waited[k] = val
            self.n_wait += 1

    def op(self, eng, fn, reads=(), writes=(), last=True):
        self._waits(eng, reads, writes)
        ins = fn()
        ev = (eng.sem, eng.cnt + 1)
        if last:
            eng.cnt += 1
            ins.then_inc(eng.sem, 1)
        for b in reads:
            b.r[id(eng.sem)] = ev
        for b in writes:
            b.w = ev
            b.r = {}
        return ins

    def dma(self, qeng, stream, fn, reads=(), writes=()):
        self._waits(qeng, reads, writes)
        ins = fn()
        stream.cnt += 16
        ins.then_inc(stream.sem, 16)
        ev = (stream.sem, stream.cnt)
        for b in reads:
            b.r[id(stream.sem)] = ev
        for b in writes:
            b.w = ev
            b.r = {}
        return ins


class TB:
    def __init__(self, t, name):
        self.t = t
        self.b = Buf(name)

    def __getitem__(self, idx):
        return self.t[idx]


class Rot:
    def __init__(self, items):
        self.items = items
        self.i = 0

    def next(self):
        it = self.items[self.i % len(self.items)]
        self.i += 1
        return it


class PsumPool:
    def __init__(self, banks):
        self.free = list(banks)

    def alloc(self):
        assert self.free, "psum pool exhausted"
        return self.free.pop(0)

    def release(self, b):
        self.free.append(b)


def unit_sizes():
    u = [4096, 2048, 4096, 4096, 4096]
    u += [4096] * 8
    u += [4096, 4096]
    for _ in range(NPASS):
        u += [4096] * (EPP // 2)
        u += [4096, 4096]
    u += [2048, 4096, 4096]
    return u


def pack_layer_weights(w_in, wa, wb, wc, w_out, we_in, we_out, wple_in, wple_gate):
    def kp(m):
        K = m.shape[0] // 128
        return m.reshape(K, 128, m.shape[1]).transpose(1, 0, 2)

    units = []
    win = kp(w_in)
    units.append(win[:, :, 0:512])
    units.append(win[:, :, 512:768])
    units.append(win[:, :, 768:1280])
    units.append(win[:, :, 1280:1792])
    units.append(win[:, :, 1792:2304])
    wak, wbk, wck = kp(wa), kp(wb), kp(wc)
    for dc in range(8):
        cs = slice(dc * 128, (dc + 1) * 128)
        parts = []
        for br in range(3):
            c0 = 2304 + br * 1024 + dc * 128
            parts.append(win[:, :, c0:c0 + 128].reshape(128, -1))
        parts.append(wak[:, :, cs].reshape(128, -1))
        parts.append(wbk[:, :, cs].reshape(128, -1))
        parts.append(wck[:, :, cs].reshape(128, -1))
        units.append(np.concatenate(parts, axis=1))
    wo = kp(w_out)
    units.append(wo[:, :, 0:512])
    units.append(wo[:, :, 512:1024])
    for pi in range(NPASS):
        for j in range(0, EPP, 2):
            e = pi * EPP + j
            units.append(np.concatenate([kp(we_in[e]).reshape(128, -1),
                                         kp(we_in[e + 1]).reshape(128, -1)], axis=1))
        eo = we_out[pi * EPP:(pi + 1) * EPP].transpose(1, 0, 2)
        units.append(eo[:, :, 0:512])
        units.append(eo[:, :, 512:1024])
    units.append(kp(wple_in))
    wg = kp(wple_gate)
    units.append(wg[:, :, 0:512])
    units.append(wg[:, :, 512:1024])
    flat = [np.ascontiguousarray(u).reshape(128, -1) for u in units]
    sizes = unit_sizes()
    assert [f.shape[1] for f in flat] == sizes, ([f.shape[1] for f in flat], sizes)
    return np.ascontiguousarray(np.concatenate(flat, axis=1), dtype=np.float32)


INTERLEAVE_QK = False


def tile_units(mode):
    if mode == "full":
        n = len(unit_sizes())
        return ([0, 1, 4, 2, 3] if INTERLEAVE_QK else [0, 1, 2, 3, 4]) + list(range(5, n))
    if mode == "kva":
        return [0, 3, 4]
    if mode == "kv":
        return [3, 4]
    return []


def tile_modes(NL, NH, NT):
    modes = {}
    for t in range(NT):
        for l in range(NL):
            if t >= NH:
                m = "full"
            elif NL == 1:
                m = "kva"
            else:
                d = NH - t
                if d == 1:
                    m = "full" if l < NL - 1 else "kva"
                else:
                    m = "kva" if l == 0 else "skip"
            modes[(t, l)] = m
    return modes


def build_program(NL, NH, NOWN=NOWN):
    NT = NH + NOWN
    NTOK = NT * T
    sizes = unit_sizes()
    TOT = sum(sizes)
    offs = np.concatenate([[0], np.cumsum(sizes)]).astype(int)
    modes = tile_modes(NL, NH, NT)

    nc = bass.Bass("TRN2", target_bir_lowering=False)
    xTd = nc.dram_tensor("xT", [D, NTOK], F32, kind="ExternalInput").ap()
    pTd = nc.dram_tensor("pT", [NL, 256, NTOK], F32, kind="ExternalInput").ap()
    wsd = nc.dram_tensor("wstream", [NL, 128, TOT], F32, kind="ExternalInput").ap()
    gcold = nc.dram_tensor("gcols", [NL, 128, NCOL], F32, kind="ExternalInput").ap()
    gnbd = nc.dram_tensor("gnb", [NL, 128, 256], F32, kind="ExternalInput").ap()
    bTd = nc.dram_tensor("sgubT", [NL, 128, 256], F32, kind="ExternalInput").ap()
    wmTd = nc.dram_tensor("sguwT", [NL, 128, 512], F32, kind="ExternalInput").ap()
    wbdd = nc.dram_tensor("poolbd", [NL, 128, 256], F32, kind="ExternalInput").ap()
    wrd = nc.dram_tensor("wrouter", [NL, 128, 8 * 36], F32, kind="ExternalInput").ap()
    brd = nc.dram_tensor("brouter", [NL, 128, 36], F32, kind="ExternalInput").ap()
    toepd = nc.dram_tensor("toep", [NL, 128, 8 * 640], F32, kind="ExternalInput").ap()
    flagd = nc.dram_tensor("flags", [128, 56], F32, kind="ExternalInput").ap()
    constd = nc.dram_tensor("consts", [128, 128 + 32 * 128], F32, kind="ExternalInput").ap()
    yTd = nc.dram_tensor("yT", [D, NOWN * T], F32, kind="ExternalOutput").ap()
    escr = nc.dram_tensor("escratch", [NL, 128, 8 * 640], BF16).ap()

    es = contextlib.ExitStack()
    with es:
        def sb(name, shape, dt):
            return TB(es.enter_context(nc.sbuf_tensor(name, shape, dt)), name)

        def views(tb, n):
            return [TB(tb.t, f"{tb.b.name}_{i}") for i in range(n)]

        xT = sb("xT_sb", [128, KC * T], F32)
        xTk = views(xT, KC)
        hT = sb("hT", [128, KC * T], BF16)
        xside = sb("xside", [128, 2 * T], F32)
        xsidek = views(xside, 2)
        hTk = views(hT, KC)
        sqr = Rot([sb(f"sq{i}", [128, T], BF16) for i in range(3)])
        rstd = sb("rstd", [128, T], F32)
        rstdtm = sb("rstdtm", [128, 4], F32)
        aext = sb("aext", [128, 2 * 528], F32)
        sA = sb("sA", [128, 2 * 528], F32)
        sB = sb("sB", [128, 2 * 528], F32)
        pooled = sb("pooled", [128, 2 * T], BF16)
        yaT = sb("yaT", [128, 2 * T], BF16)
        guT = sb("guT", [128, 2 * T], BF16)
        vtr = Rot([sb(f"vt{i}", [128, 256], BF16) for i in range(4)])
        vss = sb("vss", [128, 4], F32)
        vn = sb("vn", [128, 4 * 256], BF16)
        ybT = sb("ybT", [128, 2 * T], BF16)
        qn = sb("qn", [128, 4 * T], BF16)
        qnk = views(qn, 4)
        rsr = Rot([sb(f"rs{i}", [128, T], F32) for i in range(3)])
        kTc = [sb(f"kTc{l}", [128, 4 * 2 * T], BF16) for l in range(NL)]
        kTck = [views(kTc[l], 4) for l in range(NL)]
        Vaug = [sb(f"Vaug{l}", [128, 8 * 768], BF16) for l in range(NL)]
        atail = [sb(f"atail{l}", [128, 32], F32) for l in range(NL)]
        Etab = sb("Etab", [128, 8 * 640], BF16)
        NROT = 4
        esr = Rot([sb(f"es{i}", [128, T], BF16) for i in range(NROT)])
        ptr = Rot([sb(f"pt{i}", [128, T], BF16) for i in range(NROT)])
        rden = rsr.items[0]
        ycT = sb("ycT", [128, 4 * T], BF16)
        ycTk = views(ycT, 4)
        sigr = Rot([sb(f"sig{i}", [128, T], F32) for i in range(3)])
        mtr = Rot([sb(f"mt{i}", [128, T], F32) for i in range(3)])
        merged = sb("merged", [128, KC * T], BF16)
        mergedk = views(merged, KC)
        lg = sb("lg", [128, 36], F32)
        rsm = Rot([sb(f"rsm{i}", [128, 16], F32) for i in range(2)])
        ohg = sb("ohg", [128, 4], F32)
        gexp = sb("gexp", [128, 4], F32)
        le = sb("le", [128, 8], F32)
        m8 = sb("m8", [128, 8], F32)
        ee = sb("ee", [128, 8], F32)
        wsel = sb("wsel", [128, 8], F32)
        gates = Rot([sb(f"gates{i}", [128, 32], F32) for i in range(4)])
        gatesT = sb("gatesT", [32, T], BF16)
        pTs = sb("pTs", [128, 2 * T], BF16)
        ring = [sb(f"wslot{i}", [128, SLOT], BF16) for i in range(NSLOT)]
        ones_bf = sb("ones_bf", [128, 128], BF16)
        bd_ones = sb("bd_ones", [128, 128], BF16)
        ones_f = sb("ones_f", [128, 256], F32)
        ident = sb("ident", [128, 128], F32)
        sel = sb("sel", [32, 32 * 128], BF16)
        flags = sb("flags_sb", [128, 56], F32)
        gcols = [sb(f"gcols{l}", [128, NCOL], F32) for l in range(NL)]
        gnb = [sb(f"gnb{l}", [128, 256], F32) for l in range(NL)]
        bT = [sb(f"bT{l}", [128, 256], F32) for l in range(NL)]
        wmT = [sb(f"wmT{l}", [128, 512], BF16) for l in range(NL)]
        wbd = [sb(f"wbd{l}", [128, 256], BF16) for l in range(NL)]
        wr = [sb(f"wr{l}", [128, 8 * 36], F32) for l in range(NL)]
        brt = [sb(f"br{l}", [128, 36], F32) for l in range(NL)]

        banks = [TB(es.enter_context(nc.psum_tensor(f"ps{i}", [128, T], F32)), f"ps{i}") for i in range(8)]
        psum = PsumPool(banks)

        def sem(name):
            return es.enter_context(nc.semaphore(name))

        PE = Eng("pe", nc.tensor, sem("s_pe"), in_order=True)
        ACT = Eng("act", nc.scalar, sem("s_act"))
        DVE = Eng("dve", nc.vector, sem("s_dve"))
        POOL = Eng("pool", nc.gpsimd, sem("s_pool"))
        SP = Eng("sp", nc.sync, sem("s_sp"))
        slot_streams = [Stream(sem(f"s_slot{i}")) for i in range(NSLOT)]
        st_x = [Stream(sem(f"s_x{k}")) for k in range(KC)]
        st_p = Stream(sem("s_p"))
        st_xs = [Stream(sem(f"s_xs{k}")) for k in range(2)]
        st_y = [Stream(sem(f"s_y{k}")) for k in range(KC)]
        st_e = [Stream(sem(f"s_e{i}")) for i in range(2)]
        st_el = Stream(sem("s_el"))
        st_c = Stream(sem("s_c"))
        st_c2 = Stream(sem("s_c2"))

        S = Sched(nc)
        block = es.enter_context(nc.Block())

        def mm(out, lhsT, rhs, start, stop, reads, writes):
            return S.op(PE, lambda: nc.tensor.matmul(out, lhsT=lhsT, rhs=rhs, start=start, stop=stop),
                        reads=[r.b for r in reads], writes=[w.b for w in writes], last=True)

        def act(fn, reads, writes):
            return S.op(ACT, fn, reads=[r.b for r in reads], writes=[w.b for w in writes])

        def dve(fn, reads, writes):
            return S.op(DVE, fn, reads=[r.b for r in reads], writes=[w.b for w in writes])

        def pool(fn, reads, writes):
            return S.op(POOL, fn, reads=[r.b for r in reads], writes=[w.b for w in writes])

        def A_(out, in_, func, **kw):
            return lambda: nc.scalar.activation(out=out, in_=in_, func=func, **kw)

        unit_list = []
        for t in range(NT):
            for li in range(NL):
                for u in tile_units(modes[(t, li)]):
                    unit_list.append((li, u))
        wstate = {"issued": 0, "consumed": 0, "hold": None}

        def issue_loads(upto):
            if wstate["hold"] is not None:
                upto = min(upto, wstate["hold"] + NSLOT)
            while wstate["issued"] < min(upto, len(unit_list)):
                i = wstate["issued"]
                li, u = unit_list[i]
                n = sizes[u]
                slot = ring[i % NSLOT]
                src = wsd[li, :, int(offs[u]):int(offs[u]) + n]
                S.dma(POOL, slot_streams[i % NSLOT],
                      lambda slot=slot, src=src, n=n: nc.gpsimd.dma_start(out=slot[:, 0:n], in_=src,
                                                                            max_dma_last_dim=8192),
                      writes=[slot.b])
                wstate["issued"] += 1

        def next_unit(expect_u):
            i = wstate["consumed"]
            li, u = unit_list[i]
            assert u == expect_u, (u, expect_u)
            issue_loads(i + NSLOT)
            wstate["consumed"] += 1
            return ring[i % NSLOT]

        def body():
            def cload(dst, src):
                S.dma(SP, st_c, lambda: nc.sync.dma_start(out=dst, in_=src), writes=[])
            cload(flags[:, :], flagd[:, :])
            cload(ident[:, :], constd[:, 0:128])
            S.dma(POOL, st_c2, lambda: nc.gpsimd.dma_start(out=sel[:, :], in_=constd[0:32, 128:128 + 32 * 128],
                                                            max_dma_last_dim=8192), writes=[])
            for l in range(NL):
                cload(gcols[l][:, :], gcold[l])
                cload(gnb[l][:, :], gnbd[l])
                cload(bT[l][:, :], bTd[l])
                cload(aext[:, l * 288:(l + 1) * 288], wrd[l])
                cload(brt[l][:, :], brd[l])
            for l in range(NL):
                S.dma(POOL, st_c2, lambda l=l: nc.gpsimd.dma_start(out=wmT[l][:, :], in_=wmTd[l]), writes=[])
                S.dma(POOL, st_c2, lambda l=l: nc.gpsimd.dma_start(out=wbd[l][:, :], in_=wbdd[l]), writes=[])
            issue_loads(NSLOT)
            for e in (PE, ACT, DVE):
                e.h.wait_ge(st_c.sem, st_c.cnt)
                e.h.wait_ge(st_c2.sem, st_c2.cnt)
            dve(lambda: nc.vector.memset(ones_bf[:, :], 1.0), [], [ones_bf])
            dve(lambda: nc.vector.memset(ones_f[:, :], 1.0), [], [ones_f])
            dve(lambda: nc.vector.memset(bd_ones[:, :], 0.0), [], [bd_ones])
            dve(lambda: nc.vector.memset(bd_ones[0:64, 0:64], 1.0), [], [bd_ones])
            dve(lambda: nc.vector.memset(bd_ones[64:128, 64:128], 1.0), [], [bd_ones])
            for l in range(NL):
                dve(lambda l=l: nc.vector.memset(kTc[l][:, :], 0.0), [], kTck[l])
                dve(lambda l=l: nc.vector.memset(Vaug[l][:, :], 0.0), [], [Vaug[l]])
                dve(lambda l=l: nc.vector.memset(atail[l][:, :], 0.0), [], [atail[l]])
                for g in range(4):
                    dve(lambda l=l, g=g: nc.vector.memset(wmT[l][64:128, g * 128:g * 128 + 64], 0.0), [], [wmT[l]])
                for k in range(8):
                    dve(lambda l=l, k=k: nc.vector.tensor_scalar(
                        out=wr[l][:, k * 36:(k + 1) * 36], in0=aext[:, l * 288 + k * 36:l * 288 + (k + 1) * 36],
                        scalar1=gcols[l][:, C_FFNG + k:C_FFNG + k + 1], scalar2=None, op0=ALU.mult),
                        [aext, gcols[l]], [wr[l]])
            Ev = Etab[:, :].rearrange("p (h q) -> p h q", h=8)
            for l in range(NL):
                for h in range(8):
                    stg = sA if h % 2 == 0 else sB
                    S.dma(SP, st_e[h % 2], lambda stg=stg, h=h, l=l: nc.sync.dma_start(
                        out=stg[:, 0:640], in_=toepd[l, :, h * 640:(h + 1) * 640]), writes=[stg.b])
                    act(A_(Etab[:, h * 640:(h + 1) * 640], stg[:, 0:640], AF.Exp), [stg], [Etab])
                dve(lambda: nc.vector.memset(Ev[0:64, :, 576:640], 0.0), [], [Etab])
                dve(lambda: nc.vector.memset(Ev[64:128, :, 0:64], 0.0), [], [Etab])
                S.dma(SP, st_el, lambda l=l: nc.sync.dma_start(out=escr[l], in_=Etab[:, :]), reads=[Etab.b])
            etab_state = {"cur": NL - 1}
            nc.sync.wait_ge(st_el.sem, st_el.cnt)

            hv_col = flags[:, 0:1]
            one_col = flags[:, 1:2]

            def fm_norm(l, cbase, want_tm=False, src=None):
                if src is None:
                    src = lambda k: (xT[:, k * T:(k + 1) * T], xTk[k])
                ps = psum.alloc()
                for k in range(KC):
                    sq = sqr.next()
                    xa, xb = src(k)
                    act(A_(sq[:, :], xa, AF.Square), [xb], [sq])
                    mm(ps[:, :], ones_bf[:, :], sq[:, :], k == 0, k == KC - 1, [sq, ones_bf], [ps])
                act(A_(rstd[:, :], ps[:, :], AF.Ln, scale=1.0 / D, bias=EPS), [ps], [rstd])
                psum.release(ps)
                act(A_(rstd[:, :], rstd[:, :], AF.Exp, scale=-0.5), [rstd], [rstd])
                for k in range(KC):
                    xa, xb = src(k)
                    dve(lambda k=k, xa=xa: nc.vector.scalar_tensor_tensor(
                        out=hT[:, k * T:(k + 1) * T], in0=xa,
                        scalar=gcols[l][:, cbase + k:cbase + k + 1], in1=rstd[:, :],
                        op0=ALU.mult, op1=ALU.mult), [xb, rstd, gcols[l]], [hTk[k]])
                if want_tm:
                    ps2 = psum.alloc()
                    for tb in range(4):
                        mm(ps2[:, tb:tb + 1], rstd[0:1, tb * 128:(tb + 1) * 128], ones_f[0:1, 0:1],
                           True, True, [rstd, ones_f], [ps2])
                    dve(lambda: nc.vector.tensor_copy(out=rstdtm[:, :], in_=ps2[:, 0:4]), [ps2], [rstdtm])
                    psum.release(ps2)

            def proj_fm(w, wbase, wstride, ncols_off, rhs_tb, rhs_bufs, nk, rhs_stride):
                ps = psum.alloc()
                for kk in range(nk):
                    o = wbase + kk * wstride + ncols_off
                    mm(ps[:, :], w[:, o:o + 128], rhs_tb[:, kk * rhs_stride:kk * rhs_stride + T],
                       kk == 0, kk == nk - 1, [w, rhs_bufs[kk]], [ps])
                return ps

            def proj_h4(w, wbase, wstride, col_offs):
                pss = [psum.alloc() for _ in col_offs]
                for kk in range(KC):
                    for ps, co in zip(pss, col_offs):
                        o = wbase + kk * wstride + co
                        mm(ps[:, :], w[:, o:o + 128], hT[:, kk * T:(kk + 1) * T],
                           kk == 0, kk == KC - 1, [w, hTk[kk]], [ps])
                return pss

            def proj_h(w, wbase, wstride, ncols_off):
                return proj_fm(w, wbase, wstride, ncols_off, hT, hTk, KC, T)

            prefetched = set()
            side_tile = {"t": None}

            def xsrc(t, l, k):
                if l == 0 and side_tile["t"] == t and k >= KC - 2:
                    j = k - (KC - 2)
                    return xside[:, j * T:(j + 1) * T], xsidek[j]
                return xT[:, k * T:(k + 1) * T], xTk[k]

            def load_side(t):
                for j in range(2):
                    k = KC - 2 + j
                    S.dma(SP, st_xs[j], lambda j=j, k=k: nc.sync.dma_start(
                        out=xside[:, j * T:(j + 1) * T], in_=xTd[k * 128:(k + 1) * 128, t * T:(t + 1) * T]),
                        writes=[xsidek[j].b])
                side_tile["t"] = t

            def load_x_chunk(t, k):
                S.dma(SP, st_x[k], lambda: nc.sync.dma_start(
                    out=xT[:, k * T:(k + 1) * T], in_=xTd[k * 128:(k + 1) * 128, t * T:(t + 1) * T]),
                    writes=[xTk[k].b])

            def load_x(t):
                if t in prefetched:
                    return
                for k in range(KC):
                    if side_tile["t"] == t and k >= KC - 2:
                        continue
                    load_x_chunk(t, k)

            def load_p(t, l):
                S.dma(POOL, st_p, lambda: nc.gpsimd.dma_start(
                    out=pTs[:, :].rearrange("p (c t) -> p c t", c=2),
                    in_=pTd[l].rearrange("(c p) t -> p c t", p=128)[:, :, t * T:(t + 1) * T]),
                    writes=[pTs.b])

            def load_E(l):
                if etab_state["cur"] == l:
                    return
                S.dma(SP, st_el, lambda: nc.sync.dma_start(out=Etab[:, :], in_=escr[l]), writes=[Etab.b])
                etab_state["cur"] = l

            def qk_chunks(l, jobs, interleave=False):
                stA = {}
                DEPTH = 2

                def stageA(j):
                    w, c, which, slot_cur = jobs[j]
                    ps = proj_h(w, 0, 512, c * 128)
                    sq = sqr.next()
                    act(A_(sq[:, :], ps[:, :], AF.Square), [ps], [sq])
                    stA[j] = (ps, sq)

                def stageB(j):
                    w, c, which, slot_cur = jobs[j]
                    ps, sq = stA.pop(j)
                    ps2 = psum.alloc()
                    mm(ps2[:, :], bd_ones[:, :], sq[:, :], True, True, [bd_ones, sq], [ps2])
                    rs = rsr.next()
                    act(A_(rs[:, :], ps2[:, :], AF.Ln, scale=1.0 / 64, bias=EPS), [ps2], [rs])
                    psum.release(ps2)
                    act(A_(rs[:, :], rs[:, :], AF.Exp, scale=-0.5), [rs], [rs])
                    if which == 0:
                        dst_tb, dst = qnk[c], qn[:, c * T:(c + 1) * T]
                        gc = gcols[l][:, C_GQ:C_GQ + 1]
                    else:
                        o = (c * 2 + slot_cur) * T
                        dst_tb, dst = kTck[l][c], kTc[l][:, o:o + T]
                        gc = gcols[l][:, C_GK:C_GK + 1]
                    dve(lambda: nc.vector.scalar_tensor_tensor(
                        out=dst, in0=ps[:, :], scalar=gc, in1=rs[:, :], op0=ALU.mult, op1=ALU.mult),
                        [ps, rs, gcols[l]], [dst_tb])
                    psum.release(ps)

                if not interleave:
                    for j in range(len(jobs) + DEPTH):
                        if j < len(jobs):
                            stageA(j)
                        if j >= DEPTH:
                            stageB(j - DEPTH)
                return stageA, stageB

            def v_proj(l, w, slot_cur, flagcol):
                Vv = Vaug[l][:, :].rearrange("p (b c x) -> p b c x", b=8, c=4)
                for tb in range(4):
                    ps = psum.alloc()
                    for kk in range(KC):
                        mm(ps[:, :], hT[:, kk * T + tb * 128:kk * T + (tb + 1) * 128],
                           w[:, kk * 512:(kk + 1) * 512], kk == 0, kk == KC - 1, [hTk[kk], w], [ps])
                    bi = slot_cur * 4 + tb
                    psv = ps[:, :].rearrange("p (c two d) -> p c two d", c=4, two=2)
                    dve(lambda bi=bi, psv=psv: nc.vector.tensor_scalar(
                        out=Vv[:, bi, :, 0:64], in0=psv[:, :, 0, :], scalar1=flagcol, scalar2=None,
                        op0=ALU.mult), [ps, flags], [Vaug[l]])
                    dve(lambda bi=bi, psv=psv: nc.vector.tensor_scalar(
                        out=Vv[:, bi, :, 128:192], in0=psv[:, :, 1, :], scalar1=flagcol, scalar2=None,
                        op0=ALU.mult), [ps, flags], [Vaug[l]])
                    psum.release(ps)
                    act(A_(Vv[:, bi, :, 64:128], ones_f[:, :].rearrange("p (c d) -> p c d", c=4), AF.Copy,
                           scale=flagcol, bias=1e-10), [ones_f, flags], [Vaug[l]])

            def a_proj(w, cc):
                ps = proj_h(w, 0, 512, cc * 128)
                act(A_(aext[:, cc * 528 + 16:cc * 528 + 528], ps[:, :], AF.Copy), [ps], [aext])
                psum.release(ps)

            def save_atail(l):
                for c in range(2):
                    dve(lambda c=c: nc.vector.tensor_copy(out=atail[l][:, c * 16:(c + 1) * 16],
                                                          in_=aext[:, c * 528 + 512:c * 528 + 528]),
                        [aext], [atail[l]])

            def layer_tile_partial(t, l, mode):
                slot_cur = t % 2
                fm_norm(l, C_MIXG)
                if mode == "kva":
                    w = next_unit(0)
                    for cc in range(2):
                        a_proj(w, cc)
                    save_atail(l)
                w = next_unit(3)
                qk_chunks(l, [(w, c, 1, slot_cur) for c in range(4)])
                w = next_unit(4)
                v_proj(l, w, slot_cur, hv_col)

            def layer_tile(t, l, is_halo):
                flagcol = hv_col if is_halo else one_col
                slot_cur = t % 2
                load_E(l)

                fm_norm(l, C_MIXG, src=lambda k: xsrc(t, l, k))

                w = next_unit(0)
                u0ps = proj_h4(w, 0, 512, [0, 128, 256, 384])
                for cc in range(2):
                    act(A_(aext[:, cc * 528 + 16:cc * 528 + 528], u0ps[cc][:, :], AF.Copy), [u0ps[cc]], [aext])
                    psum.release(u0ps[cc])
                for c in range(2):
                    if t == NH:
                        pool(lambda c=c: nc.gpsimd.tensor_tensor(
                            out=aext[:, c * 528:c * 528 + 16], in0=atail[l][:, c * 16:(c + 1) * 16],
                            in1=flags[:, 40:56], op=ALU.mult), [atail[l], flags], [aext])
                    else:
                        pool(lambda c=c: nc.gpsimd.tensor_copy(
                            out=aext[:, c * 528:c * 528 + 16], in_=atail[l][:, c * 16:(c + 1) * 16]),
                            [atail[l]], [aext])

                def shadd(dst, src, c, sh, p0, p1):
                    lo = 2 * sh - 1
                    pool(lambda: nc.gpsimd.tensor_tensor(
                        out=dst[p0:p1, c * 528 + lo:c * 528 + 528],
                        in0=src[p0:p1, c * 528 + lo:c * 528 + 528],
                        in1=src[p0:p1, c * 528 + lo - sh:c * 528 + 528 - sh], op=ALU.add), [src], [dst])

                shadd(sA, aext, 0, 1, 0, 128)
                shadd(sB, sA, 0, 2, 64, 128)
                shadd(sA, aext, 1, 1, 0, 128)
                shadd(sB, sA, 1, 2, 0, 128)
                shadd(sA, sB, 1, 4, 0, 128)
                shadd(sB, sA, 1, 8, 64, 128)
                if t == NH:
                    for c in range(2):
                        for (src, p0, p1) in ((sA, 0, 64), (sB, 64, 128)):
                            pool(lambda c=c, src=src, p0=p0, p1=p1: nc.gpsimd.tensor_tensor(
                                out=src[p0:p1, c * 528 + 16:c * 528 + 32], in0=src[p0:p1, c * 528 + 16:c * 528 + 32],
                                in1=flags[p0:p1, 2 + c * 16:2 + (c + 1) * 16], op=ALU.mult), [src, flags], [src])
                for c in range(2):
                    pool(lambda c=c: nc.gpsimd.tensor_copy(out=atail[l][:, c * 16:(c + 1) * 16],
                                                           in_=aext[:, c * 528 + 512:c * 528 + 528]),
                         [aext], [atail[l]])
                for cc in range(2, 4):
                    ps = u0ps[cc]
                    act(A_(guT[:, (cc - 2) * T:(cc - 1) * T], ps[:, :], AF.Gelu_apprx_tanh), [ps], [guT])
                    psum.release(ps)
                w = next_unit(1)
                vts = []
                for tb in range(4):
                    ps = psum.alloc()
                    for kk in range(KC):
                        mm(ps[:, 0:256], hT[:, kk * T + tb * 128:kk * T + (tb + 1) * 128],
                           w[:, kk * 256:(kk + 1) * 256], kk == 0, kk == KC - 1, [hTk[kk], w], [ps])
                    vt = vtr.next()
                    act(A_(vt[:, :], ps[:, 0:256], AF.Gelu_apprx_tanh), [ps], [vt])
                    psum.release(ps)
                    act(A_(rstd[:, 0:256], vt[:, :], AF.Square, accum_out=vss[:, tb:tb + 1]), [vt], [rstd, vss])
                    vts.append(vt)
                act(A_(vss[:, :], vss[:, :], AF.Ln, scale=1.0 / 256, bias=EPS), [vss], [vss])
                act(A_(vss[:, :], vss[:, :], AF.Exp, scale=-0.5), [vss], [vss])
                for tb in range(4):
                    vt = vts[tb]
                    dve(lambda tb=tb, vt=vt: nc.vector.scalar_tensor_tensor(
                        out=vn[:, tb * 256:(tb + 1) * 256], in0=vt[:, :], scalar=vss[:, tb:tb + 1],
                        in1=gnb[l][:, :], op0=ALU.mult, op1=ALU.mult), [vt, vss, gnb[l]], [vn])
                if INTERLEAVE_QK:
                    w = next_unit(4)
                    v_proj(l, w, slot_cur, flagcol)
                    wstate["hold"] = wstate["consumed"]
                    wq = next_unit(2)
                    wk = next_unit(3)
                    qk_jobs = []
                    for c in range(4):
                        qk_jobs += [(wq, c, 0, slot_cur), (wk, c, 1, slot_cur)]
                    qkA, qkB = qk_chunks(l, qk_jobs, interleave=True)
                    qkA(0)
                    qkA(1)
                    qkB(0)
                    qkB(1)
                else:
                    wstate["hold"] = wstate["consumed"]
                    wq = next_unit(2)
                    wk = next_unit(3)
                    qk_chunks(l, [(wq, c, 0, slot_cur) for c in range(4)] + [(wk, c, 1, slot_cur) for c in range(4)])
                    wstate["hold"] = None
                    w = next_unit(4)
                    v_proj(l, w, slot_cur, flagcol)

                steps = []
                for h in range(8):
                    order = [3, 0, 1, 2, 5, 6, 7, 4]
                    for i, wi in enumerate(order):
                        steps.append((h, wi, i == 0, i == len(order) - 1))
                LAG = NROT - 1
                pv_of = {}
                pt_of = {}

                def geom(wi):
                    ca, cb = 2 * wi - 8, 2 * wi - 7
                    qlo = max(ca, 0) * 64
                    qhi = (min(cb + 8, 7) + 1) * 64
                    K0 = (wi - 4) * 128
                    return qlo, qhi, K0

                def emit_score(i):
                    h, wi, first, lastk = steps[i]
                    c, hh = h // 2, h % 2
                    qlo, qhi, K0 = geom(wi)
                    N = qhi - qlo
                    kslot = (1 - slot_cur) if wi < 4 else slot_cur
                    kb = wi % 4
                    ko = (c * 2 + kslot) * T + kb * 128
                    sc = psum.alloc()
                    mm(sc[:, 0:N], kTc[l][hh * 64:(hh + 1) * 64, ko:ko + 128],
                       qn[hh * 64:(hh + 1) * 64, c * T + qlo:c * T + qhi], True, True, [kTck[l][c], qnk[c]], [sc])
                    es_ = esr.next()
                    act(A_(es_[:, 0:N], sc[:, 0:N], AF.Exp, scale=0.125), [sc], [es_])
                    psum.release(sc)
                    pt = ptr.next()
                    eo = h * 640 + qlo - K0
                    if i % 2 == 1:
                        pool(lambda: nc.gpsimd.tensor_tensor(out=pt[:, 0:N], in0=es_[:, 0:N], in1=Etab[:, eo:eo + N],
                                                             op=ALU.mult), [es_, Etab], [pt])
                    else:
                        dve(lambda: nc.vector.tensor_tensor(out=pt[:, 0:N], in0=es_[:, 0:N], in1=Etab[:, eo:eo + N],
                                                            op=ALU.mult), [es_, Etab], [pt])
                    pt_of[i] = pt

                def emit_pv(i):
                    h, wi, first, lastk = steps[i]
                    c, hh = h // 2, h % 2
                    qlo, qhi, K0 = geom(wi)
                    N = qhi - qlo
                    if first:
                        pv_of[h] = psum.alloc()
                    pv = pv_of[h]
                    kslot = (1 - slot_cur) if wi < 4 else slot_cur
                    bi = kslot * 4 + (wi % 4)
                    vo = bi * 768 + c * 192 + (0 if hh == 0 else 64)
                    pt = pt_of.pop(i)
                    mm(pv[:, qlo:qhi], Vaug[l][:, vo:vo + 128], pt[:, 0:N], first, lastk, [Vaug[l], pt], [pv])
                    if lastk:
                        n0, n1 = (0, 64) if hh == 0 else (64, 128)
                        d0, d1 = (64, 128) if hh == 0 else (0, 64)
                        act(A_(rden[n0:n1, :], pv[d0:d1, :], AF.Ln), [pv], [rden])
                        act(A_(rden[n0:n1, :], rden[n0:n1, :], AF.Exp, scale=-1.0), [rden], [rden])
                        dve(lambda: nc.vector.tensor_tensor(out=ycT[n0:n1, c * T:(c + 1) * T], in0=pv[n0:n1, :],
                                                            in1=rden[n0:n1, :], op=ALU.mult), [pv, rden], [ycTk[c]])
                        psum.release(pv)

                def emit_pooled():
                    for c in range(2):
                        for (src, p0, p1) in ((sA, 0, 64), (sB, 64, 128)):
                            dve(lambda c=c, src=src, p0=p0, p1=p1: nc.vector.scalar_tensor_tensor(
                                out=pooled[p0:p1, c * T:(c + 1) * T], in0=src[p0:p1, c * 528 + 16:c * 528 + 528],
                                scalar=gcols[l][p0:p1, C_INVW + c:C_INVW + c + 1],
                                in1=aext[p0:p1, c * 528 + 16:c * 528 + 528], op0=ALU.mult, op1=ALU.subtract),
                                [src, aext, gcols[l]], [pooled])

                inject = {}
                for c in (range(1, 4) if INTERLEAVE_QK else ()):
                    b0 = 16 * (c - 1)
                    inject[b0 + 1] = (qkA, 2 * c)
                    inject[b0 + 5] = (qkA, 2 * c + 1)
                    inject[b0 + 9] = (qkB, 2 * c)
                    inject[b0 + 13] = (qkB, 2 * c + 1)
                for i in range(len(steps) + LAG):
                    if i in inject:
                        inject[i][0](inject[i][1])
                    if i == 24:
                        emit_pooled()
                    if i < len(steps):
                        emit_score(i)
                    if i >= LAG:
                        emit_pv(i - LAG)
                wstate["hold"] = None

                for c in range(2):
                    ps = psum.alloc()
                    mm(ps[:, :], wbd[l][:, c * 128:(c + 1) * 128], pooled[:, c * T:(c + 1) * T], True, True,
                       [wbd[l], pooled], [ps])
                    act(A_(yaT[:, c * T:(c + 1) * T], ps[:, :], AF.Copy,
                           scale=gcols[l][:, C_PSCALE + c:C_PSCALE + c + 1]), [ps, gcols[l]], [yaT])
                    psum.release(ps)

                for c in range(2):
                    ps = psum.alloc()
                    for blk in range(4):
                        for gg in range(2):
                            g = 2 * c + gg
                            mm(ps[gg * 64:(gg + 1) * 64, blk * 128:(blk + 1) * 128],
                               vn[:, blk * 256 + g * 64:blk * 256 + (g + 1) * 64],
                               wmT[l][:, g * 128:(g + 1) * 128], True, True, [vn, wmT[l]], [ps])
                    mt = mtr.next()
                    for blk in range(4):
                        dve(lambda blk=blk, mt=mt, ps=ps, c=c: nc.vector.tensor_tensor(
                            out=mt[:, blk * 128:(blk + 1) * 128], in0=ps[:, blk * 128:(blk + 1) * 128],
                            in1=bT[l][:, c * 128:(c + 1) * 128], op=ALU.add), [ps, bT[l]], [mt])
                    psum.release(ps)
                    dve(lambda mt=mt, c=c: nc.vector.tensor_tensor(
                        out=ybT[:, c * T:(c + 1) * T], in0=mt[:, :], in1=guT[:, c * T:(c + 1) * T], op=ALU.mult),
                        [mt, guT], [ybT])

                ybr = ((yaT, [yaT] * 2), (ybT, [ybT] * 2), (ycT, ycTk))
                nkb = (2, 2, 4)
                boff = (0, 256, 512)
                for dc in range(KC):
                    w = next_unit(5 + dc)
                    macc = None
                    for br in range(3):
                        psg = proj_h(w, br * 1024, 128, 0)
                        sg = sigr.next()
                        act(A_(sg[:, :], psg[:, :], AF.Sigmoid,
                               bias=gcols[l][:, C_GATEB + br * 8 + dc:C_GATEB + br * 8 + dc + 1]),
                            [psg, gcols[l]], [sg])
                        psum.release(psg)
                        psb = proj_fm(w, 3072 + boff[br], 128, 0, ybr[br][0], ybr[br][1], nkb[br], T)
                        if br == 0:
                            macc = mtr.next()
                            dve(lambda psb=psb, sg=sg, macc=macc: nc.vector.tensor_tensor(
                                out=macc[:, :], in0=psb[:, :], in1=sg[:, :], op=ALU.mult), [psb, sg], [macc])
                        else:
                            tmp = mtr.next()
                            dve(lambda psb=psb, sg=sg, tmp=tmp: nc.vector.tensor_tensor(
                                out=tmp[:, :], in0=psb[:, :], in1=sg[:, :], op=ALU.mult), [psb, sg], [tmp])
                            if br == 1:
                                dve(lambda tmp=tmp, macc=macc: nc.vector.tensor_tensor(
                                    out=macc[:, :], in0=macc[:, :], in1=tmp[:, :], op=ALU.add), [macc, tmp], [macc])
                            else:
                                dve(lambda tmp=tmp, macc=macc, dc=dc: nc.vector.tensor_tensor(
                                    out=merged[:, dc * T:(dc + 1) * T], in0=macc[:, :], in1=tmp[:, :], op=ALU.add),
                                    [macc, tmp], [mergedk[dc]])
                        psum.release(psb)
                for half in range(2):
                    w = next_unit(13 + half)
                    for o4 in range(4):
                        oc = half * 4 + o4
                        ps = proj_fm(w, 0, 512, o4 * 128, merged, mergedk, KC, T)
                        xa, xb = xsrc(t, l, oc)
                        dve(lambda ps=ps, oc=oc, xa=xa: nc.vector.tensor_tensor(
                            out=xT[:, oc * T:(oc + 1) * T], in0=xa, in1=ps[:, :], op=ALU.add),
                            [ps, xb, xTk[oc]], [xTk[oc]])
                        psum.release(ps)
                if l == NL - 1 and t >= NH and t + 1 < NT:
                    load_side(t + 1)

                load_p(t, l)

                fm_norm(l, C_FFNG, want_tm=True)
                lps = []
                for tb in range(4):
                    ps = psum.alloc()
                    for kk in range(KC):
                        mm(ps[:, 0:36], xT[:, kk * T + tb * 128:kk * T + (tb + 1) * 128],
                           wr[l][:, kk * 36:(kk + 1) * 36], kk == 0, kk == KC - 1, [xTk[kk], wr[l]], [ps])
                    lps.append(ps)
                gts = []
                for tb in range(4):
                    ps = lps[tb]
                    dve(lambda ps=ps, tb=tb: nc.vector.scalar_tensor_tensor(
                        out=lg[:, :], in0=ps[:, 0:36], scalar=rstdtm[:, tb:tb + 1], in1=brt[l][:, :],
                        op0=ALU.mult, op1=ALU.add), [ps, rstdtm, brt[l]], [lg])
                    psum.release(ps)
                    sm = rsm.next()
                    dve(lambda sm=sm: nc.vector.reduce_max(out=sm[:, 0:1], in_=lg[:, 0:4], axis=AX.X), [lg], [sm])
                    dve(lambda sm=sm: nc.vector.tensor_scalar(out=ohg[:, :], in0=lg[:, 0:4], scalar1=sm[:, 0:1],
                                                               scalar2=None, op0=ALU.is_ge), [lg, sm], [ohg])
                    dve(lambda sm=sm: nc.vector.tensor_scalar(out=sm[:, 1:2], in0=sm[:, 0:1], scalar1=-1.0,
                                                               scalar2=None, op0=ALU.mult), [sm], [sm])
                    act(A_(gexp[:, :], lg[:, 0:4], AF.Exp, bias=sm[:, 1:2], accum_out=sm[:, 2:3]), [lg, sm], [gexp, sm])
                    dve(lambda: nc.vector.tensor_scalar(out=le[:, :], in0=lg[:, 4:12], scalar1=ohg[:, 0:1],
                                                        scalar2=None, op0=ALU.mult), [lg, ohg], [le])
                    for g in range(1, 4):
                        dve(lambda g=g: nc.vector.scalar_tensor_tensor(
                            out=le[:, :], in0=lg[:, 4 + g * 8:12 + g * 8], scalar=ohg[:, g:g + 1], in1=le[:, :],
                            op0=ALU.mult, op1=ALU.add), [lg, ohg, le], [le])
                    dve(lambda: nc.vector.max(out=m8[:, :], in_=le[:, :]), [le], [m8])
                    dve(lambda sm=sm: nc.vector.tensor_scalar(out=sm[:, 3:4], in0=m8[:, 0:1], scalar1=-1.0,
                                                               scalar2=None, op0=ALU.mult), [m8], [sm])
                    act(A_(ee[:, :], le[:, :], AF.Exp, bias=sm[:, 3:4]), [le, sm], [ee])
                    dve(lambda sm=sm: nc.vector.scalar_tensor_tensor(
                        out=wsel[:, :], in0=le[:, :], scalar=m8[:, 1:2], in1=ee[:, :], op0=ALU.is_ge, op1=ALU.mult,
                        accum_out=sm[:, 4:5]), [le, m8, ee], [wsel, sm])
                    dve(lambda sm=sm: nc.vector.tensor_tensor(out=sm[:, 5:6], in0=sm[:, 4:5], in1=sm[:, 2:3],
                                                               op=ALU.mult), [sm], [sm])
                    dve(lambda sm=sm: nc.vector.reciprocal(out=sm[:, 5:6], in_=sm[:, 5:6]), [sm], [sm])
                    gt_ = gates.next()
                    for g in range(4):
                        dve(lambda g=g, gt_=gt_, sm=sm: nc.vector.tensor_scalar(
                            out=gt_[:, g * 8:(g + 1) * 8], in0=wsel[:, :], scalar1=ohg[:, g:g + 1],
                            scalar2=sm[:, 5:6], op0=ALU.mult, op1=ALU.mult), [wsel, ohg, sm], [gt_])
                    gts.append(gt_)

                def emit_gatesT():
                    gps = psum.alloc()
                    for tb in range(4):
                        gt_ = gts[tb]
                        S.op(PE, lambda gt_=gt_, tb=tb: nc.tensor.transpose(
                            out=gps[0:32, tb * 128:(tb + 1) * 128], in_=gt_[:, :], identity=ident[:, :]),
                            reads=[gt_.b, ident.b], writes=[gps.b])
                    act(A_(gatesT[:, :], gps[0:32, :], AF.Copy), [gps], [gatesT])
                    psum.release(gps)

                ub = 15
                for pi in range(NPASS):
                    for j in range(EPP):
                        if j % 2 == 0:
                            w = next_unit(ub + pi * (EPP // 2 + 2) + j // 2)
                        base = (j % 2) * 2048
                        psg = proj_h(w, base, 256, 0)
                        psu = proj_h(w, base, 256, 128)
                        sl = sigr.next()
                        act(A_(sl[:, :], psg[:, :], AF.Silu), [psg], [sl])
                        psum.release(psg)
                        dve(lambda psu=psu, sl=sl, j=j: nc.vector.tensor_tensor(
                            out=merged[:, j * T:(j + 1) * T], in0=psu[:, :], in1=sl[:, :], op=ALU.mult),
                            [psu, sl], [mergedk[j]])
                        psum.release(psu)
                    if pi == 0:
                        emit_gatesT()
                    for j in range(EPP):
                        e = pi * EPP + j
                        psb = psum.alloc()
                        mm(psb[:, :], sel[0:32, e * 128:(e + 1) * 128], gatesT[0:32, :], True, True,
                           [sel, gatesT], [psb])
                        dve(lambda psb=psb, j=j: nc.vector.tensor_tensor(
                            out=merged[:, j * T:(j + 1) * T], in0=psb[:, :], in1=merged[:, j * T:(j + 1) * T],
                            op=ALU.mult), [psb, mergedk[j]], [mergedk[j]])
                        psum.release(psb)
                    for half in range(2):
                        w = next_unit(ub + pi * (EPP // 2 + 2) + EPP // 2 + half)
                        for o4 in range(4):
                            oc = half * 4 + o4
                            ps = proj_fm(w, 0, 512, o4 * 128, merged, mergedk, EPP, T)
                            dve(lambda ps=ps, oc=oc: nc.vector.tensor_tensor(
                                out=xT[:, oc * T:(oc + 1) * T], in0=xT[:, oc * T:(oc + 1) * T], in1=ps[:, :],
                                op=ALU.add), [ps, xTk[oc]], [xTk[oc]])
                            psum.release(ps)

                fm_norm(l, C_PLEG)
                ub2 = 15 + NPASS * (EPP // 2 + 2)
                wstate["hold"] = wstate["consumed"]
                wpi = next_unit(ub2)
                last_layer = (l == NL - 1)
                for half in range(2):
                    w = next_unit(ub2 + 1 + half)
                    pre = proj_h4(w, 0, 512, [0, 128, 256, 384]) if half == 0 else None
                    for o4 in range(4):
                        oc = half * 4 + o4
                        psg = pre[o4] if pre is not None else proj_h(w, 0, 512, o4 * 128)
                        sg = sigr.next()
                        act(A_(sg[:, :], psg[:, :], AF.Sigmoid), [psg], [sg])
                        psum.release(psg)
                        pse = proj_fm(wpi, 0, 1024, oc * 128, pTs, [pTs] * 2, 2, T)
                        tmp = mtr.next()
                        dve(lambda pse=pse, sg=sg, tmp=tmp: nc.vector.tensor_tensor(
                            out=tmp[:, :], in0=pse[:, :], in1=sg[:, :], op=ALU.mult), [pse, sg], [tmp])
                        psum.release(pse)
                        dve(lambda tmp=tmp, oc=oc: nc.vector.tensor_tensor(
                            out=xT[:, oc * T:(oc + 1) * T], in0=xT[:, oc * T:(oc + 1) * T], in1=tmp[:, :],
                            op=ALU.add), [tmp, xTk[oc]], [xTk[oc]])
                        if last_layer and t >= NH:
                            to = t - NH
                            S.dma(SP, st_y[oc], lambda oc=oc, to=to: nc.sync.dma_start(
                                out=yTd[oc * 128:(oc + 1) * 128, to * T:(to + 1) * T],
                                in_=xT[:, oc * T:(oc + 1) * T]), reads=[xTk[oc].b])
                            if t + 1 < NT and 1 <= oc <= KC - 2:
                                load_x_chunk(t + 1, oc - 1)
                if last_layer and t >= NH and t + 1 < NT:
                    prefetched.add(t + 1)
                wstate["hold"] = None

            for t in range(NT):
                if any(modes[(t, l)] != "skip" for l in range(NL)):
                    load_x(t)
                for l in range(NL):
                    m = modes[(t, l)]
                    if m == "full":
                        layer_tile(t, l, t < NH)
                    elif m in ("kva", "kv"):
                        layer_tile_partial(t, l, m)
            for k in range(KC):
                nc.sync.wait_ge(st_y[k].sem, st_y[k].cnt)

        @block.sync
        def _(sync):
            body()
        assert wstate["consumed"] == len(unit_list) and wstate["issued"] == len(unit_list)
    return nc


_PROG_CACHE = {}


def _get_prog(NL, NH, NOWN):
    key = (NL, NH, NOWN)
    if key not in _PROG_CACHE:
        _PROG_CACHE[key] = build_program(NL, NH, NOWN)
    return _PROG_CACHE[key]


def _consts():
    c = np.zeros((128, 128 + 32 * 128), np.float32)
    c[:, 0:128] = np.eye(128, dtype=np.float32)
    for e in range(32):
        c[e, 128 + e * 128:128 + (e + 1) * 128] = 1.0
    return c


def _flags(first_half):
    f = np.zeros((128, 56), np.float32)
    f[:, 0] = 0.0 if first_half else 1.0
    f[:, 40:56] = 0.0 if first_half else 1.0
    f[:, 1] = 1.0
    wins = (2, 4, 8, 16)
    for c in range(2):
        for p in range(128):
            w = wins[2 * c + p // 64]
            for i in range(16):
                f[p, 2 + c * 16 + i] = (w / min(i + 1, w)) if first_half else 1.0
    return f


def _layer_tables(inp, l):
    f32 = np.float32
    g = np.zeros((128, NCOL), f32)
    g[:, C_MIXG:C_MIXG + 8] = inp["mix_norm_g"][l].reshape(8, 128).T
    g[:, C_FFNG:C_FFNG + 8] = inp["ffn_norm_g"][l].reshape(8, 128).T
    g[:, C_PLEG:C_PLEG + 8] = inp["ple_norm_g"][l].reshape(8, 128).T
    g[:, C_PSCALE:C_PSCALE + 2] = inp["pool_scale"][l].reshape(2, 128).T
    wins = np.array([2, 4, 8, 16], f32)
    for c in range(2):
        g[0:64, C_INVW + c] = 1.0 / wins[2 * c]
        g[64:128, C_INVW + c] = 1.0 / wins[2 * c + 1]
    g[:, C_GQ] = np.tile(inp["q_norm_g"][l], 2)
    g[:, C_GK] = np.tile(inp["k_norm_g"][l], 2)
    g[:, C_GATEB:C_GATEB + 24] = inp["gate_b"][l].reshape(3, 8, 128).transpose(2, 0, 1).reshape(128, 24)
    gnb = np.broadcast_to(inp["sgu_norm_g"][l][None, :], (128, 256)).astype(f32)
    sb_ = inp["sgu_b"][l]
    bT = np.zeros((128, 256), f32)
    for c in range(2):
        bT[0:64, c * 128:(c + 1) * 128] = sb_[2 * c][None, :]
        bT[64:128, c * 128:(c + 1) * 128] = sb_[2 * c + 1][None, :]
    wmT = inp["sgu_w"][l].transpose(2, 0, 1).reshape(128, 512).astype(f32)
    wbd = np.zeros((128, 256), f32)
    for c in range(2):
        for gp in range(2):
            wbd[gp * 64:(gp + 1) * 64, c * 128 + gp * 64:c * 128 + (gp + 1) * 64] = inp["pool_w"][l][2 * c + gp]
    wrt = np.concatenate([inp["w_group_router"][l], inp["w_expert_router"][l]], axis=1)
    wr = wrt.reshape(8, 128, 36).transpose(1, 0, 2).reshape(128, 288).astype(f32)
    br = np.broadcast_to(np.concatenate([inp["b_group_router"][l], inp["b_expert_router"][l]])[None, :],
                         (128, 36)).astype(f32)
    kk = np.arange(128)[:, None]
    qq = np.arange(640)[None, :]
    idx = np.clip(qq - kk, -128, 128) + 128
    toep = inp["rel_bias"][l][:, idx].transpose(1, 0, 2).reshape(128, 8 * 640).astype(f32)
    return dict(gcols=g, gnb=gnb, sgubT=bT, sguwT=wmT, poolbd=wbd, wrouter=wr, brouter=br, toep=toep)


def _run(inp, x, layers, NH):
    NL = len(layers)
    B, S_, _ = x.shape
    NOWN = S_ // 2 // T
    NT = NH + NOWN
    ncores = 2 * B
    nc = _get_prog(NL, NH, NOWN)
    tabs = [_layer_tables(inp, l) for l in layers]
    shared = {k: np.ascontiguousarray(np.stack([tb[k] for tb in tabs])) for k in tabs[0]}
    shared["wstream"] = np.ascontiguousarray(np.stack([
        pack_layer_weights(inp["w_in"][l], inp["w_branch_a"][l], inp["w_branch_b"][l], inp["w_branch_c"][l],
                           inp["w_out"][l], inp["w_expert_in"][l], inp["w_expert_out"][l],
                           inp["w_ple_in"][l], inp["w_ple_gate"][l]) for l in layers]))
    shared["consts"] = _consts()
    in_maps = []
    for c in range(ncores):
        b, half = c // 2, c % 2
        lo = half * (S_ // 2) - NH * T
        xs = np.zeros((NT * T, D), np.float32)
        ps_ = np.zeros((NL, NT * T, 256), np.float32)
        src_lo = max(lo, 0)
        xs[src_lo - lo:] = x[b, src_lo:lo + NT * T]
        for i, l in enumerate(layers):
            ps_[i, src_lo - lo:] = inp["p"][l, b, src_lo:lo + NT * T]
        m = dict(shared)
        m["xT"] = np.ascontiguousarray(xs.T)
        m["pT"] = np.ascontiguousarray(ps_.transpose(0, 2, 1))
        m["flags"] = _flags(half == 0)
        in_maps.append(m)
    res = RUNNER(nc, in_maps, core_ids=list(range(ncores)))
    out = np.zeros((B, S_, D), np.float32)
    for c in range(ncores):
        b, half = c // 2, c % 2
        out[b, half * (S_ // 2):(half + 1) * (S_ // 2)] = res.results[c]["yT"].T
    return out


FUSED = True


def RUNNER(nc, in_maps, core_ids):
    return run_bass_kernel_spmd(nc, in_maps, core_ids=core_ids)


def kernel(**inputs):
    inp = {k: np.asarray(v) for k, v in inputs.items()}
    x = inp["x"].astype(np.float32, copy=False)
    if FUSED:
        return _run(inp, x, [0, 1], 2)
    for l in range(2):
        x = _run(inp, x, [l], 1)
    return x
```

```python
import contextlib
import numpy as np
import concourse.bass as bass
import concourse.mybir as mybir
from concourse.bass_utils import run_bass_kernel_spmd

F32 = mybir.dt.float32
BF16 = mybir.dt.bfloat16
AF = mybir.ActivationFunctionType
ALU = mybir.AluOpType
AX = mybir.AxisListType

D = 1024
T = 512
KC = 8
NOWN = 8
EPS = 1e-6
NSLOT = 4
SLOT = 4096
NE = 32
EPP = 8
NPASS = NE // EPP

C_MIXG, C_FFNG, C_PLEG = 0, 8, 16
C_PSCALE, C_INVW = 24, 26
C_GQ, C_GK = 28, 29
C_GATEB = 30
NCOL = 54


class Buf:
    __slots__ = ("name", "w", "r")

    def __init__(self, name):
        self.name = name
        self.w = None
        self.r = {}


class Eng:
    def __init__(self, name, handle, sem, in_order=False):
        self.name = name
        self.h = handle
        self.sem = sem
        self.cnt = 0
        self.waited = {}
        self.in_order = in_order


class Stream:
    def __init__(self, sem):
        self.sem = sem
        self.cnt = 0


class Sched:
    def __init__(self, nc):
        self.nc = nc
        self.n_wait = 0

    def _waits(self, eng, reads, writes):
        need = {}

        def add(ev):
            if ev is None:
                return
            sem, val = ev
            k = id(sem)
            if k not in need or need[k][1] < val:
                need[k] = (sem, val)

        for b in reads:
            add(b.w)
        for b in writes:
            add(b.w)
            for ev in b.r.values():
                add(ev)
        for k, (sem, val) in need.items():
            if sem is eng.sem and eng.in_order:
                continue
            if eng.waited.get(k, 0) >= val:
                continue
            eng.h.wait_ge(sem, val)
            eng.waited[k] = val
            self.n_wait += 1

    def op(self, eng, fn, reads=(), writes=(), last=True):
        self._waits(eng, reads, writes)
        ins = fn()
        ev = (eng.sem, eng.cnt + 1)
        if last:
            eng.cnt += 1
            ins.then_inc(eng.sem, 1)
        for b in reads:
            b.r[id(eng.sem)] = ev
        for b in writes:
            b.w = ev
            b.r = {}
        return ins

    def dma(self, qeng, stream, fn, reads=(), writes=()):
        self._waits(qeng, reads, writes)
        ins = fn()
        stream.cnt += 16
        ins.then_inc(stream.sem, 16)
        ev = (stream.sem, stream.cnt)
        for b in reads:
            b.r[id(stream.sem)] = ev
        for b in writes:
            b.w = ev
            b.r = {}
        return ins


class TB:
    def __init__(self, t, name):
        self.t = t
        self.b = Buf(name)

    def __getitem__(self, idx):
        return self.t[idx]


class Rot:
    def __init__(self, items):
        self.items = items
        self.i = 0

    def next(self):
        it = self.items[self.i % len(self.items)]
        self.i += 1
        return it


class PsumPool:
    def __init__(self, banks):
        self.free = list(banks)

    def alloc(self):
        assert self.free, "psum pool exhausted"
        return self.free.pop(0)

    def release(self, b):
        self.free.append(b)


def unit_sizes():
    u = [4096, 2048, 4096, 4096, 4096]
    u += [4096] * 8
    u += [4096, 4096]
    for _ in range(NPASS):
        u += [4096] * (EPP // 2)
        u += [4096, 4096]
    u += [2048, 4096, 4096]
    return u


def pack_layer_weights(w_in, wa, wb, wc, w_out, we_in, we_out, wple_in, wple_gate):
    def kp(m):
        K = m.shape[0] // 128
        return m.reshape(K, 128, m.shape[1]).transpose(1, 0, 2)

    units = []
    win = kp(w_in)
    units.append(win[:, :, 0:512])
    units.append(win[:, :, 512:768])
    units.append(win[:, :, 768:1280])
    units.append(win[:, :, 1280:1792])
    units.append(win[:, :, 1792:2304])
    wak, wbk, wck = kp(wa), kp(wb), kp(wc)
    for dc in range(8):
        cs = slice(dc * 128, (dc + 1) * 128)
        parts = []
        for br in range(3):
            c0 = 2304 + br * 1024 + dc * 128
            parts.append(win[:, :, c0:c0 + 128].reshape(128, -1))
        parts.append(wak[:, :, cs].reshape(128, -1))
        parts.append(wbk[:, :, cs].reshape(128, -1))
        parts.append(wck[:, :, cs].reshape(128, -1))
        units.append(np.concatenate(parts, axis=1))
    wo = kp(w_out)
    units.append(wo[:, :, 0:512])
    units.append(wo[:, :, 512:1024])
    for pi in range(NPASS):
        for j in range(0, EPP, 2):
            e = pi * EPP + j
            units.append(np.concatenate([kp(we_in[e]).reshape(128, -1),
                                         kp(we_in[e + 1]).reshape(128, -1)], axis=1))
        eo = we_out[pi * EPP:(pi + 1) * EPP].transpose(1, 0, 2)
        units.append(eo[:, :, 0:512])
        units.append(eo[:, :, 512:1024])
    units.append(kp(wple_in))
    wg = kp(wple_gate)
    units.append(wg[:, :, 0:512])
    units.append(wg[:, :, 512:1024])
    flat = [np.ascontiguousarray(u).reshape(128, -1) for u in units]
    sizes = unit_sizes()
    assert [f.shape[1] for f in flat] == sizes, ([f.shape[1] for f in flat], sizes)
    return np.ascontiguousarray(np.concatenate(flat, axis=1), dtype=np.float32)


INTERLEAVE_QK = False


def tile_units(mode):
    if mode == "full":
        n = len(unit_sizes())
        return ([0, 1, 4, 2, 3] if INTERLEAVE_QK else [0, 1, 2, 3, 4]) + list(range(5, n))
    if mode == "kva":
        return [0, 3, 4]
    if mode == "kv":
        return [3, 4]
    return []


def tile_modes(NL, NH, NT):
    modes = {}
    for t in range(NT):
        for l in range(NL):
            if t >= NH:
                m = "full"
            elif NL == 1:
                m = "kva"
            else:
                d = NH - t
                if d == 1:
                    m = "full" if l < NL - 1 else "kva"
                else:
                    m = "kva" if l == 0 else "skip"
            modes[(t, l)] = m
    return modes


def build_program(NL, NH, NOWN=NOWN):
    NT = NH + NOWN
    NTOK = NT * T
    sizes = unit_sizes()
    TOT = sum(sizes)
    offs = np.concatenate([[0], np.cumsum(sizes)]).astype(int)
    modes = tile_modes(NL, NH, NT)

    nc = bass.Bass("TRN2", target_bir_lowering=False)
    xTd = nc.dram_tensor("xT", [D, NTOK], F32, kind="ExternalInput").ap()
    pTd = nc.dram_tensor("pT", [NL, 256, NTOK], F32, kind="ExternalInput").ap()
    wsd = nc.dram_tensor("wstream", [NL, 128, TOT], F32, kind="ExternalInput").ap()
    gcold = nc.dram_tensor("gcols", [NL, 128, NCOL], F32, kind="ExternalInput").ap()
    gnbd = nc.dram_tensor("gnb", [NL, 128, 256], F32, kind="ExternalInput").ap()
    bTd = nc.dram_tensor("sgubT", [NL, 128, 256], F32, kind="ExternalInput").ap()
    wmTd = nc.dram_tensor("sguwT", [NL, 128, 512], F32, kind="ExternalInput").ap()
    wbdd = nc.dram_tensor("poolbd", [NL, 128, 256], F32, kind="ExternalInput").ap()
    wrd = nc.dram_tensor("wrouter", [NL, 128, 8 * 36], F32, kind="ExternalInput").ap()
    brd = nc.dram_tensor("brouter", [NL, 128, 36], F32, kind="ExternalInput").ap()
    toepd = nc.dram_tensor("toep", [NL, 128, 8 * 640], F32, kind="ExternalInput").ap()
    flagd = nc.dram_tensor("flags", [128, 56], F32, kind="ExternalInput").ap()
    constd = nc.dram_tensor("consts", [128, 128 + 32 * 128], F32, kind="ExternalInput").ap()
    yTd = nc.dram_tensor("yT", [D, NOWN * T], F32, kind="ExternalOutput").ap()
    escr = nc.dram_tensor("escratch", [NL, 128, 8 * 640], BF16).ap()

    es = contextlib.ExitStack()
    with es:
        def sb(name, shape, dt):
            return TB(es.enter_context(nc.sbuf_tensor(name, shape, dt)), name)

        def views(tb, n):
            return [TB(tb.t, f"{tb.b.name}_{i}") for i in range(n)]

        xT = sb("xT_sb", [128, KC * T], F32)
        xTk = views(xT, KC)
        hT = sb("hT", [128, KC * T], BF16)
        xside = sb("xside", [128, 2 * T], F32)
        xsidek = views(xside, 2)
        hTk = views(hT, KC)
        sqr = Rot([sb(f"sq{i}", [128, T], BF16) for i in range(3)])
        rstd = sb("rstd", [128, T], F32)
        rstdtm = sb("rstdtm", [128, 4], F32)
        aext = sb("aext", [128, 2 * 528], F32)
        sA = sb("sA", [128, 2 * 528], F32)
        sB = sb("sB", [128, 2 * 528], F32)
        pooled = sb("pooled", [128, 2 * T], BF16)
        yaT = sb("yaT", [128, 2 * T], BF16)
        guT = sb("guT", [128, 2 * T], BF16)
        vtr = Rot([sb(f"vt{i}", [128, 256], BF16) for i in range(4)])
        vss = sb("vss", [128, 4], F32)
        vn = sb("vn", [128, 4 * 256], BF16)
        ybT = sb("ybT", [128, 2 * T], BF16)
        qn = sb("qn", [128, 4 * T], BF16)
        qnk = views(qn, 4)
        rsr = Rot([sb(f"rs{i}", [128, T], F32) for i in range(3)])
        kTc = [sb(f"kTc{l}", [128, 4 * 2 * T], BF16) for l in range(NL)]
        kTck = [views(kTc[l], 4) for l in range(NL)]
        Vaug = [sb(f"Vaug{l}", [128, 8 * 768], BF16) for l in range(NL)]
        atail = [sb(f"atail{l}", [128, 32], F32) for l in range(NL)]
        Etab = sb("Etab", [128, 8 * 640], BF16)
        NROT = 4
        esr = Rot([sb(f"es{i}", [128, T], BF16) for i in range(NROT)])
        ptr = Rot([sb(f"pt{i}", [128, T], BF16) for i in range(NROT)])
        rden = rsr.items[0]
        ycT = sb("ycT", [128, 4 * T], BF16)
        ycTk = views(ycT, 4)
        sigr = Rot([sb(f"sig{i}", [128, T], F32) for i in range(3)])
        mtr = Rot([sb(f"mt{i}", [128, T], F32) for i in range(3)])
        merged = sb("merged", [128, KC * T], BF16)
        mergedk = views(merged, KC)
        lg = sb("lg", [128, 36], F32)
        rsm = Rot([sb(f"rsm{i}", [128, 16], F32) for i in range(2)])
        ohg = sb("ohg", [128, 4], F32)
        gexp = sb("gexp", [128, 4], F32)
        le = sb("le", [128, 8], F32)
        m8 = sb("m8", [128, 8], F32)
        ee = sb("ee", [128, 8], F32)
        wsel = sb("wsel", [128, 8], F32)
        gates = Rot([sb(f"gates{i}", [128, 32], F32) for i in range(4)])
        gatesT = sb("gatesT", [32, T], BF16)
        pTs = sb("pTs", [128, 2 * T], BF16)
        ring = [sb(f"wslot{i}", [128, SLOT], BF16) for i in range(NSLOT)]
        ones_bf = sb("ones_bf", [128, 128], BF16)
        bd_ones = sb("bd_ones", [128, 128], BF16)
        ones_f = sb("ones_f", [128, 256], F32)
        ident = sb("ident", [128, 128], F32)
        sel = sb("sel", [32, 32 * 128], BF16)
        flags = sb("flags_sb", [128, 56], F32)
        gcols = [sb(f"gcols{l}", [128, NCOL], F32) for l in range(NL)]
        gnb = [sb(f"gnb{l}", [128, 256], F32) for l in range(NL)]
        bT = [sb(f"bT{l}", [128, 256], F32) for l in range(NL)]
        wmT = [sb(f"wmT{l}", [128, 512], BF16) for l in range(NL)]
        wbd = [sb(f"wbd{l}", [128, 256], BF16) for l in range(NL)]
        wr = [sb(f"wr{l}", [128, 8 * 36], F32) for l in range(NL)]
        brt = [sb(f"br{l}", [128, 36], F32) for l in range(NL)]

        banks = [TB(es.enter_context(nc.psum_tensor(f"ps{i}", [128, T], F32)), f"ps{i}") for i in range(8)]
        psum = PsumPool(banks)

        def sem(name):
            return es.enter_context(nc.semaphore(name))

        PE = Eng("pe", nc.tensor, sem("s_pe"), in_order=True)
        ACT = Eng("act", nc.scalar, sem("s_act"))
        DVE = Eng("dve", nc.vector, sem("s_dve"))
        POOL = Eng("pool", nc.gpsimd, sem("s_pool"))
        SP = Eng("sp", nc.sync, sem("s_sp"))
        slot_streams = [Stream(sem(f"s_slot{i}")) for i in range(NSLOT)]
        st_x = [Stream(sem(f"s_x{k}")) for k in range(KC)]
        st_p = Stream(sem("s_p"))
        st_xs = [Stream(sem(f"s_xs{k}")) for k in range(2)]
        st_y = [Stream(sem(f"s_y{k}")) for k in range(KC)]
        st_e = [Stream(sem(f"s_e{i}")) for i in range(2)]
        st_el = Stream(sem("s_el"))
        st_c = Stream(sem("s_c"))
        st_c2 = Stream(sem("s_c2"))

        S = Sched(nc)
        block = es.enter_context(nc.Block())

        def mm(out, lhsT, rhs, start, stop, reads, writes):
            return S.op(PE, lambda: nc.tensor.matmul(out, lhsT=lhsT, rhs=rhs, start=start, stop=stop),
                        reads=[r.b for r in reads], writes=[w.b for w in writes], last=True)

        def act(fn, reads, writes):
            return S.op(ACT, fn, reads=[r.b for r in reads], writes=[w.b for w in writes])

        def dve(fn, reads, writes):
            return S.op(DVE, fn, reads=[r.b for r in reads], writes=[w.b for w in writes])

        def pool(fn, reads, writes):
            return S.op(POOL, fn, reads=[r.b for r in reads], writes=[w.b for w in writes])

        def A_(out, in_, func, **kw):
            return lambda: nc.scalar.activation(out=out, in_=in_, func=func, **kw)

        unit_list = []
        for t in range(NT):
            for li in range(NL):
                for u in tile_units(modes[(t, li)]):
                    unit_list.append((li, u))
        wstate = {"issued": 0, "consumed": 0, "hold": None}

        def issue_loads(upto):
            if wstate["hold"] is not None:
                upto = min(upto, wstate["hold"] + NSLOT)
            while wstate["issued"] < min(upto, len(unit_list)):
                i = wstate["issued"]
                li, u = unit_list[i]
                n = sizes[u]
                slot = ring[i % NSLOT]
                src = wsd[li, :, int(offs[u]):int(offs[u]) + n]
                S.dma(POOL, slot_streams[i % NSLOT],
                      lambda slot=slot, src=src, n=n: nc.gpsimd.dma_start(out=slot[:, 0:n], in_=src,
                                                                            max_dma_last_dim=8192),
                      writes=[slot.b])
                wstate["issued"] += 1

        def next_unit(expect_u):
            i = wstate["consumed"]
            li, u = unit_list[i]
            assert u == expect_u, (u, expect_u)
            issue_loads(i + NSLOT)
            wstate["consumed"] += 1
            return ring[i % NSLOT]

        def body():
            def cload(dst, src):
                S.dma(SP, st_c, lambda: nc.sync.dma_start(out=dst, in_=src), writes=[])
            cload(flags[:, :], flagd[:, :])
            cload(ident[:, :], constd[:, 0:128])
            S.dma(POOL, st_c2, lambda: nc.gpsimd.dma_start(out=sel[:, :], in_=constd[0:32, 128:128 + 32 * 128],
                                                            max_dma_last_dim=8192), writes=[])
            for l in range(NL):
                cload(gcols[l][:, :], gcold[l])
                cload(gnb[l][:, :], gnbd[l])
                cload(bT[l][:, :], bTd[l])
                cload(aext[:, l * 288:(l + 1) * 288], wrd[l])
                cload(brt[l][:, :], brd[l])
            for l in range(NL):
                S.dma(POOL, st_c2, lambda l=l: nc.gpsimd.dma_start(out=wmT[l][:, :], in_=wmTd[l]), writes=[])
                S.dma(POOL, st_c2, lambda l=l: nc.gpsimd.dma_start(out=wbd[l][:, :], in_=wbdd[l]), writes=[])
            issue_loads(NSLOT)
            for e in (PE, ACT, DVE):
                e.h.wait_ge(st_c.sem, st_c.cnt)
                e.h.wait_ge(st_c2.sem, st_c2.cnt)
            dve(lambda: nc.vector.memset(ones_bf[:, :], 1.0), [], [ones_bf])
            dve(lambda: nc.vector.memset(ones_f[:, :], 1.0), [], [ones_f])
            dve(lambda: nc.vector.memset(bd_ones[:, :], 0.0), [], [bd_ones])
            dve(lambda: nc.vector.memset(bd_ones[0:64, 0:64], 1.0), [], [bd_ones])
            dve(lambda: nc.vector.memset(bd_ones[64:128, 64:128], 1.0), [], [bd_ones])
            for l in range(NL):
                dve(lambda l=l: nc.vector.memset(kTc[l][:, :], 0.0), [], kTck[l])
                dve(lambda l=l: nc.vector.memset(Vaug[l][:, :], 0.0), [], [Vaug[l]])
                dve(lambda l=l: nc.vector.memset(atail[l][:, :], 0.0), [], [atail[l]])
                for g in range(4):
                    dve(lambda l=l, g=g: nc.vector.memset(wmT[l][64:128, g * 128:g * 128 + 64], 0.0), [], [wmT[l]])
                for k in range(8):
                    dve(lambda l=l, k=k: nc.vector.tensor_scalar(
                        out=wr[l][:, k * 36:(k + 1) * 36], in0=aext[:, l * 288 + k * 36:l * 288 + (k + 1) * 36],
                        scalar1=gcols[l][:, C_FFNG + k:C_FFNG + k + 1], scalar2=None, op0=ALU.mult),
                        [aext, gcols[l]], [wr[l]])
            Ev = Etab[:, :].rearrange("p (h q) -> p h q", h=8)
            for l in range(NL):
                for h in range(8):
                    stg = sA if h % 2 == 0 else sB
                    S.dma(SP, st_e[h % 2], lambda stg=stg, h=h, l=l: nc.sync.dma_start(
                        out=stg[:, 0:640], in_=toepd[l, :, h * 640:(h + 1) * 640]), writes=[stg.b])
                    act(A_(Etab[:, h * 640:(h + 1) * 640], stg[:, 0:640], AF.Exp), [stg], [Etab])
                dve(lambda: nc.vector.memset(Ev[0:64, :, 576:640], 0.0), [], [Etab])
                dve(lambda: nc.vector.memset(Ev[64:128, :, 0:64], 0.0), [], [Etab])
                S.dma(SP, st_el, lambda l=l: nc.sync.dma_start(out=escr[l], in_=Etab[:, :]), reads=[Etab.b])
            etab_state = {"cur": NL - 1}
            nc.sync.wait_ge(st_el.sem, st_el.cnt)

            hv_col = flags[:, 0:1]
            one_col = flags[:, 1:2]

            def fm_norm(l, cbase, want_tm=False, src=None):
                if src is None:
                    src = lambda k: (xT[:, k * T:(k + 1) * T], xTk[k])
                ps = psum.alloc()
                for k in range(KC):
                    sq = sqr.next()
                    xa, xb = src(k)
                    act(A_(sq[:, :], xa, AF.Square), [xb], [sq])
                    mm(ps[:, :], ones_bf[:, :], sq[:, :], k == 0, k == KC - 1, [sq, ones_bf], [ps])
                act(A_(rstd[:, :], ps[:, :], AF.Ln, scale=1.0 / D, bias=EPS), [ps], [rstd])
                psum.release(ps)
                act(A_(rstd[:, :], rstd[:, :], AF.Exp, scale=-0.5), [rstd], [rstd])
                for k in range(KC):
                    xa, xb = src(k)
                    dve(lambda k=k, xa=xa: nc.vector.scalar_tensor_tensor(
                        out=hT[:, k * T:(k + 1) * T], in0=xa,
                        scalar=gcols[l][:, cbase + k:cbase + k + 1], in1=rstd[:, :],
                        op0=ALU.mult, op1=ALU.mult), [xb, rstd, gcols[l]], [hTk[k]])
                if want_tm:
                    ps2 = psum.alloc()
                    for tb in range(4):
                        mm(ps2[:, tb:tb + 1], rstd[0:1, tb * 128:(tb + 1) * 128], ones_f[0:1, 0:1],
                           True, True, [rstd, ones_f], [ps2])
                    dve(lambda: nc.vector.tensor_copy(out=rstdtm[:, :], in_=ps2[:, 0:4]), [ps2], [rstdtm])
                    psum.release(ps2)

            def proj_fm(w, wbase, wstride, ncols_off, rhs_tb, rhs_bufs, nk, rhs_stride):
                ps = psum.alloc()
                for kk in range(nk):
                    o = wbase + kk * wstride + ncols_off
                    mm(ps[:, :], w[:, o:o + 128], rhs_tb[:, kk * rhs_stride:kk * rhs_stride + T],
                       kk == 0, kk == nk - 1, [w, rhs_bufs[kk]], [ps])
                return ps

            def proj_h4(w, wbase, wstride, col_offs):
                pss = [psum.alloc() for _ in col_offs]
                for kk in range(KC):
                    for ps, co in zip(pss, col_offs):
                        o = wbase + kk * wstride + co
                        mm(ps[:, :], w[:, o:o + 128], hT[:, kk * T:(kk + 1) * T],
                           kk == 0, kk == KC - 1, [w, hTk[kk]], [ps])
                return pss

            def proj_h(w, wbase, wstride, ncols_off):
                return proj_fm(w, wbase, wstride, ncols_off, hT, hTk, KC, T)

            prefetched = set()
            side_tile = {"t": None}

            def xsrc(t, l, k):
                if l == 0 and side_tile["t"] == t and k >= KC - 2:
                    j = k - (KC - 2)
                    return xside[:, j * T:(j + 1) * T], xsidek[j]
                return xT[:, k * T:(k + 1) * T], xTk[k]

            def load_side(t):
                for j in range(2):
                    k = KC - 2 + j
                    S.dma(SP, st_xs[j], lambda j=j, k=k: nc.sync.dma_start(
                        out=xside[:, j * T:(j + 1) * T], in_=xTd[k * 128:(k + 1) * 128, t * T:(t + 1) * T]),
                        writes=[xsidek[j].b])
                side_tile["t"] = t

            def load_x_chunk(t, k):
                S.dma(SP, st_x[k], lambda: nc.sync.dma_start(
                    out=xT[:, k * T:(k + 1) * T], in_=xTd[k * 128:(k + 1) * 128, t * T:(t + 1) * T]),
                    writes=[xTk[k].b])

            def load_x(t):
                if t in prefetched:
                    return
                for k in range(KC):
                    if side_tile["t"] == t and k >= KC - 2:
                        continue
                    load_x_chunk(t, k)

            def load_p(t, l):
                S.dma(POOL, st_p, lambda: nc.gpsimd.dma_start(
                    out=pTs[:, :].rearrange("p (c t) -> p c t", c=2),
                    in_=pTd[l].rearrange("(c p) t -> p c t", p=128)[:, :, t * T:(t + 1) * T]),
                    writes=[pTs.b])

            def load_E(l):
                if etab_state["cur"] == l:
                    return
                S.dma(SP, st_el, lambda: nc.sync.dma_start(out=Etab[:, :], in_=escr[l]), writes=[Etab.b])
                etab_state["cur"] = l

            def qk_chunks(l, jobs, interleave=False):
                stA = {}
                DEPTH = 2

                def stageA(j):
                    w, c, which, slot_cur = jobs[j]
                    ps = proj_h(w, 0, 512, c * 128)
                    sq = sqr.next()
                    act(A_(sq[:, :], ps[:, :], AF.Square), [ps], [sq])
                    stA[j] = (ps, sq)

                def stageB(j):
                    w, c, which, slot_cur = jobs[j]
                    ps, sq = stA.pop(j)
                    ps2 = psum.alloc()
                    mm(ps2[:, :], bd_ones[:, :], sq[:, :], True, True, [bd_ones, sq], [ps2])
                    rs = rsr.next()
                    act(A_(rs[:, :], ps2[:, :], AF.Ln, scale=1.0 / 64, bias=EPS), [ps2], [rs])
                    psum.release(ps2)
                    act(A_(rs[:, :], rs[:, :], AF.Exp, scale=-0.5), [rs], [rs])
                    if which == 0:
                        dst_tb, dst = qnk[c], qn[:, c * T:(c + 1) * T]
                        gc = gcols[l][:, C_GQ:C_GQ + 1]
                    else:
                        o = (c * 2 + slot_cur) * T
                        dst_tb, dst = kTck[l][c], kTc[l][:, o:o + T]
                        gc = gcols[l][:, C_GK:C_GK + 1]
                    dve(lambda: nc.vector.scalar_tensor_tensor(
                        out=dst, in0=ps[:, :], scalar=gc, in1=rs[:, :], op0=ALU.mult, op1=ALU.mult),
                        [ps, rs, gcols[l]], [dst_tb])
                    psum.release(ps)

                if not interleave:
                    for j in range(len(jobs) + DEPTH):
                        if j < len(jobs):
                            stageA(j)
                        if j >= DEPTH:
                            stageB(j - DEPTH)
                return stageA, stageB

            def v_proj(l, w, slot_cur, flagcol):
                Vv = Vaug[l][:, :].rearrange("p (b c x) -> p b c x", b=8, c=4)
                for tb in range(4):
                    ps = psum.alloc()
                    for kk in range(KC):
                        mm(ps[:, :], hT[:, kk * T + tb * 128:kk * T + (tb + 1) * 128],
                           w[:, kk * 512:(kk + 1) * 512], kk == 0, kk == KC - 1, [hTk[kk], w], [ps])
                    bi = slot_cur * 4 + tb
                    psv = ps[:, :].rearrange("p (c two d) -> p c two d", c=4, two=2)
                    dve(lambda bi=bi, psv=psv: nc.vector.tensor_scalar(
                        out=Vv[:, bi, :, 0:64], in0=psv[:, :, 0, :], scalar1=flagcol, scalar2=None,
                        op0=ALU.mult), [ps, flags], [Vaug[l]])
                    dve(lambda bi=bi, psv=psv: nc.vector.tensor_scalar(
                        out=Vv[:, bi, :, 128:192], in0=psv[:, :, 1, :], scalar1=flagcol, scalar2=None,
                        op0=ALU.mult), [ps, flags], [Vaug[l]])
                    psum.release(ps)
                    act(A_(Vv[:, bi, :, 64:128], ones_f[:, :].rearrange("p (c d) -> p c d", c=4), AF.Copy,
                           scale=flagcol, bias=1e-10), [ones_f, flags], [Vaug[l]])

            def a_proj(w, cc):
                ps = proj_h(w, 0, 512, cc * 128)
                act(A_(aext[:, cc * 528 + 16:cc * 528 + 528], ps[:, :], AF.Copy), [ps], [aext])
                psum.release(ps)

            def save_atail(l):
                for c in range(2):
                    dve(lambda c=c: nc.vector.tensor_copy(out=atail[l][:, c * 16:(c + 1) * 16],
                                                          in_=aext[:, c * 528 + 512:c * 528 + 528]),
                        [aext], [atail[l]])

            def layer_tile_partial(t, l, mode):
                slot_cur = t % 2
                fm_norm(l, C_MIXG)
                if mode == "kva":
                    w = next_unit(0)
                    for cc in range(2):
                        a_proj(w, cc)
                    save_atail(l)
                w = next_unit(3)
                qk_chunks(l, [(w, c, 1, slot_cur) for c in range(4)])
                w = next_unit(4)
                v_proj(l, w, slot_cur, hv_col)

            def layer_tile(t, l, is_halo):
                flagcol = hv_col if is_halo else one_col
                slot_cur = t % 2
                load_E(l)

                fm_norm(l, C_MIXG, src=lambda k: xsrc(t, l, k))

                w = next_unit(0)
                u0ps = proj_h4(w, 0, 512, [0, 128, 256, 384])
                for cc in range(2):
                    act(A_(aext[:, cc * 528 + 16:cc * 528 + 528], u0ps[cc][:, :], AF.Copy), [u0ps[cc]], [aext])
                    psum.release(u0ps[cc])
                for c in range(2):
                    if t == NH:
                        pool(lambda c=c: nc.gpsimd.tensor_tensor(
                            out=aext[:, c * 528:c * 528 + 16], in0=atail[l][:, c * 16:(c + 1) * 16],
                            in1=flags[:, 40:56], op=ALU.mult), [atail[l], flags], [aext])
                    else:
                        pool(lambda c=c: nc.gpsimd.tensor_copy(
                            out=aext[:, c * 528:c * 528 + 16], in_=atail[l][:, c * 16:(c + 1) * 16]),
                            [atail[l]], [aext])

                def shadd(dst, src, c, sh, p0, p1):
                    lo = 2 * sh - 1
                    pool(lambda: nc.gpsimd.tensor_tensor(
                        out=dst[p0:p1, c * 528 + lo:c * 528 + 528],
                        in0=src[p0:p1, c * 528 + lo:c * 528 + 528],
                        in1=src[p0:p1, c * 528 + lo - sh:c * 528 + 528 - sh], op=ALU.add), [src], [dst])

                shadd(sA, aext, 0, 1, 0, 128)
                shadd(sB, sA, 0, 2, 64, 128)
                shadd(sA, aext, 1, 1, 0, 128)
                shadd(sB, sA, 1, 2, 0, 128)
                shadd(sA, sB, 1, 4, 0, 128)
                shadd(sB, sA, 1, 8, 64, 128)
                if t == NH:
                    for c in range(2):
                        for (src, p0, p1) in ((sA, 0, 64), (sB, 64, 128)):
                            pool(lambda c=c, src=src, p0=p0, p1=p1: nc.gpsimd.tensor_tensor(
                                out=src[p0:p1, c * 528 + 16:c * 528 + 32], in0=src[p0:p1, c * 528 + 16:c * 528 + 32],
                                in1=flags[p0:p1, 2 + c * 16:2 + (c + 1) * 16], op=ALU.mult), [src, flags], [src])
                for c in range(2):
                    pool(lambda c=c: nc.gpsimd.tensor_copy(out=atail[l][:, c * 16:(c + 1) * 16],
                                                           in_=aext[:, c * 528 + 512:c * 528 + 528]),
                         [aext], [atail[l]])
                for cc in range(2, 4):
                    ps = u0ps[cc]
                    act(A_(guT[:, (cc - 2) * T:(cc - 1) * T], ps[:, :], AF.Gelu_apprx_tanh), [ps], [guT])
                    psum.release(ps)
                w = next_unit(1)
                vts = []
                for tb in range(4):
                    ps = psum.alloc()
                    for kk in range(KC):
                        mm(ps[:, 0:256], hT[:, kk * T + tb * 128:kk * T + (tb + 1) * 128],
                           w[:, kk * 256:(kk + 1) * 256], kk == 0, kk == KC - 1, [hTk[kk], w], [ps])
                    vt = vtr.next()
                    act(A_(vt[:, :], ps[:, 0:256], AF.Gelu_apprx_tanh), [ps], [vt])
                    psum.release(ps)
                    act(A_(rstd[:, 0:256], vt[:, :], AF.Square, accum_out=vss[:, tb:tb + 1]), [vt], [rstd, vss])
                    vts.append(vt)
                act(A_(vss[:, :], vss[:, :], AF.Ln, scale=1.0 / 256, bias=EPS), [vss], [vss])
                act(A_(vss[:, :], vss[:, :], AF.Exp, scale=-0.5), [vss], [vss])
                for tb in range(4):
                    vt = vts[tb]
                    dve(lambda tb=tb, vt=vt: nc.vector.scalar_tensor_tensor(
                        out=vn[:, tb * 256:(tb + 1) * 256], in0=vt[:, :], scalar=vss[:, tb:tb + 1],
                        in1=gnb[l][:, :], op0=ALU.mult, op1=ALU.mult), [vt, vss, gnb[l]], [vn])
                if INTERLEAVE_QK:
                    w = next_unit(4)
                    v_proj(l, w, slot_cur, flagcol)
                    wstate["hold"] = wstate["consumed"]
                    wq = next_unit(2)
                    wk = next_unit(3)
                    qk_jobs = []
                    for c in range(4):
                        qk_jobs += [(wq, c, 0, slot_cur), (wk, c, 1, slot_cur)]
                    qkA, qkB = qk_chunks(l, qk_jobs, interleave=True)
                    qkA(0)
                    qkA(1)
                    qkB(0)
                    qkB(1)
                else:
                    wstate["hold"] = wstate["consumed"]
                    wq = next_unit(2)
                    wk = next_unit(3)
                    qk_chunks(l, [(wq, c, 0, slot_cur) for c in range(4)] + [(wk, c, 1, slot_cur) for c in range(4)])
                    wstate["hold"] = None
                    w = next_unit(4)
                    v_proj(l, w, slot_cur, flagcol)

                steps = []
                groups = [[3], [0, 2], [1, 6], [7, 5], [4]]
                for h in range(8):
                    for gi, g in enumerate(groups):
                        steps.append((h, g, gi == 0, gi == len(groups) - 1))
                LAG = NROT - 1
                pv_of = {}
                pt_of = {}
                mulcnt = [0]

                def geom(wi):
                    ca, cb = 2 * wi - 8, 2 * wi - 7
                    qlo = max(ca, 0) * 64
                    qhi = (min(cb + 8, 7) + 1) * 64
                    K0 = (wi - 4) * 128
                    return qlo, qhi, K0

                def emit_score(i):
                    h, g, firstg, lastg = steps[i]
                    c, hh = h // 2, h % 2
                    sc = psum.alloc()
                    parts = []
                    off = 0
                    for wi in g:
                        qlo, qhi, K0 = geom(wi)
                        N = qhi - qlo
                        kslot = (1 - slot_cur) if wi < 4 else slot_cur
                        ko = (c * 2 + kslot) * T + (wi % 4) * 128
                        mm(sc[:, off:off + N], kTc[l][hh * 64:(hh + 1) * 64, ko:ko + 128],
                           qn[hh * 64:(hh + 1) * 64, c * T + qlo:c * T + qhi], True, True, [kTck[l][c], qnk[c]], [sc])
                        parts.append((wi, off, N, qlo, qhi, K0))
                        off += N
                    es_ = esr.next()
                    act(A_(es_[:, 0:off], sc[:, 0:off], AF.Exp, scale=0.125), [sc], [es_])
                    psum.release(sc)
                    pt = ptr.next()
                    for (wi, o, N, qlo, qhi, K0) in parts:
                        eo = h * 640 + qlo - K0
                        mulcnt[0] += 1
                        if mulcnt[0] % 2 == 0:
                            pool(lambda: nc.gpsimd.tensor_tensor(out=pt[:, o:o + N], in0=es_[:, o:o + N],
                                                                 in1=Etab[:, eo:eo + N], op=ALU.mult),
                                 [es_, Etab], [pt])
                        else:
                            dve(lambda: nc.vector.tensor_tensor(out=pt[:, o:o + N], in0=es_[:, o:o + N],
                                                                in1=Etab[:, eo:eo + N], op=ALU.mult),
                                [es_, Etab], [pt])
                    pt_of[i] = (pt, parts)

                def emit_pv(i):
                    h, g, firstg, lastg = steps[i]
                    c, hh = h // 2, h % 2
                    if firstg:
                        pv_of[h] = psum.alloc()
                    pv = pv_of[h]
                    pt, parts = pt_of.pop(i)
                    for pi_, (wi, o, N, qlo, qhi, K0) in enumerate(parts):
                        first = firstg and pi_ == 0
                        lastk = lastg and pi_ == len(parts) - 1
                        kslot = (1 - slot_cur) if wi < 4 else slot_cur
                        bi = kslot * 4 + (wi % 4)
                        vo = bi * 768 + c * 192 + (0 if hh == 0 else 64)
                        mm(pv[:, qlo:qhi], Vaug[l][:, vo:vo + 128], pt[:, o:o + N], first, lastk, [Vaug[l], pt], [pv])
                    if lastg:
                        n0, n1 = (0, 64) if hh == 0 else (64, 128)
                        d0, d1 = (64, 128) if hh == 0 else (0, 64)
                        act(A_(rden[n0:n1, :], pv[d0:d1, :], AF.Ln), [pv], [rden])
                        act(A_(rden[n0:n1, :], rden[n0:n1, :], AF.Exp, scale=-1.0), [rden], [rden])
                        dve(lambda: nc.vector.tensor_tensor(out=ycT[n0:n1, c * T:(c + 1) * T], in0=pv[n0:n1, :],
                                                            in1=rden[n0:n1, :], op=ALU.mult), [pv, rden], [ycTk[c]])
                        psum.release(pv)

                def emit_pooled():
                    for c in range(2):
                        for (src, p0, p1) in ((sA, 0, 64), (sB, 64, 128)):
                            dve(lambda c=c, src=src, p0=p0, p1=p1: nc.vector.scalar_tensor_tensor(
                                out=pooled[p0:p1, c * T:(c + 1) * T], in0=src[p0:p1, c * 528 + 16:c * 528 + 528],
                                scalar=gcols[l][p0:p1, C_INVW + c:C_INVW + c + 1],
                                in1=aext[p0:p1, c * 528 + 16:c * 528 + 528], op0=ALU.mult, op1=ALU.subtract),
                                [src, aext, gcols[l]], [pooled])

                inject = {}
                for c in (range(1, 4) if INTERLEAVE_QK else ()):
                    b0 = 16 * (c - 1)
                    inject[b0 + 1] = (qkA, 2 * c)
                    inject[b0 + 5] = (qkA, 2 * c + 1)
                    inject[b0 + 9] = (qkB, 2 * c)
                    inject[b0 + 13] = (qkB, 2 * c + 1)
                for i in range(len(steps) + LAG):
                    if i in inject:
                        inject[i][0](inject[i][1])
                    if i == 15:
                        emit_pooled()
                    if i < len(steps):
                        emit_score(i)
                    if i >= LAG:
                        emit_pv(i - LAG)
                wstate["hold"] = None

                for c in range(2):
                    ps = psum.alloc()
                    mm(ps[:, :], wbd[l][:, c * 128:(c + 1) * 128], pooled[:, c * T:(c + 1) * T], True, True,
                       [wbd[l], pooled], [ps])
                    act(A_(yaT[:, c * T:(c + 1) * T], ps[:, :], AF.Copy,
                           scale=gcols[l][:, C_PSCALE + c:C_PSCALE + c + 1]), [ps, gcols[l]], [yaT])
                    psum.release(ps)

                for c in range(2):
                    ps = psum.alloc()
                    for blk in range(4):
                        for gg in range(2):
                            g = 2 * c + gg
                            mm(ps[gg * 64:(gg + 1) * 64, blk * 128:(blk + 1) * 128],
                               vn[:, blk * 256 + g * 64:blk * 256 + (g + 1) * 64],
                               wmT[l][:, g * 128:(g + 1) * 128], True, True, [vn, wmT[l]], [ps])
                    mt = mtr.next()
                    for blk in range(4):
                        dve(lambda blk=blk, mt=mt, ps=ps, c=c: nc.vector.tensor_tensor(
                            out=mt[:, blk * 128:(blk + 1) * 128], in0=ps[:, blk * 128:(blk + 1) * 128],
                            in1=bT[l][:, c * 128:(c + 1) * 128], op=ALU.add), [ps, bT[l]], [mt])
                    psum.release(ps)
                    dve(lambda mt=mt, c=c: nc.vector.tensor_tensor(
                        out=ybT[:, c * T:(c + 1) * T], in0=mt[:, :], in1=guT[:, c * T:(c + 1) * T], op=ALU.mult),
                        [mt, guT], [ybT])

                ybr = ((yaT, [yaT] * 2), (ybT, [ybT] * 2), (ycT, ycTk))
                nkb = (2, 2, 4)
                boff = (0, 256, 512)
                for dc in range(KC):
                    w = next_unit(5 + dc)
                    macc = None
                    for br in range(3):
                        psg = proj_h(w, br * 1024, 128, 0)
                        sg = sigr.next()
                        act(A_(sg[:, :], psg[:, :], AF.Sigmoid,
                               bias=gcols[l][:, C_GATEB + br * 8 + dc:C_GATEB + br * 8 + dc + 1]),
                            [psg, gcols[l]], [sg])
                        psum.release(psg)
                        psb = proj_fm(w, 3072 + boff[br], 128, 0, ybr[br][0], ybr[br][1], nkb[br], T)
                        if br == 0:
                            macc = mtr.next()
                            dve(lambda psb=psb, sg=sg, macc=macc: nc.vector.tensor_tensor(
                                out=macc[:, :], in0=psb[:, :], in1=sg[:, :], op=ALU.mult), [psb, sg], [macc])
                        else:
                            tmp = mtr.next()
                            dve(lambda psb=psb, sg=sg, tmp=tmp: nc.vector.tensor_tensor(
                                out=tmp[:, :], in0=psb[:, :], in1=sg[:, :], op=ALU.mult), [psb, sg], [tmp])
                            if br == 1:
                                dve(lambda tmp=tmp, macc=macc: nc.vector.tensor_tensor(
                                    out=macc[:, :], in0=macc[:, :], in1=tmp[:, :], op=ALU.add), [macc, tmp], [macc])
                            else:
                                dve(lambda tmp=tmp, macc=macc, dc=dc: nc.vector.tensor_tensor(
                                    out=merged[:, dc * T:(dc + 1) * T], in0=macc[:, :], in1=tmp[:, :], op=ALU.add),
                                    [macc, tmp], [mergedk[dc]])
                        psum.release(psb)
                for half in range(2):
                    w = next_unit(13 + half)
                    for o4 in range(4):
                        oc = half * 4 + o4
                        ps = proj_fm(w, 0, 512, o4 * 128, merged, mergedk, KC, T)
                        xa, xb = xsrc(t, l, oc)
                        dve(lambda ps=ps, oc=oc, xa=xa: nc.vector.tensor_tensor(
                            out=xT[:, oc * T:(oc + 1) * T], in0=xa, in1=ps[:, :], op=ALU.add),
                            [ps, xb, xTk[oc]], [xTk[oc]])
                        psum.release(ps)
                if l == NL - 1 and t >= NH and t + 1 < NT:
                    load_side(t + 1)

                load_p(t, l)

                fm_norm(l, C_FFNG, want_tm=True)
                lps = []
                for tb in range(4):
                    ps = psum.alloc()
                    for kk in range(KC):
                        mm(ps[:, 0:36], xT[:, kk * T + tb * 128:kk * T + (tb + 1) * 128],
                           wr[l][:, kk * 36:(kk + 1) * 36], kk == 0, kk == KC - 1, [xTk[kk], wr[l]], [ps])
                    lps.append(ps)
                gts = []
                for tb in range(4):
                    ps = lps[tb]
                    dve(lambda ps=ps, tb=tb: nc.vector.scalar_tensor_tensor(
                        out=lg[:, :], in0=ps[:, 0:36], scalar=rstdtm[:, tb:tb + 1], in1=brt[l][:, :],
                        op0=ALU.mult, op1=ALU.add), [ps, rstdtm, brt[l]], [lg])
                    psum.release(ps)
                    sm = rsm.next()
                    dve(lambda sm=sm: nc.vector.reduce_max(out=sm[:, 0:1], in_=lg[:, 0:4], axis=AX.X), [lg], [sm])
                    dve(lambda sm=sm: nc.vector.tensor_scalar(out=ohg[:, :], in0=lg[:, 0:4], scalar1=sm[:, 0:1],
                                                               scalar2=None, op0=ALU.is_ge), [lg, sm], [ohg])
                    dve(lambda sm=sm: nc.vector.tensor_scalar(out=sm[:, 1:2], in0=sm[:, 0:1], scalar1=-1.0,
                                                               scalar2=None, op0=ALU.mult), [sm], [sm])
                    act(A_(gexp[:, :], lg[:, 0:4], AF.Exp, bias=sm[:, 1:2], accum_out=sm[:, 2:3]), [lg, sm], [gexp, sm])
                    dve(lambda: nc.vector.tensor_scalar(out=le[:, :], in0=lg[:, 4:12], scalar1=ohg[:, 0:1],
                                                        scalar2=None, op0=ALU.mult), [lg, ohg], [le])
                    for g in range(1, 4):
                        dve(lambda g=g: nc.vector.scalar_tensor_tensor(
                            out=le[:, :], in0=lg[:, 4 + g * 8:12 + g * 8], scalar=ohg[:, g:g + 1], in1=le[:, :],
                            op0=ALU.mult, op1=ALU.add), [lg, ohg, le], [le])
                    dve(lambda: nc.vector.max(out=m8[:, :], in_=le[:, :]), [le], [m8])
                    dve(lambda sm=sm: nc.vector.tensor_scalar(out=sm[:, 3:4], in0=m8[:, 0:1], scalar1=-1.0,
                                                               scalar2=None, op0=ALU.mult), [m8], [sm])
                    act(A_(ee[:, :], le[:, :], AF.Exp, bias=sm[:, 3:4]), [le, sm], [ee])
                    dve(lambda sm=sm: nc.vector.scalar_tensor_tensor(
                        out=wsel[:, :], in0=le[:, :], scalar=m8[:, 1:2], in1=ee[:, :], op0=ALU.is_ge, op1=ALU.mult,
                        accum_out=sm[:, 4:5]), [le, m8, ee], [wsel, sm])
                    dve(lambda sm=sm: nc.vector.tensor_tensor(out=sm[:, 5:6], in0=sm[:, 4:5], in1=sm[:, 2:3],
                                                               op=ALU.mult), [sm], [sm])
                    dve(lambda sm=sm: nc.vector.reciprocal(out=sm[:, 5:6], in_=sm[:, 5:6]), [sm], [sm])
                    gt_ = gates.next()
                    for g in range(4):
                        dve(lambda g=g, gt_=gt_, sm=sm: nc.vector.tensor_scalar(
                            out=gt_[:, g * 8:(g + 1) * 8], in0=wsel[:, :], scalar1=ohg[:, g:g + 1],
                            scalar2=sm[:, 5:6], op0=ALU.mult, op1=ALU.mult), [wsel, ohg, sm], [gt_])
                    gts.append(gt_)

                def emit_gatesT():
                    gps = psum.alloc()
                    for tb in range(4):
                        gt_ = gts[tb]
                        S.op(PE, lambda gt_=gt_, tb=tb: nc.tensor.transpose(
                            out=gps[0:32, tb * 128:(tb + 1) * 128], in_=gt_[:, :], identity=ident[:, :]),
                            reads=[gt_.b, ident.b], writes=[gps.b])
                    act(A_(gatesT[:, :], gps[0:32, :], AF.Copy), [gps], [gatesT])
                    psum.release(gps)

                ub = 15
                for pi in range(NPASS):
                    for j in range(EPP):
                        if j % 2 == 0:
                            w = next_unit(ub + pi * (EPP // 2 + 2) + j // 2)
                        base = (j % 2) * 2048
                        psg = proj_h(w, base, 256, 0)
                        psu = proj_h(w, base, 256, 128)
                        sl = sigr.next()
                        act(A_(sl[:, :], psg[:, :], AF.Silu), [psg], [sl])
                        psum.release(psg)
                        dve(lambda psu=psu, sl=sl, j=j: nc.vector.tensor_tensor(
                            out=merged[:, j * T:(j + 1) * T], in0=psu[:, :], in1=sl[:, :], op=ALU.mult),
                            [psu, sl], [mergedk[j]])
                        psum.release(psu)
                    if pi == 0:
                        emit_gatesT()
                    for j in range(EPP):
                        e = pi * EPP + j
                        psb = psum.alloc()
                        mm(psb[:, :], sel[0:32, e * 128:(e + 1) * 128], gatesT[0:32, :], True, True,
                           [sel, gatesT], [psb])
                        dve(lambda psb=psb, j=j: nc.vector.tensor_tensor(
                            out=merged[:, j * T:(j + 1) * T], in0=psb[:, :], in1=merged[:, j * T:(j + 1) * T],
                            op=ALU.mult), [psb, mergedk[j]], [mergedk[j]])
                        psum.release(psb)
                    for half in range(2):
                        w = next_unit(ub + pi * (EPP // 2 + 2) + EPP // 2 + half)
                        for o4 in range(4):
                            oc = half * 4 + o4
                            ps = proj_fm(w, 0, 512, o4 * 128, merged, mergedk, EPP, T)
                            dve(lambda ps=ps, oc=oc: nc.vector.tensor_tensor(
                                out=xT[:, oc * T:(oc + 1) * T], in0=xT[:, oc * T:(oc + 1) * T], in1=ps[:, :],
                                op=ALU.add), [ps, xTk[oc]], [xTk[oc]])
                            psum.release(ps)

                fm_norm(l, C_PLEG)
                ub2 = 15 + NPASS * (EPP // 2 + 2)
                wstate["hold"] = wstate["consumed"]
                wpi = next_unit(ub2)
                last_layer = (l == NL - 1)
                for half in range(2):
                    w = next_unit(ub2 + 1 + half)
                    pre = proj_h4(w, 0, 512, [0, 128, 256, 384]) if half == 0 else None
                    for o4 in range(4):
                        oc = half * 4 + o4
                        psg = pre[o4] if pre is not None else proj_h(w, 0, 512, o4 * 128)
                        sg = sigr.next()
                        act(A_(sg[:, :], psg[:, :], AF.Sigmoid), [psg], [sg])
                        psum.release(psg)
                        pse = proj_fm(wpi, 0, 1024, oc * 128, pTs, [pTs] * 2, 2, T)
                        tmp = mtr.next()
                        dve(lambda pse=pse, sg=sg, tmp=tmp: nc.vector.tensor_tensor(
                            out=tmp[:, :], in0=pse[:, :], in1=sg[:, :], op=ALU.mult), [pse, sg], [tmp])
                        psum.release(pse)
                        dve(lambda tmp=tmp, oc=oc: nc.vector.tensor_tensor(
                            out=xT[:, oc * T:(oc + 1) * T], in0=xT[:, oc * T:(oc + 1) * T], in1=tmp[:, :],
                            op=ALU.add), [tmp, xTk[oc]], [xTk[oc]])
                        if last_layer and t >= NH:
                            to = t - NH
                            S.dma(SP, st_y[oc], lambda oc=oc, to=to: nc.sync.dma_start(
                                out=yTd[oc * 128:(oc + 1) * 128, to * T:(to + 1) * T],
                                in_=xT[:, oc * T:(oc + 1) * T]), reads=[xTk[oc].b])
                            if t + 1 < NT and 1 <= oc <= KC - 2:
                                load_x_chunk(t + 1, oc - 1)
                if last_layer and t >= NH and t + 1 < NT:
                    prefetched.add(t + 1)
                wstate["hold"] = None

            for t in range(NT):
                if any(modes[(t, l)] != "skip" for l in range(NL)):
                    load_x(t)
                for l in range(NL):
                    m = modes[(t, l)]
                    if m == "full":
                        layer_tile(t, l, t < NH)
                    elif m in ("kva", "kv"):
                        layer_tile_partial(t, l, m)
            for k in range(KC):
                nc.sync.wait_ge(st_y[k].sem, st_y[k].cnt)

        @block.sync
        def _(sync):
            body()
        assert wstate["consumed"] == len(unit_list) and wstate["issued"] == len(unit_list)
    return nc


_PROG_CACHE = {}


def _get_prog(NL, NH, NOWN):
    key = (NL, NH, NOWN)
    if key not in _PROG_CACHE:
        _PROG_CACHE[key] = build_program(NL, NH, NOWN)
    return _PROG_CACHE[key]


def _consts():
    c = np.zeros((128, 128 + 32 * 128), np.float32)
    c[:, 0:128] = np.eye(128, dtype=np.float32)
    for e in range(32):
        c[e, 128 + e * 128:128 + (e + 1) * 128] = 1.0
    return c


def _flags(first_half):
    f = np.zeros((128, 56), np.float32)
    f[:, 0] = 0.0 if first_half else 1.0
    f[:, 40:56] = 0.0 if first_half else 1.0
    f[:, 1] = 1.0
    wins = (2, 4, 8, 16)
    for c in range(2):
        for p in range(128):
            w = wins[2 * c + p // 64]
            for i in range(16):
                f[p, 2 + c * 16 + i] = (w / min(i + 1, w)) if first_half else 1.0
    return f


def _layer_tables(inp, l):
    f32 = np.float32
    g = np.zeros((128, NCOL), f32)
    g[:, C_MIXG:C_MIXG + 8] = inp["mix_norm_g"][l].reshape(8, 128).T
    g[:, C_FFNG:C_FFNG + 8] = inp["ffn_norm_g"][l].reshape(8, 128).T
    g[:, C_PLEG:C_PLEG + 8] = inp["ple_norm_g"][l].reshape(8, 128).T
    g[:, C_PSCALE:C_PSCALE + 2] = inp["pool_scale"][l].reshape(2, 128).T
    wins = np.array([2, 4, 8, 16], f32)
    for c in range(2):
        g[0:64, C_INVW + c] = 1.0 / wins[2 * c]
        g[64:128, C_INVW + c] = 1.0 / wins[2 * c + 1]
    g[:, C_GQ] = np.tile(inp["q_norm_g"][l], 2)
    g[:, C_GK] = np.tile(inp["k_norm_g"][l], 2)
    g[:, C_GATEB:C_GATEB + 24] = inp["gate_b"][l].reshape(3, 8, 128).transpose(2, 0, 1).reshape(128, 24)
    gnb = np.broadcast_to(inp["sgu_norm_g"][l][None, :], (128, 256)).astype(f32)
    sb_ = inp["sgu_b"][l]
    bT = np.zeros((128, 256), f32)
    for c in range(2):
        bT[0:64, c * 128:(c + 1) * 128] = sb_[2 * c][None, :]
        bT[64:128, c * 128:(c + 1) * 128] = sb_[2 * c + 1][None, :]
    wmT = inp["sgu_w"][l].transpose(2, 0, 1).reshape(128, 512).astype(f32)
    wbd = np.zeros((128, 256), f32)
    for c in range(2):
        for gp in range(2):
            wbd[gp * 64:(gp + 1) * 64, c * 128 + gp * 64:c * 128 + (gp + 1) * 64] = inp["pool_w"][l][2 * c + gp]
    wrt = np.concatenate([inp["w_group_router"][l], inp["w_expert_router"][l]], axis=1)
    wr = wrt.reshape(8, 128, 36).transpose(1, 0, 2).reshape(128, 288).astype(f32)
    br = np.broadcast_to(np.concatenate([inp["b_group_router"][l], inp["b_expert_router"][l]])[None, :],
                         (128, 36)).astype(f32)
    kk = np.arange(128)[:, None]
    qq = np.arange(640)[None, :]
    idx = np.clip(qq - kk, -128, 128) + 128
    toep = inp["rel_bias"][l][:, idx].transpose(1, 0, 2).reshape(128, 8 * 640).astype(f32)
    return dict(gcols=g, gnb=gnb, sgubT=bT, sguwT=wmT, poolbd=wbd, wrouter=wr, brouter=br, toep=toep)


def _run(inp, x, layers, NH):
    NL = len(layers)
    B, S_, _ = x.shape
    NOWN = S_ // 2 // T
    NT = NH + NOWN
    ncores = 2 * B
    nc = _get_prog(NL, NH, NOWN)
    tabs = [_layer_tables(inp, l) for l in layers]
    shared = {k: np.ascontiguousarray(np.stack([tb[k] for tb in tabs])) for k in tabs[0]}
    shared["wstream"] = np.ascontiguousarray(np.stack([
        pack_layer_weights(inp["w_in"][l], inp["w_branch_a"][l], inp["w_branch_b"][l], inp["w_branch_c"][l],
                           inp["w_out"][l], inp["w_expert_in"][l], inp["w_expert_out"][l],
                           inp["w_ple_in"][l], inp["w_ple_gate"][l]) for l in layers]))
    shared["consts"] = _consts()
    in_maps = []
    for c in range(ncores):
        b, half = c // 2, c % 2
        lo = half * (S_ // 2) - NH * T
        xs = np.zeros((NT * T, D), np.float32)
        ps_ = np.zeros((NL, NT * T, 256), np.float32)
        src_lo = max(lo, 0)
        xs[src_lo - lo:] = x[b, src_lo:lo + NT * T]
        for i, l in enumerate(layers):
            ps_[i, src_lo - lo:] = inp["p"][l, b, src_lo:lo + NT * T]
        m = dict(shared)
        m["xT"] = np.ascontiguousarray(xs.T)
        m["pT"] = np.ascontiguousarray(ps_.transpose(0, 2, 1))
        m["flags"] = _flags(half == 0)
        in_maps.append(m)
    res = RUNNER(nc, in_maps, core_ids=list(range(ncores)))
    out = np.zeros((B, S_, D), np.float32)
    for c in range(ncores):
        b, half = c // 2, c % 2
        out[b, half * (S_ // 2):(half + 1) * (S_ // 2)] = res.results[c]["yT"].T
    return out


FUSED = True


def RUNNER(nc, in_maps, core_ids):
    return run_bass_kernel_spmd(nc, in_maps, core_ids=core_ids)


def kernel(**inputs):
    inp = {k: np.asarray(v) for k, v in inputs.items()}
    x = inp["x"].astype(np.float32, copy=False)
    if FUSED:
        return _run(inp, x, [0, 1], 2)
    for l in range(2):
        x = _run(inp, x, [l], 1)
    return x
```

```python
import contextlib
import numpy as np
import concourse.bass as bass
import concourse.mybir as mybir
from concourse.bass_utils import run_bass_kernel_spmd

F32 = mybir.dt.float32
BF16 = mybir.dt.bfloat16
AF = mybir.ActivationFunctionType
ALU = mybir.AluOpType
AX = mybir.AxisListType

D = 1024
T = 512
KC = 8
NOWN = 8
EPS = 1e-6
NSLOT = 4
SLOT = 4096
NE = 32
EPP = 8
NPASS = NE // EPP

C_MIXG, C_FFNG, C_PLEG = 0, 8, 16
C_PSCALE, C_INVW = 24, 26
C_GQ, C_GK = 28, 29
C_GATEB = 30
NCOL = 54


class Buf:
    __slots__ = ("name", "w", "r")

    def __init__(self, name):
        self.name = name
        self.w = None
        self.r = {}


class Eng:
    def __init__(self, name, handle, sem, in_order=False):
        self.name = name
        self.h = handle
        self.sem = sem
        self.cnt = 0
        self.waited = {}
        self.in_order = in_order


class Stream:
    def __init__(self, sem):
        self.sem = sem
        self.cnt = 0


class Sched:
    def __init__(self, nc):
        self.nc = nc
        self.n_wait = 0

    def _waits(self, eng, reads, writes):
        need = {}

        def add(ev):
            if ev is None:
                return
            sem, val = ev
            k = id(sem)
            if k not in need or need[k][1] < val:
                need[k] = (sem, val)

        for b in reads:
            add(b.w)
        for b in writes:
            add(b.w)
            for ev in b.r.values():
                add(ev)
        for k, (sem, val) in need.items():
            if sem is eng.sem and eng.in_order:
                continue
            if eng.waited.get(k, 0) >= val:
                continue
            eng.h.wait_ge(sem, val)
            eng.waited[k] = val
            self.n_wait += 1

    def op(self, eng, fn, reads=(), writes=(), last=True):
        self._waits(eng, reads, writes)
        ins = fn()
        ev = (eng.sem, eng.cnt + 1)
        if last:
            eng.cnt += 1
            ins.then_inc(eng.sem, 1)
        for b in reads:
            b.r[id(eng.sem)] = ev
        for b in writes:
            b.w = ev
            b.r = {}
        return ins

    def dma(self, qeng, stream, fn, reads=(), writes=()):
        self._waits(qeng, reads, writes)
        ins = fn()
        stream.cnt += 16
        ins.then_inc(stream.sem, 16)
        ev = (stream.sem, stream.cnt)
        for b in reads:
            b.r[id(stream.sem)] = ev
        for b in writes:
            b.w = ev
            b.r = {}
        return ins


class TB:
    def __init__(self, t, name):
        self.t = t
        self.b = Buf(name)

    def __getitem__(self, idx):
        return self.t[idx]


class Rot:
    def __init__(self, items):
        self.items = items
        self.i = 0

    def next(self):
        it = self.items[self.i % len(self.items)]
        self.i += 1
        return it


class PsumPool:
    def __init__(self, banks):
        self.free = list(banks)

    def alloc(self):
        assert self.free, "psum pool exhausted"
        return self.free.pop(0)

    def release(self, b):
        self.free.append(b)


def unit_sizes():
    u = [4096, 2048, 4096, 4096, 4096]
    u += [4096] * 8
    u += [4096, 4096]
    for _ in range(NPASS):
        u += [4096] * (EPP // 2)
        u += [4096, 4096]
    u += [2048, 4096, 4096]
    return u


def pack_layer_weights(w_in, wa, wb, wc, w_out, we_in, we_out, wple_in, wple_gate):
    def kp(m):
        K = m.shape[0] // 128
        return m.reshape(K, 128, m.shape[1]).transpose(1, 0, 2)

    units = []
    win = kp(w_in)
    units.append(win[:, :, 0:512])
    units.append(win[:, :, 512:768])
    units.append(win[:, :, 768:1280])
    units.append(win[:, :, 1280:1792])
    units.append(win[:, :, 1792:2304])
    wak, wbk, wck = kp(wa), kp(wb), kp(wc)
    for dc in range(8):
        cs = slice(dc * 128, (dc + 1) * 128)
        parts = []
        for br in range(3):
            c0 = 2304 + br * 1024 + dc * 128
            parts.append(win[:, :, c0:c0 + 128].reshape(128, -1))
        parts.append(wak[:, :, cs].reshape(128, -1))
        parts.append(wbk[:, :, cs].reshape(128, -1))
        parts.append(wck[:, :, cs].reshape(128, -1))
        units.append(np.concatenate(parts, axis=1))
    wo = kp(w_out)
    units.append(wo[:, :, 0:512])
    units.append(wo[:, :, 512:1024])
    for pi in range(NPASS):
        for j in range(0, EPP, 2):
            e = pi * EPP + j
            units.append(np.concatenate([kp(we_in[e]).reshape(128, -1),
                                         kp(we_in[e + 1]).reshape(128, -1)], axis=1))
        eo = we_out[pi * EPP:(pi + 1) * EPP].transpose(1, 0, 2)
        units.append(eo[:, :, 0:512])
        units.append(eo[:, :, 512:1024])
    units.append(kp(wple_in))
    wg = kp(wple_gate)
    units.append(wg[:, :, 0:512])
    units.append(wg[:, :, 512:1024])
    flat = [np.ascontiguousarray(u).reshape(128, -1) for u in units]
    sizes = unit_sizes()
    assert [f.shape[1] for f in flat] == sizes, ([f.shape[1] for f in flat], sizes)
    return np.ascontiguousarray(np.concatenate(flat, axis=1), dtype=np.float32)


INTERLEAVE_QK = False


def tile_units(mode):
    if mode == "full":
        n = len(unit_sizes())
        return ([0, 1, 4, 2, 3] if INTERLEAVE_QK else [0, 1, 2, 3, 4]) + list(range(5, n))
    if mode == "kva":
        return [0, 3, 4]
    if mode == "kv":
        return [3, 4]
    return []


def tile_modes(NL, NH, NT):
    modes = {}
    for t in range(NT):
        for l in range(NL):
            if t >= NH:
                m = "full"
            elif NL == 1:
                m = "kva"
            else:
                d = NH - t
                if d == 1:
                    m = "full" if l < NL - 1 else "kva"
                else:
                    m = "kva" if l == 0 else "skip"
            modes[(t, l)] = m
    return modes


def build_program(NL, NH, NOWN=NOWN):
    NT = NH + NOWN
    NTOK = NT * T
    sizes = unit_sizes()
    TOT = sum(sizes)
    offs = np.concatenate([[0], np.cumsum(sizes)]).astype(int)
    modes = tile_modes(NL, NH, NT)

    nc = bass.Bass("TRN2", target_bir_lowering=False)
    xTd = nc.dram_tensor("xT", [D, NTOK], F32, kind="ExternalInput").ap()
    pTd = nc.dram_tensor("pT", [NL, 256, NTOK], F32, kind="ExternalInput").ap()
    wsd = nc.dram_tensor("wstream", [NL, 128, TOT], F32, kind="ExternalInput").ap()
    gcold = nc.dram_tensor("gcols", [NL, 128, NCOL], F32, kind="ExternalInput").ap()
    gnbd = nc.dram_tensor("gnb", [NL, 128, 256], F32, kind="ExternalInput").ap()
    bTd = nc.dram_tensor("sgubT", [NL, 128, 256], F32, kind="ExternalInput").ap()
    wmTd = nc.dram_tensor("sguwT", [NL, 128, 512], F32, kind="ExternalInput").ap()
    wbdd = nc.dram_tensor("poolbd", [NL, 128, 256], F32, kind="ExternalInput").ap()
    wrd = nc.dram_tensor("wrouter", [NL, 128, 8 * 36], F32, kind="ExternalInput").ap()
    brd = nc.dram_tensor("brouter", [NL, 128, 36], F32, kind="ExternalInput").ap()
    toepd = nc.dram_tensor("toep", [NL, 128, 8 * 640], F32, kind="ExternalInput").ap()
    flagd = nc.dram_tensor("flags", [128, 56], F32, kind="ExternalInput").ap()
    constd = nc.dram_tensor("consts", [128, 128 + 32 * 128], F32, kind="ExternalInput").ap()
    yTd = nc.dram_tensor("yT", [D, NOWN * T], F32, kind="ExternalOutput").ap()
    escr = nc.dram_tensor("escratch", [NL, 128, 8 * 640], BF16).ap()

    es = contextlib.ExitStack()
    with es:
        def sb(name, shape, dt):
            return TB(es.enter_context(nc.sbuf_tensor(name, shape, dt)), name)

        def views(tb, n):
            return [TB(tb.t, f"{tb.b.name}_{i}") for i in range(n)]

        xT = sb("xT_sb", [128, KC * T], F32)
        xTk = views(xT, KC)
        hT = sb("hT", [128, KC * T], BF16)
        xside = sb("xside", [128, 2 * T], F32)
        xsidek = views(xside, 2)
        hTk = views(hT, KC)
        sqr = Rot([sb(f"sq{i}", [128, T], BF16) for i in range(3)])
        rstd = sb("rstd", [128, T], F32)
        rstdtm = sb("rstdtm", [128, 4], F32)
        aext = sb("aext", [128, 2 * 528], F32)
        sA = sb("sA", [128, 2 * 528], F32)
        sB = sb("sB", [128, 2 * 528], F32)
        pooled = sb("pooled", [128, 2 * T], BF16)
        yaT = sb("yaT", [128, 2 * T], BF16)
        guT = sb("guT", [128, 2 * T], BF16)
        vtr = Rot([sb(f"vt{i}", [128, 256], BF16) for i in range(4)])
        vss = sb("vss", [128, 4], F32)
        vn = sb("vn", [128, 4 * 256], BF16)
        ybT = sb("ybT", [128, 2 * T], BF16)
        qn = sb("qn", [128, 4 * T], BF16)
        qnk = views(qn, 4)
        rsr = Rot([sb(f"rs{i}", [128, T], F32) for i in range(3)])
        kTc = [sb(f"kTc{l}", [128, 4 * 2 * T], BF16) for l in range(NL)]
        kTck = [views(kTc[l], 4) for l in range(NL)]
        Vaug = [sb(f"Vaug{l}", [128, 8 * 768], BF16) for l in range(NL)]
        atail = [sb(f"atail{l}", [128, 32], F32) for l in range(NL)]
        Etab = sb("Etab", [128, 8 * 640], BF16)
        NROT = 4
        esr = Rot([sb(f"es{i}", [128, T], BF16) for i in range(NROT)])
        ptr = Rot([sb(f"pt{i}", [128, T], BF16) for i in range(NROT)])
        rden = rsr.items[0]
        ycT = sb("ycT", [128, 4 * T], BF16)
        ycTk = views(ycT, 4)
        sigr = Rot([sb(f"sig{i}", [128, T], F32) for i in range(3)])
        mtr = Rot([sb(f"mt{i}", [128, T], F32) for i in range(3)])
        merged = sb("merged", [128, KC * T], BF16)
        mergedk = views(merged, KC)
        lg = sb("lg", [128, 36], F32)
        rsm = Rot([sb(f"rsm{i}", [128, 16], F32) for i in range(2)])
        ohg = sb("ohg", [128, 4], F32)
        gexp = sb("gexp", [128, 4], F32)
        le = sb("le", [128, 8], F32)
        m8 = sb("m8", [128, 8], F32)
        ee = sb("ee", [128, 8], F32)
        wsel = sb("wsel", [128, 8], F32)
        gates = Rot([sb(f"gates{i}", [128, 32], F32) for i in range(4)])
        gatesT = sb("gatesT", [32, T], BF16)
        pTs = sb("pTs", [128, 2 * T], BF16)
        ring = [sb(f"wslot{i}", [128, SLOT], BF16) for i in range(NSLOT)]
        ones_bf = sb("ones_bf", [128, 128], BF16)
        bd_ones = sb("bd_ones", [128, 128], BF16)
        ones_f = sb("ones_f", [128, 256], F32)
        ident = sb("ident", [128, 128], F32)
        sel = sb("sel", [32, 32 * 128], BF16)
        flags = sb("flags_sb", [128, 56], F32)
        gcols = [sb(f"gcols{l}", [128, NCOL], F32) for l in range(NL)]
        gnb = [sb(f"gnb{l}", [128, 256], F32) for l in range(NL)]
        bT = [sb(f"bT{l}", [128, 256], F32) for l in range(NL)]
        wmT = [sb(f"wmT{l}", [128, 512], BF16) for l in range(NL)]
        wbd = [sb(f"wbd{l}", [128, 256], BF16) for l in range(NL)]
        wr = [sb(f"wr{l}", [128, 8 * 36], F32) for l in range(NL)]
        brt = [sb(f"br{l}", [128, 36], F32) for l in range(NL)]

        banks = [TB(es.enter_context(nc.psum_tensor(f"ps{i}", [128, T], F32)), f"ps{i}") for i in range(8)]
        psum = PsumPool(banks)

        def sem(name):
            return es.enter_context(nc.semaphore(name))

        PE = Eng("pe", nc.tensor, sem("s_pe"), in_order=True)
        ACT = Eng("act", nc.scalar, sem("s_act"))
        DVE = Eng("dve", nc.vector, sem("s_dve"))
        POOL = Eng("pool", nc.gpsimd, sem("s_pool"))
        SP = Eng("sp", nc.sync, sem("s_sp"))
        slot_streams = [Stream(sem(f"s_slot{i}")) for i in range(NSLOT)]
        st_x = [Stream(sem(f"s_x{k}")) for k in range(KC)]
        st_p = Stream(sem("s_p"))
        st_xs = [Stream(sem(f"s_xs{k}")) for k in range(2)]
        st_y = [Stream(sem(f"s_y{k}")) for k in range(KC)]
        st_e = [Stream(sem(f"s_e{i}")) for i in range(2)]
        st_el = Stream(sem("s_el"))
        st_c = Stream(sem("s_c"))
        st_c2 = Stream(sem("s_c2"))

        S = Sched(nc)
        block = es.enter_context(nc.Block())

        def mm(out, lhsT, rhs, start, stop, reads, writes):
            return S.op(PE, lambda: nc.tensor.matmul(out, lhsT=lhsT, rhs=rhs, start=start, stop=stop),
                        reads=[r.b for r in reads], writes=[w.b for w in writes], last=True)

        def act(fn, reads, writes):
            return S.op(ACT, fn, reads=[r.b for r in reads], writes=[w.b for w in writes])

        def dve(fn, reads, writes):
            return S.op(DVE, fn, reads=[r.b for r in reads], writes=[w.b for w in writes])

        def pool(fn, reads, writes):
            return S.op(POOL, fn, reads=[r.b for r in reads], writes=[w.b for w in writes])

        def A_(out, in_, func, **kw):
            return lambda: nc.scalar.activation(out=out, in_=in_, func=func, **kw)

        unit_list = []
        for t in range(NT):
            for li in range(NL):
                for u in tile_units(modes[(t, li)]):
                    unit_list.append((li, u))
        wstate = {"issued": 0, "consumed": 0, "hold": None}

        def issue_loads(upto):
            if wstate["hold"] is not None:
                upto = min(upto, wstate["hold"] + NSLOT)
            while wstate["issued"] < min(upto, len(unit_list)):
                i = wstate["issued"]
                li, u = unit_list[i]
                n = sizes[u]
                slot = ring[i % NSLOT]
                src = wsd[li, :, int(offs[u]):int(offs[u]) + n]
                S.dma(POOL, slot_streams[i % NSLOT],
                      lambda slot=slot, src=src, n=n: nc.gpsimd.dma_start(out=slot[:, 0:n], in_=src,
                                                                            max_dma_last_dim=8192),
                      writes=[slot.b])
                wstate["issued"] += 1

        def next_unit(expect_u):
            i = wstate["consumed"]
            li, u = unit_list[i]
            assert u == expect_u, (u, expect_u)
            issue_loads(i + NSLOT)
            wstate["consumed"] += 1
            return ring[i % NSLOT]

        def body():
            def cload(dst, src):
                S.dma(SP, st_c, lambda: nc.sync.dma_start(out=dst, in_=src), writes=[])
            cload(flags[:, :], flagd[:, :])
            cload(ident[:, :], constd[:, 0:128])
            S.dma(POOL, st_c2, lambda: nc.gpsimd.dma_start(out=sel[:, :], in_=constd[0:32, 128:128 + 32 * 128],
                                                            max_dma_last_dim=8192), writes=[])
            for l in range(NL):
                cload(gcols[l][:, :], gcold[l])
                cload(gnb[l][:, :], gnbd[l])
                cload(bT[l][:, :], bTd[l])
                cload(aext[:, l * 288:(l + 1) * 288], wrd[l])
                cload(brt[l][:, :], brd[l])
            for l in range(NL):
                S.dma(POOL, st_c2, lambda l=l: nc.gpsimd.dma_start(out=wmT[l][:, :], in_=wmTd[l]), writes=[])
                S.dma(POOL, st_c2, lambda l=l: nc.gpsimd.dma_start(out=wbd[l][:, :], in_=wbdd[l]), writes=[])
            issue_loads(NSLOT)
            for e in (PE, ACT, DVE):
                e.h.wait_ge(st_c.sem, st_c.cnt)
                e.h.wait_ge(st_c2.sem, st_c2.cnt)
            dve(lambda: nc.vector.memset(ones_bf[:, :], 1.0), [], [ones_bf])
            dve(lambda: nc.vector.memset(ones_f[:, :], 1.0), [], [ones_f])
            dve(lambda: nc.vector.memset(bd_ones[:, :], 0.0), [], [bd_ones])
            dve(lambda: nc.vector.memset(bd_ones[0:64, 0:64], 1.0), [], [bd_ones])
            dve(lambda: nc.vector.memset(bd_ones[64:128, 64:128], 1.0), [], [bd_ones])
            for l in range(NL):
                dve(lambda l=l: nc.vector.memset(kTc[l][:, :], 0.0), [], kTck[l])
                dve(lambda l=l: nc.vector.memset(Vaug[l][:, :], 0.0), [], [Vaug[l]])
                dve(lambda l=l: nc.vector.memset(atail[l][:, :], 0.0), [], [atail[l]])
                for g in range(4):
                    dve(lambda l=l, g=g: nc.vector.memset(wmT[l][64:128, g * 128:g * 128 + 64], 0.0), [], [wmT[l]])
                for k in range(8):
                    dve(lambda l=l, k=k: nc.vector.tensor_scalar(
                        out=wr[l][:, k * 36:(k + 1) * 36], in0=aext[:, l * 288 + k * 36:l * 288 + (k + 1) * 36],
                        scalar1=gcols[l][:, C_FFNG + k:C_FFNG + k + 1], scalar2=None, op0=ALU.mult),
                        [aext, gcols[l]], [wr[l]])
            Ev = Etab[:, :].rearrange("p (h q) -> p h q", h=8)
            for l in range(NL):
                for h in range(8):
                    stg = sA if h % 2 == 0 else sB
                    S.dma(SP, st_e[h % 2], lambda stg=stg, h=h, l=l: nc.sync.dma_start(
                        out=stg[:, 0:640], in_=toepd[l, :, h * 640:(h + 1) * 640]), writes=[stg.b])
                    act(A_(Etab[:, h * 640:(h + 1) * 640], stg[:, 0:640], AF.Exp), [stg], [Etab])
                dve(lambda: nc.vector.memset(Ev[0:64, :, 576:640], 0.0), [], [Etab])
                dve(lambda: nc.vector.memset(Ev[64:128, :, 0:64], 0.0), [], [Etab])
                S.dma(SP, st_el, lambda l=l: nc.sync.dma_start(out=escr[l], in_=Etab[:, :]), reads=[Etab.b])
            etab_state = {"cur": NL - 1}
            nc.sync.wait_ge(st_el.sem, st_el.cnt)

            hv_col = flags[:, 0:1]
            one_col = flags[:, 1:2]

            def fm_norm(l, cbase, want_tm=False, src=None):
                if src is None:
                    src = lambda k: (xT[:, k * T:(k + 1) * T], xTk[k])
                ps = psum.alloc()
                for k in range(KC):
                    sq = sqr.next()
                    xa, xb = src(k)
                    act(A_(sq[:, :], xa, AF.Square), [xb], [sq])
                    mm(ps[:, :], ones_bf[:, :], sq[:, :], k == 0, k == KC - 1, [sq, ones_bf], [ps])
                act(A_(rstd[:, :], ps[:, :], AF.Ln, scale=1.0 / D, bias=EPS), [ps], [rstd])
                psum.release(ps)
                act(A_(rstd[:, :], rstd[:, :], AF.Exp, scale=-0.5), [rstd], [rstd])
                for k in range(KC):
                    xa, xb = src(k)
                    dve(lambda k=k, xa=xa: nc.vector.scalar_tensor_tensor(
                        out=hT[:, k * T:(k + 1) * T], in0=xa,
                        scalar=gcols[l][:, cbase + k:cbase + k + 1], in1=rstd[:, :],
                        op0=ALU.mult, op1=ALU.mult), [xb, rstd, gcols[l]], [hTk[k]])
                if want_tm:
                    ps2 = psum.alloc()
                    for tb in range(4):
                        mm(ps2[:, tb:tb + 1], rstd[0:1, tb * 128:(tb + 1) * 128], ones_f[0:1, 0:1],
                           True, True, [rstd, ones_f], [ps2])
                    dve(lambda: nc.vector.tensor_copy(out=rstdtm[:, :], in_=ps2[:, 0:4]), [ps2], [rstdtm])
                    psum.release(ps2)

            def proj_fm(w, wbase, wstride, ncols_off, rhs_tb, rhs_bufs, nk, rhs_stride):
                ps = psum.alloc()
                for kk in range(nk):
                    o = wbase + kk * wstride + ncols_off
                    mm(ps[:, :], w[:, o:o + 128], rhs_tb[:, kk * rhs_stride:kk * rhs_stride + T],
                       kk == 0, kk == nk - 1, [w, rhs_bufs[kk]], [ps])
                return ps

            def proj_h4(w, wbase, wstride, col_offs):
                pss = [psum.alloc() for _ in col_offs]
                for kk in range(KC):
                    for ps, co in zip(pss, col_offs):
                        o = wbase + kk * wstride + co
                        mm(ps[:, :], w[:, o:o + 128], hT[:, kk * T:(kk + 1) * T],
                           kk == 0, kk == KC - 1, [w, hTk[kk]], [ps])
                return pss

            def proj_h(w, wbase, wstride, ncols_off):
                return proj_fm(w, wbase, wstride, ncols_off, hT, hTk, KC, T)

            prefetched = set()
            side_tile = {"t": None}

            def xsrc(t, l, k):
                if l == 0 and side_tile["t"] == t and k >= KC - 2:
                    j = k - (KC - 2)
                    return xside[:, j * T:(j + 1) * T], xsidek[j]
                return xT[:, k * T:(k + 1) * T], xTk[k]

            def load_side(t):
                for j in range(2):
                    k = KC - 2 + j
                    S.dma(SP, st_xs[j], lambda j=j, k=k: nc.sync.dma_start(
                        out=xside[:, j * T:(j + 1) * T], in_=xTd[k * 128:(k + 1) * 128, t * T:(t + 1) * T]),
                        writes=[xsidek[j].b])
                side_tile["t"] = t

            def load_x_chunk(t, k):
                S.dma(SP, st_x[k], lambda: nc.sync.dma_start(
                    out=xT[:, k * T:(k + 1) * T], in_=xTd[k * 128:(k + 1) * 128, t * T:(t + 1) * T]),
                    writes=[xTk[k].b])

            def load_x(t):
                if t in prefetched:
                    return
                for k in range(KC):
                    if side_tile["t"] == t and k >= KC - 2:
                        continue
                    load_x_chunk(t, k)

            def load_p(t, l):
                S.dma(POOL, st_p, lambda: nc.gpsimd.dma_start(
                    out=pTs[:, :].rearrange("p (c t) -> p c t", c=2),
                    in_=pTd[l].rearrange("(c p) t -> p c t", p=128)[:, :, t * T:(t + 1) * T]),
                    writes=[pTs.b])

            def load_E(l):
                if etab_state["cur"] == l:
                    return
                S.dma(SP, st_el, lambda: nc.sync.dma_start(out=Etab[:, :], in_=escr[l]), writes=[Etab.b])
                etab_state["cur"] = l

            def qk_chunks(l, jobs, interleave=False):
                stA = {}
                DEPTH = 2

                def stageA(j):
                    w, c, which, slot_cur = jobs[j]
                    ps = proj_h(w, 0, 512, c * 128)
                    sq = sqr.next()
                    act(A_(sq[:, :], ps[:, :], AF.Square), [ps], [sq])
                    stA[j] = (ps, sq)

                def stageB(j):
                    w, c, which, slot_cur = jobs[j]
                    ps, sq = stA.pop(j)
                    ps2 = psum.alloc()
                    mm(ps2[:, :], bd_ones[:, :], sq[:, :], True, True, [bd_ones, sq], [ps2])
                    rs = rsr.next()
                    act(A_(rs[:, :], ps2[:, :], AF.Ln, scale=1.0 / 64, bias=EPS), [ps2], [rs])
                    psum.release(ps2)
                    act(A_(rs[:, :], rs[:, :], AF.Exp, scale=-0.5), [rs], [rs])
                    if which == 0:
                        dst_tb, dst = qnk[c], qn[:, c * T:(c + 1) * T]
                        gc = gcols[l][:, C_GQ:C_GQ + 1]
                    else:
                        o = (c * 2 + slot_cur) * T
                        dst_tb, dst = kTck[l][c], kTc[l][:, o:o + T]
                        gc = gcols[l][:, C_GK:C_GK + 1]
                    dve(lambda: nc.vector.scalar_tensor_tensor(
                        out=dst, in0=ps[:, :], scalar=gc, in1=rs[:, :], op0=ALU.mult, op1=ALU.mult),
                        [ps, rs, gcols[l]], [dst_tb])
                    psum.release(ps)

                if not interleave:
                    for j in range(len(jobs) + DEPTH):
                        if j < len(jobs):
                            stageA(j)
                        if j >= DEPTH:
                            stageB(j - DEPTH)
                return stageA, stageB

            def v_proj(l, w, slot_cur, flagcol):
                Vv = Vaug[l][:, :].rearrange("p (b c x) -> p b c x", b=8, c=4)
                for tb in range(4):
                    ps = psum.alloc()
                    for kk in range(KC):
                        mm(ps[:, :], hT[:, kk * T + tb * 128:kk * T + (tb + 1) * 128],
                           w[:, kk * 512:(kk + 1) * 512], kk == 0, kk == KC - 1, [hTk[kk], w], [ps])
                    bi = slot_cur * 4 + tb
                    psv = ps[:, :].rearrange("p (c two d) -> p c two d", c=4, two=2)
                    dve(lambda bi=bi, psv=psv: nc.vector.tensor_scalar(
                        out=Vv[:, bi, :, 0:64], in0=psv[:, :, 0, :], scalar1=flagcol, scalar2=None,
                        op0=ALU.mult), [ps, flags], [Vaug[l]])
                    dve(lambda bi=bi, psv=psv: nc.vector.tensor_scalar(
                        out=Vv[:, bi, :, 128:192], in0=psv[:, :, 1, :], scalar1=flagcol, scalar2=None,
                        op0=ALU.mult), [ps, flags], [Vaug[l]])
                    psum.release(ps)
                    act(A_(Vv[:, bi, :, 64:128], ones_f[:, :].rearrange("p (c d) -> p c d", c=4), AF.Copy,
                           scale=flagcol, bias=1e-10), [ones_f, flags], [Vaug[l]])

            def a_proj(w, cc):
                ps = proj_h(w, 0, 512, cc * 128)
                act(A_(aext[:, cc * 528 + 16:cc * 528 + 528], ps[:, :], AF.Copy), [ps], [aext])
                psum.release(ps)

            def save_atail(l):
                for c in range(2):
                    dve(lambda c=c: nc.vector.tensor_copy(out=atail[l][:, c * 16:(c + 1) * 16],
                                                          in_=aext[:, c * 528 + 512:c * 528 + 528]),
                        [aext], [atail[l]])

            def layer_tile_partial(t, l, mode):
                slot_cur = t % 2
                fm_norm(l, C_MIXG)
                if mode == "kva":
                    w = next_unit(0)
                    for cc in range(2):
                        a_proj(w, cc)
                    save_atail(l)
                w = next_unit(3)
                qk_chunks(l, [(w, c, 1, slot_cur) for c in range(4)])
                w = next_unit(4)
                v_proj(l, w, slot_cur, hv_col)

            def layer_tile(t, l, is_halo):
                flagcol = hv_col if is_halo else one_col
                slot_cur = t % 2
                load_E(l)

                fm_norm(l, C_MIXG, src=lambda k: xsrc(t, l, k))

                w = next_unit(0)
                u0ps = proj_h4(w, 0, 512, [0, 128, 256, 384])
                for cc in range(2):
                    act(A_(aext[:, cc * 528 + 16:cc * 528 + 528], u0ps[cc][:, :], AF.Copy), [u0ps[cc]], [aext])
                    psum.release(u0ps[cc])
                for c in range(2):
                    if t == NH:
                        pool(lambda c=c: nc.gpsimd.tensor_tensor(
                            out=aext[:, c * 528:c * 528 + 16], in0=atail[l][:, c * 16:(c + 1) * 16],
                            in1=flags[:, 40:56], op=ALU.mult), [atail[l], flags], [aext])
                    else:
                        pool(lambda c=c: nc.gpsimd.tensor_copy(
                            out=aext[:, c * 528:c * 528 + 16], in_=atail[l][:, c * 16:(c + 1) * 16]),
                            [atail[l]], [aext])

                def shadd(dst, src, c, sh, p0, p1):
                    lo = 2 * sh - 1
                    pool(lambda: nc.gpsimd.tensor_tensor(
                        out=dst[p0:p1, c * 528 + lo:c * 528 + 528],
                        in0=src[p0:p1, c * 528 + lo:c * 528 + 528],
                        in1=src[p0:p1, c * 528 + lo - sh:c * 528 + 528 - sh], op=ALU.add), [src], [dst])

                shadd(sA, aext, 0, 1, 0, 128)
                shadd(sB, sA, 0, 2, 64, 128)
                shadd(sA, aext, 1, 1, 0, 128)
                shadd(sB, sA, 1, 2, 0, 128)
                shadd(sA, sB, 1, 4, 0, 128)
                shadd(sB, sA, 1, 8, 64, 128)
                if t == NH:
                    for c in range(2):
                        for (src, p0, p1) in ((sA, 0, 64), (sB, 64, 128)):
                            pool(lambda c=c, src=src, p0=p0, p1=p1: nc.gpsimd.tensor_tensor(
                                out=src[p0:p1, c * 528 + 16:c * 528 + 32], in0=src[p0:p1, c * 528 + 16:c * 528 + 32],
                                in1=flags[p0:p1, 2 + c * 16:2 + (c + 1) * 16], op=ALU.mult), [src, flags], [src])
                for c in range(2):
                    pool(lambda c=c: nc.gpsimd.tensor_copy(out=atail[l][:, c * 16:(c + 1) * 16],
                                                           in_=aext[:, c * 528 + 512:c * 528 + 528]),
                         [aext], [atail[l]])
                for cc in range(2, 4):
                    ps = u0ps[cc]
                    act(A_(guT[:, (cc - 2) * T:(cc - 1) * T], ps[:, :], AF.Gelu_apprx_tanh), [ps], [guT])
                    psum.release(ps)
                w = next_unit(1)
                vts = []
                for tb in range(4):
                    ps = psum.alloc()
                    for kk in range(KC):
                        mm(ps[:, 0:256], hT[:, kk * T + tb * 128:kk * T + (tb + 1) * 128],
                           w[:, kk * 256:(kk + 1) * 256], kk == 0, kk == KC - 1, [hTk[kk], w], [ps])
                    vt = vtr.next()
                    act(A_(vt[:, :], ps[:, 0:256], AF.Gelu_apprx_tanh), [ps], [vt])
                    psum.release(ps)
                    act(A_(rstd[:, 0:256], vt[:, :], AF.Square, accum_out=vss[:, tb:tb + 1]), [vt], [rstd, vss])
                    vts.append(vt)
                act(A_(vss[:, :], vss[:, :], AF.Ln, scale=1.0 / 256, bias=EPS), [vss], [vss])
                act(A_(vss[:, :], vss[:, :], AF.Exp, scale=-0.5), [vss], [vss])
                for tb in range(4):
                    vt = vts[tb]
                    dve(lambda tb=tb, vt=vt: nc.vector.scalar_tensor_tensor(
                        out=vn[:, tb * 256:(tb + 1) * 256], in0=vt[:, :], scalar=vss[:, tb:tb + 1],
                        in1=gnb[l][:, :], op0=ALU.mult, op1=ALU.mult), [vt, vss, gnb[l]], [vn])
                if INTERLEAVE_QK:
                    w = next_unit(4)
                    v_proj(l, w, slot_cur, flagcol)
                    wstate["hold"] = wstate["consumed"]
                    wq = next_unit(2)
                    wk = next_unit(3)
                    qk_jobs = []
                    for c in range(4):
                        qk_jobs += [(wq, c, 0, slot_cur), (wk, c, 1, slot_cur)]
                    qkA, qkB = qk_chunks(l, qk_jobs, interleave=True)
                    qkA(0)
                    qkA(1)
                    qkB(0)
                    qkB(1)
                else:
                    wstate["hold"] = wstate["consumed"]
                    wq = next_unit(2)
                    wk = next_unit(3)
                    qk_chunks(l, [(wq, c, 0, slot_cur) for c in range(4)] + [(wk, c, 1, slot_cur) for c in range(4)])
                    wstate["hold"] = None
                    w = next_unit(4)
                    v_proj(l, w, slot_cur, flagcol)

                steps = []
                groups = [[3], [0, 2], [1, 6], [7, 5], [4]]
                for h in range(8):
                    for gi, g in enumerate(groups):
                        steps.append((h, g, gi == 0, gi == len(groups) - 1))
                LAG = NROT - 1
                pv_of = {}
                pt_of = {}
                mulcnt = [0]

                def geom(wi):
                    ca, cb = 2 * wi - 8, 2 * wi - 7
                    qlo = max(ca, 0) * 64
                    qhi = (min(cb + 8, 7) + 1) * 64
                    K0 = (wi - 4) * 128
                    return qlo, qhi, K0

                def emit_score(i):
                    h, g, firstg, lastg = steps[i]
                    c, hh = h // 2, h % 2
                    sc = psum.alloc()
                    parts = []
                    off = 0
                    for wi in g:
                        qlo, qhi, K0 = geom(wi)
                        N = qhi - qlo
                        kslot = (1 - slot_cur) if wi < 4 else slot_cur
                        ko = (c * 2 + kslot) * T + (wi % 4) * 128
                        mm(sc[:, off:off + N], kTc[l][hh * 64:(hh + 1) * 64, ko:ko + 128],
                           qn[hh * 64:(hh + 1) * 64, c * T + qlo:c * T + qhi], True, True, [kTck[l][c], qnk[c]], [sc])
                        parts.append((wi, off, N, qlo, qhi, K0))
                        off += N
                    es_ = esr.next()
                    act(A_(es_[:, 0:off], sc[:, 0:off], AF.Exp, scale=0.125), [sc], [es_])
                    psum.release(sc)
                    pt = ptr.next()
                    for (wi, o, N, qlo, qhi, K0) in parts:
                        eo = h * 640 + qlo - K0
                        mulcnt[0] += 1
                        if mulcnt[0] % 3 == 0:
                            pool(lambda: nc.gpsimd.tensor_tensor(out=pt[:, o:o + N], in0=es_[:, o:o + N],
                                                                 in1=Etab[:, eo:eo + N], op=ALU.mult),
                                 [es_, Etab], [pt])
                        else:
                            dve(lambda: nc.vector.tensor_tensor(out=pt[:, o:o + N], in0=es_[:, o:o + N],
                                                                in1=Etab[:, eo:eo + N], op=ALU.mult),
                                [es_, Etab], [pt])
                    pt_of[i] = (pt, parts)

                def emit_pv(i):
                    h, g, firstg, lastg = steps[i]
                    c, hh = h // 2, h % 2
                    if firstg:
                        pv_of[h] = psum.alloc()
                    pv = pv_of[h]
                    pt, parts = pt_of.pop(i)
                    for pi_, (wi, o, N, qlo, qhi, K0) in enumerate(parts):
                        first = firstg and pi_ == 0
                        lastk = lastg and pi_ == len(parts) - 1
                        kslot = (1 - slot_cur) if wi < 4 else slot_cur
                        bi = kslot * 4 + (wi % 4)
                        vo = bi * 768 + c * 192 + (0 if hh == 0 else 64)
                        mm(pv[:, qlo:qhi], Vaug[l][:, vo:vo + 128], pt[:, o:o + N], first, lastk, [Vaug[l], pt], [pv])
                    if lastg:
                        n0, n1 = (0, 64) if hh == 0 else (64, 128)
                        d0, d1 = (64, 128) if hh == 0 else (0, 64)
                        act(A_(rden[n0:n1, :], pv[d0:d1, :], AF.Ln), [pv], [rden])
                        act(A_(rden[n0:n1, :], rden[n0:n1, :], AF.Exp, scale=-1.0), [rden], [rden])
                        dve(lambda: nc.vector.tensor_tensor(out=ycT[n0:n1, c * T:(c + 1) * T], in0=pv[n0:n1, :],
                                                            in1=rden[n0:n1, :], op=ALU.mult), [pv, rden], [ycTk[c]])
                        psum.release(pv)

                def emit_pooled():
                    for c in range(2):
                        for (src, p0, p1) in ((sA, 0, 64), (sB, 64, 128)):
                            dve(lambda c=c, src=src, p0=p0, p1=p1: nc.vector.scalar_tensor_tensor(
                                out=pooled[p0:p1, c * T:(c + 1) * T], in0=src[p0:p1, c * 528 + 16:c * 528 + 528],
                                scalar=gcols[l][p0:p1, C_INVW + c:C_INVW + c + 1],
                                in1=aext[p0:p1, c * 528 + 16:c * 528 + 528], op0=ALU.mult, op1=ALU.subtract),
                                [src, aext, gcols[l]], [pooled])

                inject = {}
                for c in (range(1, 4) if INTERLEAVE_QK else ()):
                    b0 = 16 * (c - 1)
                    inject[b0 + 1] = (qkA, 2 * c)
                    inject[b0 + 5] = (qkA, 2 * c + 1)
                    inject[b0 + 9] = (qkB, 2 * c)
                    inject[b0 + 13] = (qkB, 2 * c + 1)
                for i in range(len(steps) + LAG):
                    if i in inject:
                        inject[i][0](inject[i][1])
                    if i == 15:
                        emit_pooled()
                    if i < len(steps):
                        emit_score(i)
                    if i >= LAG:
                        emit_pv(i - LAG)
                wstate["hold"] = None

                for c in range(2):
                    ps = psum.alloc()
                    mm(ps[:, :], wbd[l][:, c * 128:(c + 1) * 128], pooled[:, c * T:(c + 1) * T], True, True,
                       [wbd[l], pooled], [ps])
                    act(A_(yaT[:, c * T:(c + 1) * T], ps[:, :], AF.Copy,
                           scale=gcols[l][:, C_PSCALE + c:C_PSCALE + c + 1]), [ps, gcols[l]], [yaT])
                    psum.release(ps)

                for c in range(2):
                    ps = psum.alloc()
                    for blk in range(4):
                        for gg in range(2):
                            g = 2 * c + gg
                            mm(ps[gg * 64:(gg + 1) * 64, blk * 128:(blk + 1) * 128],
                               vn[:, blk * 256 + g * 64:blk * 256 + (g + 1) * 64],
                               wmT[l][:, g * 128:(g + 1) * 128], True, True, [vn, wmT[l]], [ps])
                    mt = mtr.next()
                    for blk in range(4):
                        dve(lambda blk=blk, mt=mt, ps=ps, c=c: nc.vector.tensor_tensor(
                            out=mt[:, blk * 128:(blk + 1) * 128], in0=ps[:, blk * 128:(blk + 1) * 128],
                            in1=bT[l][:, c * 128:(c + 1) * 128], op=ALU.add), [ps, bT[l]], [mt])
                    psum.release(ps)
                    dve(lambda mt=mt, c=c: nc.vector.tensor_tensor(
                        out=ybT[:, c * T:(c + 1) * T], in0=mt[:, :], in1=guT[:, c * T:(c + 1) * T], op=ALU.mult),
                        [mt, guT], [ybT])

                ybr = ((yaT, [yaT] * 2), (ybT, [ybT] * 2), (ycT, ycTk))
                nkb = (2, 2, 4)
                boff = (0, 256, 512)
                for dc in range(KC):
                    w = next_unit(5 + dc)
                    macc = None
                    for br in range(3):
                        psg = proj_h(w, br * 1024, 128, 0)
                        sg = sigr.next()
                        act(A_(sg[:, :], psg[:, :], AF.Sigmoid,
                               bias=gcols[l][:, C_GATEB + br * 8 + dc:C_GATEB + br * 8 + dc + 1]),
                            [psg, gcols[l]], [sg])
                        psum.release(psg)
                        psb = proj_fm(w, 3072 + boff[br], 128, 0, ybr[br][0], ybr[br][1], nkb[br], T)
                        if br == 0:
                            macc = mtr.next()
                            dve(lambda psb=psb, sg=sg, macc=macc: nc.vector.tensor_tensor(
                                out=macc[:, :], in0=psb[:, :], in1=sg[:, :], op=ALU.mult), [psb, sg], [macc])
                        else:
                            tmp = mtr.next()
                            dve(lambda psb=psb, sg=sg, tmp=tmp: nc.vector.tensor_tensor(
                                out=tmp[:, :], in0=psb[:, :], in1=sg[:, :], op=ALU.mult), [psb, sg], [tmp])
                            if br == 1:
                                dve(lambda tmp=tmp, macc=macc: nc.vector.tensor_tensor(
                                    out=macc[:, :], in0=macc[:, :], in1=tmp[:, :], op=ALU.add), [macc, tmp], [macc])
                            else:
                                dve(lambda tmp=tmp, macc=macc, dc=dc: nc.vector.tensor_tensor(
                                    out=merged[:, dc * T:(dc + 1) * T], in0=macc[:, :], in1=tmp[:, :], op=ALU.add),
                                    [macc, tmp], [mergedk[dc]])
                        psum.release(psb)
                for half in range(2):
                    w = next_unit(13 + half)
                    for o4 in range(4):
                        oc = half * 4 + o4
                        ps = proj_fm(w, 0, 512, o4 * 128, merged, mergedk, KC, T)
                        xa, xb = xsrc(t, l, oc)
                        dve(lambda ps=ps, oc=oc, xa=xa: nc.vector.tensor_tensor(
                            out=xT[:, oc * T:(oc + 1) * T], in0=xa, in1=ps[:, :], op=ALU.add),
                            [ps, xb, xTk[oc]], [xTk[oc]])
                        psum.release(ps)
                if l == NL - 1 and t >= NH and t + 1 < NT:
                    load_side(t + 1)

                load_p(t, l)

                fm_norm(l, C_FFNG, want_tm=True)
                lps = []
                for tb in range(4):
                    ps = psum.alloc()
                    for kk in range(KC):
                        mm(ps[:, 0:36], xT[:, kk * T + tb * 128:kk * T + (tb + 1) * 128],
                           wr[l][:, kk * 36:(kk + 1) * 36], kk == 0, kk == KC - 1, [xTk[kk], wr[l]], [ps])
                    lps.append(ps)
                gts = []
                for tb in range(4):
                    ps = lps[tb]
                    dve(lambda ps=ps, tb=tb: nc.vector.scalar_tensor_tensor(
                        out=lg[:, :], in0=ps[:, 0:36], scalar=rstdtm[:, tb:tb + 1], in1=brt[l][:, :],
                        op0=ALU.mult, op1=ALU.add), [ps, rstdtm, brt[l]], [lg])
                    psum.release(ps)
                    sm = rsm.next()
                    dve(lambda sm=sm: nc.vector.reduce_max(out=sm[:, 0:1], in_=lg[:, 0:4], axis=AX.X), [lg], [sm])
                    dve(lambda sm=sm: nc.vector.tensor_scalar(out=ohg[:, :], in0=lg[:, 0:4], scalar1=sm[:, 0:1],
                                                               scalar2=None, op0=ALU.is_ge), [lg, sm], [ohg])
                    dve(lambda sm=sm: nc.vector.tensor_scalar(out=sm[:, 1:2], in0=sm[:, 0:1], scalar1=-1.0,
                                                               scalar2=None, op0=ALU.mult), [sm], [sm])
                    act(A_(gexp[:, :], lg[:, 0:4], AF.Exp, bias=sm[:, 1:2], accum_out=sm[:, 2:3]), [lg, sm], [gexp, sm])
                    dve(lambda: nc.vector.tensor_scalar(out=le[:, :], in0=lg[:, 4:12], scalar1=ohg[:, 0:1],
                                                        scalar2=None, op0=ALU.mult), [lg, ohg], [le])
                    for g in range(1, 4):
                        dve(lambda g=g: nc.vector.scalar_tensor_tensor(
                            out=le[:, :], in0=lg[:, 4 + g * 8:12 + g * 8], scalar=ohg[:, g:g + 1], in1=le[:, :],
                            op0=ALU.mult, op1=ALU.add), [lg, ohg, le], [le])
                    dve(lambda: nc.vector.max(out=m8[:, :], in_=le[:, :]), [le], [m8])
                    dve(lambda sm=sm: nc.vector.tensor_scalar(out=sm[:, 3:4], in0=m8[:, 0:1], scalar1=-1.0,
                                                               scalar2=None, op0=ALU.mult), [m8], [sm])
                    act(A_(ee[:, :], le[:, :], AF.Exp, bias=sm[:, 3:4]), [le, sm], [ee])
                    dve(lambda sm=sm: nc.vector.scalar_tensor_tensor(
                        out=wsel[:, :], in0=le[:, :], scalar=m8[:, 1:2], in1=ee[:, :], op0=ALU.is_ge, op1=ALU.mult,
                        accum_out=sm[:, 4:5]), [le, m8, ee], [wsel, sm])
                    dve(lambda sm=sm: nc.vector.tensor_tensor(out=sm[:, 5:6], in0=sm[:, 4:5], in1=sm[:, 2:3],
                                                               op=ALU.mult), [sm], [sm])
                    dve(lambda sm=sm: nc.vector.reciprocal(out=sm[:, 5:6], in_=sm[:, 5:6]), [sm], [sm])
                    gt_ = gates.next()
                    for g in range(4):
                        dve(lambda g=g, gt_=gt_, sm=sm: nc.vector.tensor_scalar(
                            out=gt_[:, g * 8:(g + 1) * 8], in0=wsel[:, :], scalar1=ohg[:, g:g + 1],
                            scalar2=sm[:, 5:6], op0=ALU.mult, op1=ALU.mult), [wsel, ohg, sm], [gt_])
                    gts.append(gt_)

                def emit_gatesT():
                    gps = psum.alloc()
                    for tb in range(4):
                        gt_ = gts[tb]
                        S.op(PE, lambda gt_=gt_, tb=tb: nc.tensor.transpose(
                            out=gps[0:32, tb * 128:(tb + 1) * 128], in_=gt_[:, :], identity=ident[:, :]),
                            reads=[gt_.b, ident.b], writes=[gps.b])
                    act(A_(gatesT[:, :], gps[0:32, :], AF.Copy), [gps], [gatesT])
                    psum.release(gps)

                ub = 15
                for pi in range(NPASS):
                    for j in range(EPP):
                        if j % 2 == 0:
                            w = next_unit(ub + pi * (EPP // 2 + 2) + j // 2)
                        base = (j % 2) * 2048
                        psg = proj_h(w, base, 256, 0)
                        psu = proj_h(w, base, 256, 128)
                        sl = sigr.next()
                        act(A_(sl[:, :], psg[:, :], AF.Silu), [psg], [sl])
                        psum.release(psg)
                        dve(lambda psu=psu, sl=sl, j=j: nc.vector.tensor_tensor(
                            out=merged[:, j * T:(j + 1) * T], in0=psu[:, :], in1=sl[:, :], op=ALU.mult),
                            [psu, sl], [mergedk[j]])
                        psum.release(psu)
                    if pi == 0:
                        emit_gatesT()
                    for j in range(EPP):
                        e = pi * EPP + j
                        psb = psum.alloc()
                        mm(psb[:, :], sel[0:32, e * 128:(e + 1) * 128], gatesT[0:32, :], True, True,
                           [sel, gatesT], [psb])
                        dve(lambda psb=psb, j=j: nc.vector.tensor_tensor(
                            out=merged[:, j * T:(j + 1) * T], in0=psb[:, :], in1=merged[:, j * T:(j + 1) * T],
                            op=ALU.mult), [psb, mergedk[j]], [mergedk[j]])
                        psum.release(psb)
                    for half in range(2):
                        w = next_unit(ub + pi * (EPP // 2 + 2) + EPP // 2 + half)
                        for o4 in range(4):
                            oc = half * 4 + o4
                            ps = proj_fm(w, 0, 512, o4 * 128, merged, mergedk, EPP, T)
                            dve(lambda ps=ps, oc=oc: nc.vector.tensor_tensor(
                                out=xT[:, oc * T:(oc + 1) * T], in0=xT[:, oc * T:(oc + 1) * T], in1=ps[:, :],
                                op=ALU.add), [ps, xTk[oc]], [xTk[oc]])
                            psum.release(ps)

                fm_norm(l, C_PLEG)
                ub2 = 15 + NPASS * (EPP // 2 + 2)
                wstate["hold"] = wstate["consumed"]
                wpi = next_unit(ub2)
                last_layer = (l == NL - 1)
                for half in range(2):
                    w = next_unit(ub2 + 1 + half)
                    pre = proj_h4(w, 0, 512, [0, 128, 256, 384]) if half == 0 else None
                    for o4 in range(4):
                        oc = half * 4 + o4
                        psg = pre[o4] if pre is not None else proj_h(w, 0, 512, o4 * 128)
                        sg = sigr.next()
                        act(A_(sg[:, :], psg[:, :], AF.Sigmoid), [psg], [sg])
                        psum.release(psg)
                        pse = proj_fm(wpi, 0, 1024, oc * 128, pTs, [pTs] * 2, 2, T)
                        tmp = mtr.next()
                        dve(lambda pse=pse, sg=sg, tmp=tmp: nc.vector.tensor_tensor(
                            out=tmp[:, :], in0=pse[:, :], in1=sg[:, :], op=ALU.mult), [pse, sg], [tmp])
                        psum.release(pse)
                        dve(lambda tmp=tmp, oc=oc: nc.vector.tensor_tensor(
                            out=xT[:, oc * T:(oc + 1) * T], in0=xT[:, oc * T:(oc + 1) * T], in1=tmp[:, :],
                            op=ALU.add), [tmp, xTk[oc]], [xTk[oc]])
                        if last_layer and t >= NH:
                            to = t - NH
                            S.dma(SP, st_y[oc], lambda oc=oc, to=to: nc.sync.dma_start(
                                out=yTd[oc * 128:(oc + 1) * 128, to * T:(to + 1) * T],
                                in_=xT[:, oc * T:(oc + 1) * T]), reads=[xTk[oc].b])
                            if t + 1 < NT and 1 <= oc <= KC - 2:
                                load_x_chunk(t + 1, oc - 1)
                if last_layer and t >= NH and t + 1 < NT:
                    prefetched.add(t + 1)
                wstate["hold"] = None

            for t in range(NT):
                if any(modes[(t, l)] != "skip" for l in range(NL)):
                    load_x(t)
                for l in range(NL):
                    m = modes[(t, l)]
                    if m == "full":
                        layer_tile(t, l, t < NH)
                    elif m in ("kva", "kv"):
                        layer_tile_partial(t, l, m)
            for k in range(KC):
                nc.sync.wait_ge(st_y[k].sem, st_y[k].cnt)

        @block.sync
        def _(sync):
            body()
        assert wstate["consumed"] == len(unit_list) and wstate["issued"] == len(unit_list)
    return nc


_PROG_CACHE = {}


def _get_prog(NL, NH, NOWN):
    key = (NL, NH, NOWN)
    if key not in _PROG_CACHE:
        _PROG_CACHE[key] = build_program(NL, NH, NOWN)
    return _PROG_CACHE[key]


def _consts():
    c = np.zeros((128, 128 + 32 * 128), np.float32)
    c[:, 0:128] = np.eye(128, dtype=np.float32)
    for e in range(32):
        c[e, 128 + e * 128:128 + (e + 1) * 128] = 1.0
    return c


def _flags(first_half):
    f = np.zeros((128, 56), np.float32)
    f[:, 0] = 0.0 if first_half else 1.0
    f[:, 40:56] = 0.0 if first_half else 1.0
    f[:, 1] = 1.0
    wins = (2, 4, 8, 16)
    for c in range(2):
        for p in range(128):
            w = wins[2 * c + p // 64]
            for i in range(16):
                f[p, 2 + c * 16 + i] = (w / min(i + 1, w)) if first_half else 1.0
    return f


def _layer_tables(inp, l):
    f32 = np.float32
    g = np.zeros((128, NCOL), f32)
    g[:, C_MIXG:C_MIXG + 8] = inp["mix_norm_g"][l].reshape(8, 128).T
    g[:, C_FFNG:C_FFNG + 8] = inp["ffn_norm_g"][l].reshape(8, 128).T
    g[:, C_PLEG:C_PLEG + 8] = inp["ple_norm_g"][l].reshape(8, 128).T
    g[:, C_PSCALE:C_PSCALE + 2] = inp["pool_scale"][l].reshape(2, 128).T
    wins = np.array([2, 4, 8, 16], f32)
    for c in range(2):
        g[0:64, C_INVW + c] = 1.0 / wins[2 * c]
        g[64:128, C_INVW + c] = 1.0 / wins[2 * c + 1]
    g[:, C_GQ] = np.tile(inp["q_norm_g"][l], 2)
    g[:, C_GK] = np.tile(inp["k_norm_g"][l], 2)
    g[:, C_GATEB:C_GATEB + 24] = inp["gate_b"][l].reshape(3, 8, 128).transpose(2, 0, 1).reshape(128, 24)
    gnb = np.broadcast_to(inp["sgu_norm_g"][l][None, :], (128, 256)).astype(f32)
    sb_ = inp["sgu_b"][l]
    bT = np.zeros((128, 256), f32)
    for c in range(2):
        bT[0:64, c * 128:(c + 1) * 128] = sb_[2 * c][None, :]
        bT[64:128, c * 128:(c + 1) * 128] = sb_[2 * c + 1][None, :]
    wmT = inp["sgu_w"][l].transpose(2, 0, 1).reshape(128, 512).astype(f32)
    wbd = np.zeros((128, 256), f32)
    for c in range(2):
        for gp in range(2):
            wbd[gp * 64:(gp + 1) * 64, c * 128 + gp * 64:c * 128 + (gp + 1) * 64] = inp["pool_w"][l][2 * c + gp]
    wrt = np.concatenate([inp["w_group_router"][l], inp["w_expert_router"][l]], axis=1)
    wr = wrt.reshape(8, 128, 36).transpose(1, 0, 2).reshape(128, 288).astype(f32)
    br = np.broadcast_to(np.concatenate([inp["b_group_router"][l], inp["b_expert_router"][l]])[None, :],
                         (128, 36)).astype(f32)
    kk = np.arange(128)[:, None]
    qq = np.arange(640)[None, :]
    idx = np.clip(qq - kk, -128, 128) + 128
    toep = inp["rel_bias"][l][:, idx].transpose(1, 0, 2).reshape(128, 8 * 640).astype(f32)
    return dict(gcols=g, gnb=gnb, sgubT=bT, sguwT=wmT, poolbd=wbd, wrouter=wr, brouter=br, toep=toep)


def _run(inp, x, layers, NH):
    NL = len(layers)
    B, S_, _ = x.shape
    NOWN = S_ // 2 // T
    NT = NH + NOWN
    ncores = 2 * B
    nc = _get_prog(NL, NH, NOWN)
    tabs = [_layer_tables(inp, l) for l in layers]
    shared = {k: np.ascontiguousarray(np.stack([tb[k] for tb in tabs])) for k in tabs[0]}
    shared["wstream"] = np.ascontiguousarray(np.stack([
        pack_layer_weights(inp["w_in"][l], inp["w_branch_a"][l], inp["w_branch_b"][l], inp["w_branch_c"][l],
                           inp["w_out"][l], inp["w_expert_in"][l], inp["w_expert_out"][l],
                           inp["w_ple_in"][l], inp["w_ple_gate"][l]) for l in layers]))
    shared["consts"] = _consts()
    in_maps = []
    for c in range(ncores):
        b, half = c // 2, c % 2
        lo = half * (S_ // 2) - NH * T
        xs = np.zeros((NT * T, D), np.float32)
        ps_ = np.zeros((NL, NT * T, 256), np.float32)
        src_lo = max(lo, 0)
        xs[src_lo - lo:] = x[b, src_lo:lo + NT * T]
        for i, l in enumerate(layers):
            ps_[i, src_lo - lo:] = inp["p"][l, b, src_lo:lo + NT * T]
        m = dict(shared)
        m["xT"] = np.ascontiguousarray(xs.T)
        m["pT"] = np.ascontiguousarray(ps_.transpose(0, 2, 1))
        m["flags"] = _flags(half == 0)
        in_maps.append(m)
    res = RUNNER(nc, in_maps, core_ids=list(range(ncores)))
    out = np.zeros((B, S_, D), np.float32)
    for c in range(ncores):
        b, half = c // 2, c % 2
        out[b, half * (S_ // 2):(half + 1) * (S_ // 2)] = res.results[c]["yT"].T
    return out


FUSED = True


def RUNNER(nc, in_maps, core_ids):
    return run_bass_kernel_spmd(nc, in_maps, core_ids=core_ids)


def kernel(**inputs):
    inp = {k: np.asarray(v) for k, v in inputs.items()}
    x = inp["x"].astype(np.float32, copy=False)
    if FUSED:
        return _run(inp, x, [0, 1], 2)
    for l in range(2):
        x = _run(inp, x, [l], 1)
    return x
```
